# Optimizing a Trainium2 kernel written in Bass

```python
import math
import jax, jax.numpy as jnp
from jax import lax
import numpy as np

D_MODEL = 2048
BATCH = 1
SEQ = 8192
DEPTH = 2

CONV_CH = D_MODEL // 4
NSA_HEAD_DIM = 128
NSA_HEADS = D_MODEL // 256
NSA_KV_HEADS = NSA_HEADS // 4
NSA_WIDTH = NSA_HEADS * NSA_HEAD_DIM
NSA_KV = NSA_KV_HEADS * NSA_HEAD_DIM
RET_HEADS = D_MODEL // 512
RET_KEY_DIM = 128
RET_VAL_DIM = 128
RET_QK = RET_HEADS * RET_KEY_DIM
RET_WIDTH = RET_HEADS * RET_VAL_DIM
MIX_WIDTH = CONV_CH + NSA_WIDTH + RET_WIDTH
IN_SPLITS = (CONV_CH, CONV_CH, NSA_WIDTH, NSA_KV, NSA_KV, NSA_KV, NSA_KV, NSA_KV, NSA_KV, 3 * NSA_HEADS,
             RET_QK, RET_QK, RET_WIDTH, RET_WIDTH)
IN_WIDTH = 2 * CONV_CH + NSA_WIDTH + 6 * NSA_KV + 3 * NSA_HEADS + 2 * RET_QK + 2 * RET_WIDTH

CONV_WIDTH = 31
CMP_BLOCK = 32
CMP_STRIDE = 16
SEL_BLOCK = 64
SEL_TOPN = 16
WINDOW = 512
Q_BLOCK = 128
ROPE_THETA = 500000.0
ROPE_DIM = NSA_HEAD_DIM // 4
RET_CHUNK = 128
RET_THETA = 10000.0
N_EXPERTS = 32
TOP_K = 4
D_EXPERT = D_MODEL
SWIGLU_LIMIT = 7.0
SWIGLU_ALPHA = 1.702
MOE_ROWS = 256

NORM_EPS = 1e-6
NEG = -1e30
BIG = 1e30
F32 = jnp.float32

kernel_name = 'hybrid_conv_nsa_retention_moe_adaln'


def rms_norm(x, g):
    xf = x.astype(F32)
    y = xf * lax.rsqrt(jnp.mean(xf * xf, axis=-1, keepdims=True) + NORM_EPS)
    return (y * g.astype(F32)).astype(x.dtype)


def layer_norm(x, g, b):
    xf = x.astype(F32)
    mu = jnp.mean(xf, axis=-1, keepdims=True)
    var = jnp.mean(jnp.square(xf - mu), axis=-1, keepdims=True)
    y = (xf - mu) * lax.rsqrt(var + NORM_EPS) * g.astype(F32) + b.astype(F32)
    return y.astype(x.dtype)


def rotary(x, pos, rot_dim, theta):
    half = rot_dim // 2
    inv = theta ** (-jnp.arange(half, dtype=F32) * 2.0 / rot_dim)
    ang = pos.astype(F32)[:, None] * inv[None, :]
    cos, sin = jnp.cos(ang), jnp.sin(ang)
    xf = x.astype(F32)
    x1, x2 = xf[..., :half], xf[..., half:rot_dim]
    out = jnp.concatenate([x1 * cos - x2 * sin, x2 * cos + x1 * sin, xf[..., rot_dim:]], axis=-1)
    return out.astype(x.dtype)


def split_heads(t, n):
    b, s, _ = t.shape
    return t.reshape(b, s, n, -1).transpose(0, 2, 1, 3)


def split_cols(t, sizes):
    out, off = [], 0
    for s in sizes:
        out.append(t[..., off:off + s])
        off += s
    return out


def masked_softmax(s, mask):
    return jax.nn.softmax(jnp.where(mask, s, NEG), axis=-1)


def conv_module(val, gate, dw_w, dw_b, ln_g, ln_b):
    h = val * jax.nn.sigmoid(gate)
    h = lax.conv_general_dilated(h, dw_w[:, None, :].astype(h.dtype), window_strides=(1,),
                                 padding=[(CONV_WIDTH - 1, 0)],
                                 dimension_numbers=('NWC', 'WIO', 'NWC'),
                                 feature_group_count=CONV_CH) + dw_b
    h = layer_norm(h, ln_g, ln_b)
    return jax.nn.silu(h)


def nsa_attention(q, k_cmp, v_cmp, k_sel, v_sel, k_win, v_win, gates,
                  pe_k, pe_v, cmp_k_w1, cmp_k_w2, cmp_v_w1, cmp_v_w2):
    B, S, _ = q.shape
    hd, hkv = NSA_HEAD_DIM, NSA_KV_HEADS
    grp = NSA_HEADS // NSA_KV_HEADS
    scale = hd ** -0.5
    pos = jnp.arange(S)
    q = rotary(split_heads(q, NSA_HEADS), pos, ROPE_DIM, ROPE_THETA)
    k_cmp = rotary(split_heads(k_cmp, hkv), pos, ROPE_DIM, ROPE_THETA)
    k_sel = rotary(split_heads(k_sel, hkv), pos, ROPE_DIM, ROPE_THETA)
    k_win = rotary(split_heads(k_win, hkv), pos, ROPE_DIM, ROPE_THETA)
    v_cmp, v_sel, v_win = split_heads(v_cmp, hkv), split_heads(v_sel, hkv), split_heads(v_win, hkv)

    n_cmp = (S - CMP_BLOCK) // CMP_STRIDE + 1
    cmp_idx = np.arange(n_cmp)[:, None] * CMP_STRIDE + np.arange(CMP_BLOCK)[None, :]
    cmp_end = jnp.asarray(cmp_idx[:, -1])

    def compress(t, pe, w1, w2):
        blocks = t[:, :, cmp_idx, :] + pe
        flat = blocks.reshape(B, hkv, n_cmp, CMP_BLOCK * hd)
        return jax.nn.silu(flat @ w1) @ w2

    kc = compress(k_cmp, pe_k, cmp_k_w1, cmp_k_w2)
    vc = compress(v_cmp, pe_v, cmp_v_w1, cmp_v_w2)

    n_sel = S // SEL_BLOCK
    top_n = min(SEL_TOPN, n_sel)
    sel_start = np.arange(n_sel) * SEL_BLOCK
    cover = jnp.asarray(((cmp_idx[:, :1] <= sel_start[None, :] + SEL_BLOCK - 1)
                         & (cmp_idx[:, -1:] >= sel_start[None, :])).astype(np.float32))
    k_sel_blk = k_sel.reshape(B, hkv, n_sel, SEL_BLOCK, hd)
    v_sel_blk = v_sel.reshape(B, hkv, n_sel, SEL_BLOCK, hd)
    b_ix = jnp.arange(B)[:, None, None, None]
    h_ix = jnp.arange(hkv)[None, :, None, None]

    pad = ((0, 0), (0, 0), (WINDOW, 0), (0, 0))
    k_win_p, v_win_p = jnp.pad(k_win, pad), jnp.pad(v_win, pad)

    def query_block(qb):
        q0 = qb * Q_BLOCK
        qg = lax.dynamic_slice_in_dim(q, q0, Q_BLOCK, axis=2).reshape(B, hkv, grp, Q_BLOCK, hd)
        tpos = q0 + jnp.arange(Q_BLOCK)
        s = jnp.einsum('bkgqd,bknd->bkgqn', qg, kc).astype(F32) * scale
        mc = cmp_end[None, :] <= tpos[:, None]
        p = masked_softmax(s, mc) * jnp.any(mc, axis=-1)[:, None].astype(F32)
        o_c = jnp.einsum('bkgqn,bknd->bkgqd', p.astype(vc.dtype), vc)
        imp = jnp.einsum('bkgqn,nj->bkqj', p, cover)
        blk = jnp.arange(n_sel)
        imp = jnp.where(blk[None, :] * SEL_BLOCK <= tpos[:, None], imp, NEG)
        forced = (blk[None, :] == 0) | (blk[None, :] == (tpos // SEL_BLOCK)[:, None])
        imp = jnp.where(forced, BIG, imp)
        top_v, top_i = lax.top_k(imp, top_n)
        kg = k_sel_blk[b_ix, h_ix, top_i].reshape(B, hkv, Q_BLOCK, top_n * SEL_BLOCK, hd)
        vg = v_sel_blk[b_ix, h_ix, top_i].reshape(B, hkv, Q_BLOCK, top_n * SEL_BLOCK, hd)
        kpos = top_i[..., None] * SEL_BLOCK + jnp.arange(SEL_BLOCK)
        ms = ((top_v > 0.5 * NEG)[..., None] & (kpos <= tpos[:, None, None]))
        ms = ms.reshape(B, hkv, Q_BLOCK, top_n * SEL_BLOCK)
        s = jnp.einsum('bkgqd,bkqmd->bkgqm', qg, kg).astype(F32) * scale
        p = masked_softmax(s, ms[:, :, None])
        o_s = jnp.einsum('bkgqm,bkqmd->bkgqd', p.astype(vg.dtype), vg)
        kw = lax.dynamic_slice_in_dim(k_win_p, q0, WINDOW + Q_BLOCK, axis=2)
        vw = lax.dynamic_slice_in_dim(v_win_p, q0, WINDOW + Q_BLOCK, axis=2)
        kpw = q0 - WINDOW + jnp.arange(WINDOW + Q_BLOCK)
        mw = ((kpw[None, :] <= tpos[:, None]) & (kpw[None, :] > tpos[:, None] - WINDOW)
              & (kpw[None, :] >= 0))
        s = jnp.einsum('bkgqd,bkmd->bkgqm', qg, kw).astype(F32) * scale
        p = masked_softmax(s, mw)
        o_w = jnp.einsum('bkgqm,bkmd->bkgqd', p.astype(vw.dtype), vw)
        return o_c, o_s, o_w

    o_c, o_s, o_w = lax.map(query_block, jnp.arange(S // Q_BLOCK))

    def merge(o):
        return o.transpose(1, 0, 4, 2, 3, 5).reshape(B, S, NSA_HEADS, hd)

    g = jax.nn.sigmoid(gates.astype(F32)).reshape(B, S, 3, NSA_HEADS, 1).astype(q.dtype)
    o = g[:, :, 0] * merge(o_c) + g[:, :, 1] * merge(o_s) + g[:, :, 2] * merge(o_w)
    return o.reshape(B, S, NSA_WIDTH)


def retention(q, k, v, g, gn_g, gn_b):
    B, S, _ = q.shape
    H, dk, dv, C = RET_HEADS, RET_KEY_DIM, RET_VAL_DIM, RET_CHUNK
    n_ch = S // C
    pos = jnp.arange(S)
    q = rotary(split_heads(q, H), pos, dk, RET_THETA).astype(F32)
    k = rotary(split_heads(k, H), pos, dk, RET_THETA).astype(F32) * (dk ** -0.5)
    v = split_heads(v, H).astype(F32)
    qc = q.reshape(B, H, n_ch, C, dk)
    kc = k.reshape(B, H, n_ch, C, dk)
    vc = v.reshape(B, H, n_ch, C, dv)
    log_g = jnp.log(1.0 - 2.0 ** (-5.0 - jnp.arange(H, dtype=F32)))
    i = jnp.arange(C, dtype=F32)
    diff = i[:, None] - i[None, :]
    dmat = jnp.where(diff >= 0, jnp.exp(log_g[:, None, None] * jnp.maximum(diff, 0.0)), 0.0)
    inner = jnp.einsum('bhncd,bhnmd->bhncm', qc, kc) * dmat[:, None]
    inner = jnp.einsum('bhncm,bhnme->bhnce', inner, vc)
    zeta = jnp.exp(log_g[:, None] * (C - 1.0 - i))
    kv = jnp.einsum('bhnmd,bhnme->bhnde', kc * zeta[:, None, :, None], vc)
    decay_c = jnp.exp(log_g * C)[:, None, None]

    def step(state, kv_n):
        return decay_c * state + kv_n, state

    _, s_prev = lax.scan(step, jnp.zeros((B, H, dk, dv), F32), jnp.moveaxis(kv, 2, 0))
    s_prev = jnp.moveaxis(s_prev, 0, 2)
    xi = jnp.exp(log_g[:, None] * (i + 1.0))
    cross = jnp.einsum('bhncd,bhnde->bhnce', qc, s_prev) * xi[:, None, :, None]
    o = (inner + cross).reshape(B, H, S, dv)
    mu = jnp.mean(o, axis=-1, keepdims=True)
    var = jnp.mean(jnp.square(o - mu), axis=-1, keepdims=True)
    o = ((o - mu) * lax.rsqrt(var + NORM_EPS)).transpose(0, 2, 1, 3).reshape(B, S, RET_WIDTH)
    o = o * gn_g.astype(F32) + gn_b.astype(F32)
    return (o * jax.nn.silu(g.astype(F32))).astype(g.dtype)


def hybrid_mixer(h, w_in, w_out, conv_dw_w, conv_dw_b, conv_ln_g, conv_ln_b,
                 pe_k, pe_v, cmp_k_w1, cmp_k_w2, cmp_v_w1, cmp_v_w2, ret_gn_g, ret_gn_b):
    proj = h @ w_in
    (cv, cg, nq, nkc, nvc, nks, nvs, nkw, nvw, ngt, rq, rk, rv, rg) = split_cols(proj, IN_SPLITS)
    y_conv = conv_module(cv, cg, conv_dw_w, conv_dw_b, conv_ln_g, conv_ln_b)
    y_nsa = nsa_attention(nq, nkc, nvc, nks, nvs, nkw, nvw, ngt,
                          pe_k, pe_v, cmp_k_w1, cmp_k_w2, cmp_v_w1, cmp_v_w2)
    y_ret = retention(rq, rk, rv, rg, ret_gn_g, ret_gn_b)
    y = jnp.concatenate([y_conv, y_nsa, y_ret], axis=-1)
    return y @ w_out


def moe_ffn(h, router_w, router_b, w_gu, b_gu, w_dn, b_dn):
    B, S, D = h.shape
    T = B * S
    xf = h.reshape(T, D)
    logits = (xf @ router_w + router_b).astype(F32)
    top_val, top_exp = lax.top_k(logits, TOP_K)
    gate = jax.nn.softmax(top_val, axis=-1)
    n_assign = T * TOP_K
    flat_e = top_exp.reshape(-1)
    flat_t = jnp.repeat(jnp.arange(T, dtype=jnp.int32), TOP_K)
    flat_w = gate.reshape(-1)
    order = jnp.argsort(flat_e)
    se, st, sw = flat_e[order], flat_t[order], flat_w[order]
    counts = jnp.bincount(flat_e, length=N_EXPERTS)
    starts = jnp.cumsum(counts) - counts
    padded = (counts + MOE_ROWS - 1) // MOE_ROWS * MOE_ROWS
    pend = jnp.cumsum(padded)
    pstart = pend - padded
    dest = pstart[se] + jnp.arange(n_assign) - starts[se]
    n_slots = (n_assign + MOE_ROWS - 1) // MOE_ROWS * MOE_ROWS + N_EXPERTS * MOE_ROWS
    n_blocks = n_slots // MOE_ROWS
    slot_tok = jnp.zeros((n_slots,), jnp.int32).at[dest].set(st)
    slot_w = jnp.zeros((n_slots,), F32).at[dest].set(sw)
    blk_exp = jnp.minimum(jnp.searchsorted(pend, jnp.arange(n_blocks) * MOE_ROWS, side='right'),
                          N_EXPERTS - 1)

    def expert_block(args):
        tok, wt, e = args
        xb = xf[tok]
        gu = xb @ w_gu[e] + b_gu[e]
        glu, lin = gu[:, :D_EXPERT], gu[:, D_EXPERT:]
        glu = jnp.minimum(glu, SWIGLU_LIMIT)
        lin = jnp.clip(lin, -SWIGLU_LIMIT, SWIGLU_LIMIT)
        act = glu * jax.nn.sigmoid(SWIGLU_ALPHA * glu) * (lin + 1)
        return (act @ w_dn[e] + b_dn[e]) * wt[:, None].astype(xf.dtype)

    ys = lax.map(expert_block, (slot_tok.reshape(n_blocks, MOE_ROWS),
                                slot_w.reshape(n_blocks, MOE_ROWS), blk_exp))
    out = jax.ops.segment_sum(ys.reshape(n_slots, D), slot_tok, num_segments=T)
    return out.reshape(B, S, D)


def _uinit(key, shape, std):
    a = std * math.sqrt(3.0)
    return jax.random.uniform(key, shape, F32, -a, a)


def setup_inputs(seed: int = 0) -> dict:
    key = jax.random.key(seed)
    ks = jax.random.split(key, 32)
    hd = NSA_HEAD_DIM
    nrm = jax.random.normal
    return {
        'x': nrm(ks[0], (BATCH, SEQ, D_MODEL), F32),
        'c': nrm(ks[1], (BATCH, D_MODEL), F32),
        'ada_w': _uinit(ks[2], (DEPTH, D_MODEL, 6 * D_MODEL), 0.5 * D_MODEL ** -0.5),
        'ada_b': 0.01 * nrm(ks[3], (DEPTH, 6 * D_MODEL), F32),
        'norm_mix_g': 1.0 + 0.01 * nrm(ks[4], (DEPTH, D_MODEL), F32),
        'w_in': _uinit(ks[5], (DEPTH, D_MODEL, IN_WIDTH), D_MODEL ** -0.5),
        'conv_dw_w': _uinit(ks[6], (DEPTH, CONV_WIDTH, CONV_CH), CONV_WIDTH ** -0.5),
        'conv_dw_b': 0.01 * nrm(ks[7], (DEPTH, CONV_CH), F32),
        'conv_ln_g': 1.0 + 0.01 * nrm(ks[8], (DEPTH, CONV_CH), F32),
        'conv_ln_b': 0.01 * nrm(ks[9], (DEPTH, CONV_CH), F32),
        'nsa_pe_k': 0.02 * nrm(ks[10], (DEPTH, CMP_BLOCK, hd), F32),
        'nsa_pe_v': 0.02 * nrm(ks[11], (DEPTH, CMP_BLOCK, hd), F32),
        'nsa_cmp_k_w1': _uinit(ks[12], (DEPTH, CMP_BLOCK * hd, hd), (CMP_BLOCK * hd) ** -0.5),
        'nsa_cmp_k_w2': _uinit(ks[13], (DEPTH, hd, hd), hd ** -0.5),
        'nsa_cmp_v_w1': _uinit(ks[14], (DEPTH, CMP_BLOCK * hd, hd), (CMP_BLOCK * hd) ** -0.5),
        'nsa_cmp_v_w2': _uinit(ks[15], (DEPTH, hd, hd), hd ** -0.5),
        'ret_gn_g': 1.0 + 0.01 * nrm(ks[16], (DEPTH, RET_WIDTH), F32),
        'ret_gn_b': 0.01 * nrm(ks[17], (DEPTH, RET_WIDTH), F32),
        'w_out': _uinit(ks[18], (DEPTH, MIX_WIDTH, D_MODEL), MIX_WIDTH ** -0.5),
        'norm_ffn_g': 1.0 + 0.01 * nrm(ks[19], (DEPTH, D_MODEL), F32),
        'router_w': _uinit(ks[20], (DEPTH, D_MODEL, N_EXPERTS), D_MODEL ** -0.5),
        'router_b': 0.01 * nrm(ks[21], (DEPTH, N_EXPERTS), F32),
        'moe_w_gate_up': _uinit(ks[22], (DEPTH, N_EXPERTS, D_MODEL, 2 * D_EXPERT), D_MODEL ** -0.5),
        'moe_b_gate_up': 0.01 * nrm(ks[23], (DEPTH, N_EXPERTS, 2 * D_EXPERT), F32),
        'moe_w_down': _uinit(ks[24], (DEPTH, N_EXPERTS, D_EXPERT, D_MODEL), D_EXPERT ** -0.5),
        'moe_b_down': 0.01 * nrm(ks[25], (DEPTH, N_EXPERTS, D_MODEL), F32),
        'final_norm_g': 1.0 + 0.01 * nrm(ks[26], (D_MODEL,), F32),
    }


def reference(x, c, ada_w, ada_b, norm_mix_g, w_in, conv_dw_w, conv_dw_b, conv_ln_g, conv_ln_b,
              nsa_pe_k, nsa_pe_v, nsa_cmp_k_w1, nsa_cmp_k_w2, nsa_cmp_v_w1, nsa_cmp_v_w2,
              ret_gn_g, ret_gn_b, w_out, norm_ffn_g, router_w, router_b,
              moe_w_gate_up, moe_b_gate_up, moe_w_down, moe_b_down, final_norm_g):
    b = c.shape[0]
    for l in range(DEPTH):
        mod = (jax.nn.silu(c) @ ada_w[l] + ada_b[l]).reshape(b, 6, D_MODEL)[:, :, None, :]
        shift_a, scale_a, gate_a, shift_f, scale_f, gate_f = [mod[:, i] for i in range(6)]
        h = rms_norm(x, norm_mix_g[l]) * (1 + scale_a) + shift_a
        y = hybrid_mixer(h, w_in[l], w_out[l], conv_dw_w[l], conv_dw_b[l], conv_ln_g[l], conv_ln_b[l],
                         nsa_pe_k[l], nsa_pe_v[l], nsa_cmp_k_w1[l], nsa_cmp_k_w2[l],
                         nsa_cmp_v_w1[l], nsa_cmp_v_w2[l], ret_gn_g[l], ret_gn_b[l])
        x = x + gate_a * y
        h = rms_norm(x, norm_ffn_g[l]) * (1 + scale_f) + shift_f
        x = x + gate_f * moe_ffn(h, router_w[l], router_b[l], moe_w_gate_up[l], moe_b_gate_up[l],
                                 moe_w_down[l], moe_b_down[l])
    return rms_norm(x, final_norm_g)
```

```python
from contextlib import ExitStack

import numpy as np
import ml_dtypes
import concourse.bass as bass
import concourse.mybir as mybir
from concourse.bass_utils import run_bass_kernel_spmd

F32 = mybir.dt.float32
BF16 = mybir.dt.bfloat16
AF = mybir.ActivationFunctionType
ALU = mybir.AluOpType
AX = mybir.AxisListType

NCORES = 8
D = 2048
S = 8192
DEPTH = 2
INW = 5656
EPS = 1e-6

ENGS = ("pe", "act", "dve", "pool", "sp")
N_DMA_SEMS = 8


class Prog:
    def __init__(self, nc):
        self.nc = nc
        self.streams = {e: [] for e in ENGS}
        self.count = {e: 0 for e in ENGS}
        self.waited = {e: {} for e in ENGS}
        self.last_w = {}
        self.readers = {}
        self.dma_n = [0] * N_DMA_SEMS
        self.dma_rr = 0
        self.out_tokens = []

    def _deps(self, reads, writes):
        deps = []
        for b in reads:
            t = self.last_w.get(b)
            if t is not None:
                deps.append(t)
        for b in writes:
            t = self.last_w.get(b)
            if t is not None:
                deps.append(t)
            deps.extend(self.readers.get(b, {}).values())
        return deps

    def _wait(self, eng, tok):
        key, val = tok
        if key == eng == "pe":
            return
        w = self.waited[eng]
        if w.get(key, 0) >= val:
            return
        w[key] = val
        self.streams[eng].append(("wait", key, val))

    def _commit(self, tok, reads, writes):
        for b in writes:
            self.last_w[b] = tok
            self.readers[b] = {}
        for b in reads:
            if b in writes:
                continue
            self.readers.setdefault(b, {})[tok[0]] = tok

    def op(self, eng, fn, reads=(), writes=()):
        for t in self._deps(reads, writes):
            self._wait(eng, t)
        self.count[eng] += 1
        tok = (eng, self.count[eng])
        self.streams[eng].append(("op", fn))
        self._commit(tok, reads, writes)
        return tok

    def dma(self, out, in_, reads=(), writes=(), q="sp", is_output=False, **kw):
        for t in self._deps(reads, writes):
            self._wait(q, t)
        s = self.dma_rr
        self.dma_rr = (self.dma_rr + 1) % N_DMA_SEMS
        key = ("dma", s)
        if self.dma_n[s] > 0:
            self._wait(q, (key, 16 * self.dma_n[s]))
        self.dma_n[s] += 1
        tok = (key, 16 * self.dma_n[s])
        self.streams[q].append(("dma", out, in_, s, kw))
        self._commit(tok, reads, writes)
        if is_output:
            self.out_tokens.append(tok)
        return tok

    def barrier(self):
        toks = [(e, self.count[e]) for e in ENGS if self.count[e] > 0]
        toks += [(("dma", k), 16 * self.dma_n[k]) for k in range(N_DMA_SEMS) if self.dma_n[k] > 0]
        for e in ENGS:
            for t in toks:
                self._wait(e, t)

    def emit(self):
        nc = self.nc
        with ExitStack() as st:
            esem = {e: st.enter_context(nc.semaphore("s_" + e)) for e in ENGS}
            dsem = [st.enter_context(nc.semaphore("d%d" % i)) for i in range(N_DMA_SEMS)]
            for t in self.out_tokens:
                self._wait("sp", t)
            block = st.enter_context(nc.Block())

            def semof(key):
                return dsem[key[1]] if isinstance(key, tuple) else esem[key]

            def replay(ename):
                def f(e):
                    for it in self.streams[ename]:
                        if it[0] == "wait":
                            e.wait_ge(semof(it[1]), it[2])
                        elif it[0] == "op":
                            it[1](e).then_inc(esem[ename], 1)
                        else:
                            _, out, in_, s, kw = it
                            e.dma_start(out=out, in_=in_, **kw).then_inc(dsem[s], 16)
                return f

            block.tensor(replay("pe"))
            block.scalar(replay("act"))
            block.vector(replay("dve"))
            block.gpsimd(replay("pool"))
            block.sync(replay("sp"))


class Ctx:
    def __init__(self):
        self.nc = bass.Bass("TRN2", target_bir_lowering=False)
        self.st = ExitStack()
        self.P = Prog(self.nc)

    def din(self, name, shape, dt=F32):
        return self.nc.dram_tensor(name, list(shape), dt, kind="ExternalInput").ap()

    def sbn(self, name, shape, dt=F32):
        return self.sb("s_" + name, shape, dt)

    def dout(self, name, shape, dt=F32):
        return self.nc.dram_tensor(name, list(shape), dt, kind="ExternalOutput").ap()

    def sb(self, name, shape, dt=F32):
        return self.st.enter_context(self.nc.sbuf_tensor("sb_" + name, list(shape), dt))

    def ps(self, name, shape, dt=F32):
        return self.st.enter_context(self.nc.psum_tensor("ps_" + name, list(shape), dt))

    def finish(self):
        self.P.emit()
        self.st.close()
        return self.nc


def run(nc, in_maps):
    res = run_bass_kernel_spmd(nc, in_maps, core_ids=list(range(NCORES)))
    return res.results


def build_k0():
    C = Ctx()
    P = C.P
    c_in = C.din("c", [128, 16])
    w = C.din("w", [DEPTH, D, 1536])
    b = C.din("b", [DEPTH, 1536])
    out = C.dout("out", [DEPTH, 1536])
    craw = C.sb("craw", [128, 16])
    sc = C.sb("sc", [128, 16])
    wt = [C.sb("wt%d" % i, [128, 16, 512]) for i in range(2)]
    bt = [C.sb("bt%d" % i, [1, 512]) for i in range(2)]
    ot = [C.sb("ot%d" % i, [1, 512]) for i in range(2)]
    pp = [C.ps("pp%d" % i, [1, 512]) for i in range(2)]
    P.dma(craw[:], c_in, writes=["craw"])
    P.op("act", lambda e: e.activation(out=sc[:], in_=craw[:], func=AF.Silu), reads=["craw"], writes=["sc"])
    it = 0
    for l in range(DEPTH):
        wl = w[l].rearrange("(kc p) n -> p kc n", p=128)
        for n in range(3):
            k = it % 2
            it += 1
            P.dma(wt[k][:], wl[:, :, n * 512:(n + 1) * 512], writes=[("wt", k)])
            P.dma(bt[k][:], b[l:l + 1, n * 512:(n + 1) * 512], writes=[("bt", k)])
            for kc in range(16):
                P.op("pe", lambda e, k=k, kc=kc: e.matmul(pp[k][:], lhsT=sc[:, kc:kc + 1], rhs=wt[k][:, kc, :],
                                                          start=(kc == 0), stop=(kc == 15)),
                     reads=["sc", ("wt", k)], writes=[("pp", k)])
            P.op("dve", lambda e, k=k: e.tensor_tensor(out=ot[k][:], in0=pp[k][:], in1=bt[k][:], op=ALU.add),
                 reads=[("pp", k), ("bt", k)], writes=[("ot", k)])
            P.dma(out[l:l + 1, n * 512:(n + 1) * 512], ot[k][:], reads=[("ot", k)], is_output=True)
    return C.finish()


def run_k0(c, ada_w, ada_b):
    nc = build_k0()
    cin = np.ascontiguousarray(c.reshape(16, 128).T)
    in_maps = []
    for i in range(NCORES):
        sl = slice(1536 * i, 1536 * (i + 1))
        in_maps.append({"c": cin, "w": np.ascontiguousarray(ada_w[:, :, sl]),
                        "b": np.ascontiguousarray(ada_b[:, sl])})
    res = run(nc, in_maps)
    return np.concatenate([r["out"] for r in res], axis=1)


ORIG_SPLITS = (512, 512, 1024, 256, 256, 256, 256, 256, 256, 24, 512, 512, 512, 512)
ORIG_OFF = np.concatenate([[0], np.cumsum(ORIG_SPLITS)])
NEW_ORDER = (0, 1, 2, 3, 4, 5, 6, 7, 8, 10, 11, 12, 13, 9)
COL_PERM = np.concatenate([np.arange(ORIG_OFF[j], ORIG_OFF[j + 1]) for j in NEW_ORDER])
O_CV, O_CG, O_NQ, O_KC, O_VC, O_KS, O_VS, O_KW, O_VW, O_RQ, O_RK, O_RV, O_RG, O_GT = (
    0, 512, 1024, 2048, 2304, 2560, 2816, 3072, 3328, 3584, 4096, 4608, 5120, 5632)
CH_KIND = ["plain", "plain", "nsa4", "nsa4", "nsa2", "nsa2", "nsa2", "retq", "retk", "plain", "plain", "gate"]


def rot_tables():
    pos = np.arange(S, dtype=np.float32)
    invn = np.power(np.float32(500000.0), -np.arange(16, dtype=np.float32) * np.float32(2.0) / np.float32(32))
    angn = pos[:, None] * invn[None, :].astype(np.float32)
    invr = np.power(np.float32(10000.0), -np.arange(64, dtype=np.float32) * np.float32(2.0) / np.float32(128))
    angr = pos[:, None] * invr[None, :].astype(np.float32)
    return (np.cos(angn).astype(np.float32), np.sin(angn).astype(np.float32),
            np.cos(angr).astype(np.float32), np.sin(angr).astype(np.float32))


def ret_consts():
    lg = np.log(1.0 - 2.0 ** (-5.0 - np.arange(4, dtype=np.float64)))
    return lg


def ret_ztab():
    lg = ret_consts()
    i = (np.arange(S) % 128).astype(np.float64)
    zq = np.exp(lg[None, :] * (i[:, None] - 127.0))
    zk = np.exp(lg[None, :] * (127.0 - i[:, None])) * (128.0 ** -0.5)
    return np.concatenate([zq, zk], axis=1).astype(np.float32)


def emit_norm_T(C, x_src, A, Sh, hT, ident, ntiles, pfx=""):
    P = C.P
    xt = [C.sb(pfx + "nx%d" % i, [128, D]) for i in range(2)]
    sq = C.sb(pfx + "nsq", [128, D])
    hb = [C.sb(pfx + "nhb%d" % i, [128, D], BF16) for i in range(2)]
    st = [C.sb(pfx + "nst%d" % i, [128, 2]) for i in range(2)]
    pT = C.ps(pfx + "npT", [128, 16, 128], BF16)
    for t in range(ntiles):
        k = t % 2
        P.dma(xt[k][:], x_src(t), writes=[(pfx + "nx", k)])
        P.op("act", lambda e, k=k: e.activation(out=sq[:], in_=xt[k][:], func=AF.Square),
             reads=[(pfx + "nx", k)], writes=[pfx + "nsq"])
        P.op("dve", lambda e, k=k: e.reduce_sum(out=st[k][:, 0:1], in_=sq[:], axis=AX.X),
             reads=[pfx + "nsq"], writes=[(pfx + "nst", k)])
        P.op("act", lambda e, k=k: e.activation(out=st[k][:, 1:2], in_=st[k][:, 0:1], func=AF.Sqrt,
                                                scale=1.0 / D, bias=EPS),
             reads=[(pfx + "nst", k)], writes=[(pfx + "nst", k)])
        P.op("dve", lambda e, k=k: e.reciprocal(out=st[k][:, 0:1], in_=st[k][:, 1:2]),
             reads=[(pfx + "nst", k)], writes=[(pfx + "nst", k)])
        P.op("dve", lambda e, k=k: e.scalar_tensor_tensor(out=sq[:], in0=xt[k][:], scalar=st[k][:, 0:1], in1=A[:],
                                                          op0=ALU.mult, op1=ALU.mult),
             reads=[(pfx + "nx", k), (pfx + "nst", k), "A"], writes=[pfx + "nsq"])
        P.op("dve", lambda e, k=k: e.tensor_tensor(out=hb[k][:], in0=sq[:], in1=Sh[:], op=ALU.add),
             reads=[pfx + "nsq", "Sh"], writes=[(pfx + "nhb", k)])
        for kc in range(16):
            P.op("pe", lambda e, k=k, kc=kc: e.transpose(out=pT[:, kc, :], in_=hb[k][:, kc * 128:(kc + 1) * 128],
                                                         identity=ident[:]),
                 reads=[(pfx + "nhb", k), "ident"], writes=[pfx + "npT"])
        P.op("act", lambda e, t=t: e.copy(out=hT[t][:], in_=pT[:]), reads=[pfx + "npT"], writes=[("hT", t)])


def build_k1(NT=8):
    C = Ctx()
    P = C.P
    x = C.din("x", [NT * 128, D])
    w = C.din("w", [D, INW])
    modA = C.din("modA", [128, D])
    modS = C.din("modS", [128, D])
    gN = C.din("gN", [128, D])
    identd = C.din("ident", [128, 128], BF16)
    tabn = C.din("tabn", [2, NT * 128, 16])
    tabr = C.din("tabr", [2, NT * 128, 64])
    ztd = C.din("zt", [NT * 128, 8])
    o32 = C.dout("o32", [NT * 128, INW])
    o16 = C.dout("o16", [NT * 128, INW], BF16)

    ident = C.sb("ident", [128, 128], BF16)
    A = C.sb("A", [128, D])
    Sh = C.sb("Sh", [128, D])
    tn = C.sb("tn", [128, 2, NT, 16])
    tr = C.sb("tr", [128, 2, NT, 64])
    zt = C.sb("zt", [128, NT, 8])
    hT = [C.sb("hT%d" % t, [128, 16, 128], BF16) for t in range(NT)]
    P.dma(ident[:], identd, writes=["ident"])
    for ci in range(2):
        P.dma(tn[:, ci], tabn[ci].rearrange("(t p) f -> p t f", p=128), writes=["tn"])
    P.dma(zt[:], ztd.rearrange("(t p) f -> p t f", p=128), writes=["zt"])
    for ci in range(2):
        P.dma(tr[:, ci], tabr[ci].rearrange("(t p) f -> p t f", p=128), writes=["tr"])
    gtmp = C.sb("gtmp", [128, D])
    P.dma(A[:], modA, writes=["A"])
    P.dma(gtmp[:], gN, writes=["gtmp"])
    P.dma(Sh[:], modS, writes=["Sh"])
    P.op("dve", lambda e: e.scalar_tensor_tensor(out=A[:], in0=A[:], scalar=1.0, in1=gtmp[:], op0=ALU.add, op1=ALU.mult),
         reads=["gtmp"], writes=["A"])
    emit_norm_T(C, lambda t: x[t * 128:(t + 1) * 128, :], A, Sh, hT, ident, NT)

    wst = [C.sb("wst%d" % i, [128, 8, 512]) for i in range(2)]
    wbf = [C.sb("wbf%d" % i, [128, 16, 512], BF16) for i in range(2)]
    ob32 = [C.sb("ob32_%d" % i, [128, 512]) for i in range(2)]
    ob16 = [C.sb("ob16_%d" % i, [128, 512], BF16) for i in range(2)]
    tmp = [C.sb("rt%d" % i, [128, 4, 64]) for i in range(4)]
    pY = [C.ps("pY%d" % i, [128, 512]) for i in range(2)]
    wv = w.rearrange("(kc p) n -> p kc n", p=128)
    nst = 0
    nev = 0
    for cc in range(12):
        kind = CH_KIND[cc]
        ncol = 512 if kind != "gate" else 24
        c0 = cc * 512
        wb = cc % 2
        for half in range(2):
            sbuf = nst % 2
            nst += 1
            P.dma(wst[sbuf][:, :, 0:ncol], wv[:, half * 8:(half + 1) * 8, c0:c0 + ncol], writes=[("wst", sbuf)])
            P.op("pool", lambda e, sbuf=sbuf, wb=wb, half=half, ncol=ncol: e.tensor_copy(
                out=wbf[wb][:, half * 8:(half + 1) * 8, 0:ncol], in_=wst[sbuf][:, :, 0:ncol]),
                reads=[("wst", sbuf)], writes=[("wbf", wb, half)])
        for t in range(NT):
            pb = nev % 2
            nev += 1
            for kc in range(16):
                P.op("pe", lambda e, pb=pb, t=t, kc=kc, wb=wb, ncol=ncol: e.matmul(
                    pY[pb][:, 0:ncol], lhsT=hT[t][:, kc, :], rhs=wbf[wb][:, kc, 0:ncol], start=(kc == 0), stop=(kc == 15)),
                    reads=[("hT", t), ("wbf", wb, kc // 8)], writes=[("pY", pb)])
            ps = pY[pb]
            o3 = ob32[pb]
            o1 = ob16[pb]
            R = []
            PSX = [("pY", pb)]
            W32 = [("ob32", pb)]
            if kind in ("plain", "gate"):
                P.op("act", lambda e, ps=ps, o3=o3, ncol=ncol: e.copy(out=o3[:, 0:ncol], in_=ps[:, 0:ncol]), reads=R, writes=W32 + PSX)
            elif kind in ("nsa4", "nsa2"):
                nh = 4 if kind == "nsa4" else 2
                P.op("act", lambda e, ps=ps, o3=o3: e.copy(out=o3[:], in_=ps[:]), reads=R, writes=W32 + PSX)
                psv = ps[:].rearrange("p (h d) -> p h d", d=128)
                o3v = o3[:].rearrange("p (h d) -> p h d", d=128)
                cs = tn[:, 0, t:t + 1, :].to_broadcast([128, nh, 16])
                sn = tn[:, 1, t:t + 1, :].to_broadcast([128, nh, 16])
                x1 = psv[:, 0:nh, 0:16]
                x2 = psv[:, 0:nh, 16:32]
                tv = [tm[:, 0:nh, 0:16] for tm in tmp]
                for (dst, a, tb) in ((tv[0], x1, cs), (tv[1], x2, sn), (tv[2], x2, cs), (tv[3], x1, sn)):
                    P.op("dve", lambda e, dst=dst, a=a, tb=tb: e.tensor_tensor(out=dst, in0=a, in1=tb, op=ALU.mult),
                         reads=R + ["tn"], writes=["rtmp"] + PSX)
                P.op("dve", lambda e, o3v=o3v, tv=tv, nh=nh: e.tensor_tensor(out=o3v[:, 0:nh, 0:16], in0=tv[0], in1=tv[1], op=ALU.subtract),
                     reads=["rtmp"], writes=W32)
                P.op("dve", lambda e, o3v=o3v, tv=tv, nh=nh: e.tensor_tensor(out=o3v[:, 0:nh, 16:32], in0=tv[2], in1=tv[3], op=ALU.add),
                     reads=["rtmp"], writes=W32)
            else:
                ci = 0
                zo = 0 if kind == "retq" else 4
                psv = ps[:].rearrange("p (h d) -> p h d", d=128)
                o3v = o3[:].rearrange("p (h d) -> p h d", d=128)
                cs = tr[:, ci, t:t + 1, :].to_broadcast([128, 4, 64])
                sn = tr[:, ci + 1, t:t + 1, :].to_broadcast([128, 4, 64])
                x1 = psv[:, :, 0:64]
                x2 = psv[:, :, 64:128]
                tv = [tm[:] for tm in tmp]
                for (dst, a, tb) in ((tv[0], x1, cs), (tv[1], x2, sn), (tv[2], x2, cs), (tv[3], x1, sn)):
                    P.op("dve", lambda e, dst=dst, a=a, tb=tb: e.tensor_tensor(out=dst, in0=a, in1=tb, op=ALU.mult),
                         reads=R + ["tr"], writes=["rtmp"] + PSX)
                P.op("dve", lambda e, o3v=o3v, tv=tv: e.tensor_tensor(out=o3v[:, :, 0:64], in0=tv[0], in1=tv[1], op=ALU.subtract),
                     reads=["rtmp"], writes=W32)
                P.op("dve", lambda e, o3v=o3v, tv=tv: e.tensor_tensor(out=o3v[:, :, 64:128], in0=tv[2], in1=tv[3], op=ALU.add),
                     reads=["rtmp"], writes=W32)
                zb = zt[:, t, zo:zo + 4].unsqueeze(2).to_broadcast([128, 4, 128])
                P.op("dve", lambda e, o3v=o3v, zb=zb: e.tensor_tensor(out=o3v, in0=o3v, in1=zb, op=ALU.mult),
                     reads=["zt"], writes=W32)
            P.op("dve", lambda e, o3=o3, o1=o1, ncol=ncol: e.tensor_copy(out=o1[:, 0:ncol], in_=o3[:, 0:ncol]),
                 reads=W32, writes=[("ob16", pb)])
            P.dma(o32[t * 128:(t + 1) * 128, c0:c0 + ncol], o3[:, 0:ncol], reads=W32, is_output=True)
            P.dma(o16[t * 128:(t + 1) * 128, c0:c0 + ncol], o1[:, 0:ncol], reads=[("ob16", pb)], is_output=True)
    return C.finish()


def bc(v):
    return np.ascontiguousarray(np.broadcast_to(np.asarray(v, np.float32)[None, :], (128, v.shape[0])))


def run_k1(x2d, w_in_l, mod_l, g_l, tabs):
    nc = build_k1()
    cn, sn, cr, sr = tabs
    sc = np.float32(128 ** -0.5)
    wre = np.ascontiguousarray(w_in_l[:, COL_PERM])
    ident = np.eye(128, dtype=np.float32).astype(ml_dtypes.bfloat16)
    in_maps = []
    for i in range(NCORES):
        sl = slice(1024 * i, 1024 * (i + 1))
        in_maps.append({
            "x": np.ascontiguousarray(x2d[sl]), "w": wre,
            "modA": bc(mod_l[D:2 * D]), "modS": bc(mod_l[0:D]), "gN": bc(g_l), "ident": ident,
            "tabn": np.ascontiguousarray(np.stack([cn[sl], sn[sl]])),
            "tabr": np.ascontiguousarray(np.stack([cr[sl], sr[sl]])), "zt": np.ascontiguousarray(ret_ztab()[sl]),
        })
    res = run(nc, in_maps)
    return (np.concatenate([r["o32"] for r in res], axis=0), np.concatenate([r["o16"] for r in res], axis=0))


SCALE = 128 ** -0.5


def build_k2a(NS=8):
    SU = NS * 1024
    NKT = SU // 128
    NCMP = (SU - 32) // 16 + 1
    NCH = (NCMP + 127) // 128
    C = Ctx()
    P = C.P
    identd = C.din("ident", [128, 128], BF16)
    ident32d = C.din("ident32", [128, 128])
    cvh = C.din("cvh", [128, 4, NS, 158])
    cgh = C.din("cgh", [128, 4, NS, 158])
    dwd = C.din("dw", [128, 4, 32])
    lngd = C.din("lng", [128, 512])
    lnbd = C.din("lnb", [128, 512])
    kzd = C.din("kz", [SU, 512], BF16)
    rvd = C.din("rv", [SU, 512], BF16)
    qpTd = C.din("qpT", [128, NS, 4, 128], BF16)
    kzTd = C.din("kzT", [128, NS, 4, 128], BF16)
    rgd = C.din("rg", [NS * 128, 512])
    rvod = C.din("rvo", [NS * 128, 512], BF16)
    gngd = C.din("gng", [128, 512])
    gnbd = C.din("gnb", [128, 512])
    decd = C.din("dec", [128, 512])
    trid = C.din("tri", [128, 128], BF16)
    indd = C.din("ind", [128, 8])
    kcmpTd = C.din("kcmpT", [2, 2, 128, SU], BF16)
    w1d = C.din("w1", [2, 4096, 128])
    w2d = C.din("w2", [2, 128, 128])
    peTd = C.din("peT", [2, 128, 32])
    qTd = C.din("qT", [128, NS, 8, 128], BF16)
    ksTd = C.din("ksT", [2, 128, SU], BF16)
    vsd = C.din("vs", [SU, 256], BF16)
    kwTd = C.din("kwT", [128, NS, 2, 640], BF16)
    vwd = C.din("vw", [128, NS, 2, 5, 128], BF16)
    gtd = C.din("gt", [NS * 128, 24])
    coverd = C.din("cover", [128, NCH, 128], BF16)
    nt16d = C.din("nt16", [128, NCH, 128])
    b64d = C.din("b64", [128, 128])
    fmd = C.din("fm", [128, 128])
    f0d = C.din("f0", [128, 128])
    q0d = C.din("q0c", [128, 2, NS])
    cmaskd = C.din("cmask", [128, 8, 128], BF16)
    wmaskd = C.din("wmask", [128, NS, 5, 128], BF16)
    ycat = C.dout("ycat", [NS * 128, 2048])

    ident = C.sb("ident", [128, 128], BF16)
    ident32 = C.sb("ident32", [128, 128])
    ones = C.sb("ones", [128, 1], BF16)
    P.dma(ident[:], identd, writes=["ident"])
    P.dma(ident32[:], ident32d, writes=["ident32"])
    P.op("pool", lambda e: e.memset(ones[:], 1.0), writes=["ones"])

    NB = 8
    bank = [C.ps("bk%d" % i, [128, 512]) for i in range(NB)]

    def bk(i):
        return ("bk", i)

    kcT = C.sb("kcT", [128, 2, NCH * 128], BF16)
    vc = C.sb("vc", [128, 2, NCH, 128], BF16)
    P.op("pool", lambda e: e.memset(kcT[:], 0.0), writes=["kcT"])
    P.op("pool", lambda e: e.memset(vc[:], 0.0), writes=["vc"])
    with ExitStack() as ph1:
        def sb1(name, shape, dt=F32):
            return ph1.enter_context(C.nc.sbuf_tensor("sb_" + name, list(shape), dt))
        w1s = sb1("w1s", [128, 32, 128])
        w1b = sb1("w1b", [128, 32, 128], BF16)
        w2s = sb1("w2s", [128, 128])
        w2b = sb1("w2b", [128, 128], BF16)
        pes = sb1("pes", [128, 32])
        peb = sb1("peb", [128, 32], BF16)
        bia = sb1("bia", [128, 1])
        xT = sb1("xT", [128, SU], BF16)
        a1 = sb1("a1", [128, NCH * 128], BF16)
        P.op("pool", lambda e: e.memset(a1[:], 0.0), writes=["a1"])
        for kv in range(2):
            P.dma(w1s[:], w1d[kv].rearrange("(l d) o -> d l o", d=128), writes=["w1s"])
            P.dma(w2s[:], w2d[kv], writes=["w2s"])
            P.dma(pes[:], peTd[kv], writes=["pes"])
            P.op("pool", lambda e: e.tensor_copy(out=w1b[:], in_=w1s[:]), reads=["w1s"], writes=["w1b"])
            P.op("pool", lambda e: e.tensor_copy(out=w2b[:], in_=w2s[:]), reads=["w2s"], writes=["w2b"])
            P.op("pool", lambda e: e.tensor_copy(out=peb[:], in_=pes[:]), reads=["pes"], writes=["peb"])
            for l in range(32):
                P.op("pe", lambda e, l=l: e.matmul(bank[0][:, 0:1], lhsT=w1b[:, l, :], rhs=peb[:, l:l + 1],
                                                   start=(l == 0), stop=(l == 31)),
                     reads=["w1b", "peb"], writes=[bk(0)])
            P.op("act", lambda e: e.copy(out=bia[:], in_=bank[0][:, 0:1]), writes=["bia", bk(0)])
            for hd in range(2):
                P.dma(xT[:], kcmpTd[kv, hd], writes=["xT"])
                for l in range(32):
                    P.op("pe", lambda e, l=l: e.matmul(bank[1][:, 0:NCMP], lhsT=w1b[:, l, :],
                                                       rhs=xT[:, l:l + 16 * (NCMP - 1) + 1:16],
                                                       start=(l == 0), stop=(l == 31)),
                         reads=["w1b", "xT"], writes=[bk(1)])
                P.op("act", lambda e: e.activation(out=a1[:, 0:NCMP], in_=bank[1][:, 0:NCMP], func=AF.Silu, bias=bia[:, 0:1]),
                     reads=["bia"], writes=["a1", bk(1)])
                if kv == 0:
                    P.op("pe", lambda e: e.matmul(bank[2][:, 0:NCMP], lhsT=w2b[:], rhs=a1[:, 0:NCMP], start=True, stop=True),
                         reads=["w2b", "a1"], writes=[bk(2)])
                    P.op("act", lambda e, hd=hd: e.copy(out=kcT[:, hd, 0:NCMP], in_=bank[2][:, 0:NCMP]), writes=["kcT", bk(2)])
                else:
                    for ch in range(NCH):
                        P.op("pe", lambda e, ch=ch: e.matmul(bank[2][:, ch * 128:(ch + 1) * 128], lhsT=a1[:, ch * 128:(ch + 1) * 128],
                                                             rhs=w2b[:], start=True, stop=True),
                             reads=["w2b", "a1"], writes=[bk(2)])
                    P.op("act", lambda e, hd=hd: e.copy(out=vc[:, hd, :, :], in_=bank[2][:, 0:NCH * 128].rearrange("p (c d) -> p c d", d=128)),
                         writes=["vc", bk(2)])
        P.barrier()
    Tst = C.sb("Tst", [128, 512])
    Tacc = C.sb("Tacc", [128, NS, 512])
    Tb = C.sb("Tb", [128, NS, 512], BF16)
    dec = C.sb("dec", [128, 512])
    ind = C.sb("ind", [128, 8])
    kzt = [C.sb("kzt%d" % i, [128, 512], BF16) for i in range(2)]
    rvt = [C.sb("rvt%d" % i, [128, 512], BF16) for i in range(2)]
    P.dma(dec[:], decd, writes=["dec"])
    P.dma(ind[:], indd, writes=["ind"])
    P.op("pool", lambda e: e.memset(Tst[:], 0.0), writes=["Tst"])
    P.op("pool", lambda e: e.memset(Tacc[:], 0.0), writes=["Tacc"])
    for m in range(NKT):
        k = m % 2
        j = m // 8
        P.op("dve", lambda e, j=j, m=m: e.scalar_tensor_tensor(out=Tacc[:, j, :], in0=Tst[:], scalar=ind[:, m % 8:m % 8 + 1],
                                                              in1=Tacc[:, j, :], op0=ALU.mult, op1=ALU.add),
             reads=["Tst", "ind"], writes=["Tacc"])
        if m == NKT - 1:
            break
        P.dma(kzt[k][:], kzd[m * 128:(m + 1) * 128, :], writes=[("kzt", k)])
        P.dma(rvt[k][:], rvd[m * 128:(m + 1) * 128, :], writes=[("rvt", k)])
        for h in range(4):
            P.op("pe", lambda e, k=k, h=h: e.matmul(bank[3][:, h * 128:(h + 1) * 128], lhsT=kzt[k][:, h * 128:(h + 1) * 128],
                                                    rhs=rvt[k][:, h * 128:(h + 1) * 128], start=True, stop=True),
                 reads=[("kzt", k), ("rvt", k)], writes=[bk(3)])
        P.op("dve", lambda e: e.tensor_tensor(out=Tst[:], in0=Tst[:], in1=bank[3][:], op=ALU.add), writes=["Tst", bk(3)])
        P.op("dve", lambda e: e.tensor_tensor(out=Tst[:], in0=Tst[:], in1=dec[:], op=ALU.mult), reads=["dec"], writes=["Tst"])
    P.op("act", lambda e: e.copy(out=Tb[:], in_=Tacc[:]), reads=["Tacc"], writes=["Tb"])

    ksT = C.sb("ksT", [128, 2, SU], BF16)
    vs = C.sb("vs", [128, NKT, 256], BF16)
    for g in range(2):
        P.dma(ksT[:, g, :], ksTd[g], writes=["ksT"])
    for c4 in range(0, NKT, 16):
        n4 = min(16, NKT - c4)
        P.dma(vs[:, c4:c4 + n4, :], vsd[c4 * 128:(c4 + n4) * 128, :].rearrange("(t p) f -> p t f", p=128), writes=["vs"])
    cover = C.sb("cover", [128, NCH, 128], BF16)
    nt16 = C.sb("nt16", [128, NCH, 128])
    b64 = C.sb("b64", [128, 128])
    fm = C.sb("fm", [128, 128])
    f0 = C.sb("f0", [128, 128])
    q0c = C.sb("q0c", [128, 2, NS])
    cmask = C.sb("cmask", [128, 8, 128], BF16)
    wmask = C.sb("wmask", [128, NS, 5, 128], BF16)
    tri = C.sb("tri", [128, 128], BF16)
    dw = C.sb("dw", [128, 4, 32])
    lng = C.sb("lng", [128, 512])
    lnb = C.sb("lnb", [128, 512])
    gng = C.sb("gng", [128, 512])
    gnb = C.sb("gnb", [128, 512])
    for (t_, d_, nm) in ((cover, coverd, "cover"), (nt16, nt16d, "nt16"), (b64, b64d, "b64"), (fm, fmd, "fm"), (f0, f0d, "f0"),
                         (q0c, q0d, "q0c"), (cmask, cmaskd, "cmask"), (wmask, wmaskd, "wmask"), (tri, trid, "tri"), (dw, dwd, "dw"),
                         (lng, lngd, "lng"), (lnb, lnbd, "lnb"), (gng, gngd, "gng"), (gnb, gnbd, "gnb")):
        P.dma(t_[:], d_, writes=[nm])

    yt = C.sb("yt", [128, 2048])
    cv = C.sb("cv", [128, 4, 158])
    cg = C.sb("cg", [128, 4, 158])
    cacc = C.sb("cacc", [128, 4, 128])
    w512 = [C.sb("w512_%d" % i, [128, 512]) for i in range(3)]
    st8 = C.sb("st8", [128, 16])
    qpT = C.sb("qpT", [128, 4, 128], BF16)
    kzT = C.sb("kzT", [128, 4, 128], BF16)
    rg = C.sb("rg", [128, 512])
    rvo = C.sb("rvo", [128, 512], BF16)
    innT = C.sb("innT", [128, 4, 128], BF16)
    qT = C.sb("qT", [128, 8, 128], BF16)
    kwT = C.sb("kwT", [128, 2, 640], BF16)
    vw = C.sb("vw", [128, 2, 5, 128], BF16)
    gt = C.sb("gt", [128, 24])
    gsg = C.sb("gsg", [128, 24])
    eT = [C.sb("eT%d" % i, [128, 512]) for i in range(2)]
    eTm = [C.sb("eTm%d" % i, [128, 4, 128], BF16) for i in range(2)]
    mk = C.sb("mk", [128, NCH, 128], BF16)
    m2 = C.sb("m2", [128, 128], BF16)
    Et = [C.sb("Et%d" % i, [128, 128], BF16) for i in range(2)]
    imp = C.sb("imp", [128, 128])
    impw = C.sb("impw", [128, 128])
    vld = C.sb("vld", [128, 128])
    sel = C.sb("sel", [128, 128], BF16)
    selT = C.sb("selT", [128, 128], BF16)
    mx8 = C.sb("mx8", [128, 16])
    lrec = C.sb("lrec", [128, 8])
    ynsa = C.sb("ynsa", [128, 4, 128])
    B_S, B_O, B_L, B_M, B_X = 4, 5, 6, 7, 3
    sbanks = [4, 0]
    nev = [0]

    def attend(spsum_bank, mask_fn, vfn, first, last, extra_reads=()):
        b = nev[0] % 2
        nev[0] += 1
        P.op("act", lambda e, b=b: e.activation(out=eT[b][:], in_=bank[spsum_bank][:], func=AF.Exp, scale=SCALE),
             writes=[("eT", b), bk(spsum_bank)])
        mask_fn(b)
        for h in range(4):
            P.op("pe", lambda e, b=b, h=h: e.matmul(bank[B_O][:, h * 128:(h + 1) * 128], lhsT=eTm[b][:, h, :], rhs=vfn(),
                                                    start=(first and h == 0), stop=last, skip_group_check=True),
                 reads=[("eTm", b)] + list(extra_reads), writes=[bk(B_O)])
        for h in range(4):
            P.op("pe", lambda e, b=b, h=h: e.matmul(bank[B_L][:, h:h + 1], lhsT=eTm[b][:, h, :], rhs=ones[:],
                                                    start=(first and h == 0), stop=last, skip_group_check=True),
                 reads=[("eTm", b), "ones"], writes=[bk(B_L)])

    def finish_branch(g, br, first_branch):
        P.op("dve", lambda e: e.tensor_scalar(out=lrec[:, 0:4], in0=bank[B_L][:, 0:4], scalar1=1e-30, scalar2=None, op0=ALU.max),
             writes=["lrec", bk(B_L)])
        P.op("dve", lambda e: e.reciprocal(out=lrec[:, 4:8], in_=lrec[:, 0:4]), writes=["lrec"])
        P.op("dve", lambda e: e.tensor_tensor(out=lrec[:, 0:4], in0=lrec[:, 4:8], in1=gsg[:, br * 8 + 4 * g:br * 8 + 4 * g + 4], op=ALU.mult),
             reads=["gsg"], writes=["lrec"])
        wb = lrec[:, 0:4].unsqueeze(2).to_broadcast([128, 4, 128])
        ov = bank[B_O][:].rearrange("p (h d) -> p h d", d=128)
        if first_branch:
            P.op("dve", lambda e: e.tensor_tensor(out=ynsa[:], in0=ov, in1=wb, op=ALU.mult), reads=["lrec"], writes=["ynsa", bk(B_O)])
        else:
            tv = w512[0][:].rearrange("p (h d) -> p h d", d=128)
            P.op("dve", lambda e: e.tensor_tensor(out=tv, in0=ov, in1=wb, op=ALU.mult), reads=["lrec"], writes=[("w512", 0), bk(B_O)])
            P.op("dve", lambda e: e.tensor_tensor(out=ynsa[:], in0=ynsa[:], in1=tv, op=ALU.add), reads=[("w512", 0)], writes=["ynsa"])

    def layer_norm_free(src_tag, x3, nh, dd, gam, bet, out3, out_tag, gtag, btag):
        inv = 1.0 / dd
        P.op("dve", lambda e: e.reduce_sum(out=st8[:, 0:nh], in_=x3, axis=AX.X), reads=[src_tag], writes=["st8"])
        P.op("dve", lambda e: e.tensor_scalar(out=st8[:, 0:nh], in0=st8[:, 0:nh], scalar1=-inv, scalar2=None, op0=ALU.mult), writes=["st8"])
        mb = st8[:, 0:nh].unsqueeze(2).to_broadcast([128, nh, dd])
        P.op("dve", lambda e: e.tensor_tensor(out=x3, in0=x3, in1=mb, op=ALU.add), reads=["st8"], writes=[src_tag])
        sq3 = w512[1][:, 0:nh * dd].rearrange("p (h d) -> p h d", d=dd)
        P.op("act", lambda e: e.activation(out=sq3, in_=x3, func=AF.Square), reads=[src_tag], writes=[("w512", 1)])
        P.op("dve", lambda e: e.reduce_sum(out=st8[:, 4:4 + nh], in_=sq3, axis=AX.X), reads=[("w512", 1)], writes=["st8"])
        P.op("act", lambda e: e.activation(out=st8[:, 8:8 + nh], in_=st8[:, 4:4 + nh], func=AF.Sqrt, scale=inv, bias=EPS), writes=["st8"])
        P.op("dve", lambda e: e.reciprocal(out=st8[:, 12:12 + nh], in_=st8[:, 8:8 + nh]), writes=["st8"])
        rb = st8[:, 12:12 + nh].unsqueeze(2).to_broadcast([128, nh, dd])
        P.op("dve", lambda e: e.tensor_tensor(out=x3, in0=x3, in1=rb, op=ALU.mult), reads=["st8"], writes=[src_tag])
        g3 = gam[:, 0:nh * dd].rearrange("p (h d) -> p h d", d=dd)
        b3 = bet[:, 0:nh * dd].rearrange("p (h d) -> p h d", d=dd)
        P.op("dve", lambda e: e.tensor_tensor(out=x3, in0=x3, in1=g3, op=ALU.mult), reads=[gtag], writes=[src_tag])
        P.op("dve", lambda e: e.tensor_tensor(out=out3, in0=x3, in1=b3, op=ALU.add), reads=[src_tag, btag], writes=[out_tag])

    for j in range(NS):
        P.dma(cv[:], cvh[:, :, j, :], writes=["cv"])
        P.dma(cg[:], cgh[:, :, j, :], writes=["cg"])
        P.dma(qpT[:], qpTd[:, j], writes=["qpT"])
        P.dma(kzT[:], kzTd[:, j], writes=["kzT"])
        P.dma(rg[:], rgd[j * 128:(j + 1) * 128, :], writes=["rg"])
        P.dma(qT[:], qTd[:, j], writes=["qT"])
        P.dma(kwT[:], kwTd[:, j], writes=["kwT"])
        P.dma(vw[:], vwd[:, j], writes=["vw"])
        P.dma(gt[:], gtd[j * 128:(j + 1) * 128, :], writes=["gt"])
        P.op("act", lambda e: e.activation(out=cg[:], in_=cg[:], func=AF.Sigmoid), writes=["cg"])
        P.op("dve", lambda e: e.tensor_tensor(out=cv[:], in0=cv[:], in1=cg[:], op=ALU.mult), reads=["cg"], writes=["cv"])
        for ch in range(4):
            en = "dve"
            tg = ("cacc", ch)
            P.op(en, lambda e, ch=ch: e.tensor_scalar(out=cacc[:, ch, :], in0=cv[:, ch, 0:128], scalar1=dw[:, ch, 0:1],
                                                      scalar2=dw[:, ch, 31:32], op0=ALU.mult, op1=ALU.add),
                 reads=["cv", "dw"], writes=[tg])
            for w in range(1, 31):
                P.op(en, lambda e, ch=ch, w=w: e.scalar_tensor_tensor(out=cacc[:, ch, :], in0=cv[:, ch, w:w + 128],
                                                                        scalar=dw[:, ch, w:w + 1], in1=cacc[:, ch, :],
                                                                        op0=ALU.mult, op1=ALU.add),
                     reads=["cv", "dw"], writes=[tg])
        for ch in range(4):
            P.op("pe", lambda e, ch=ch: e.transpose(out=bank[B_X][:, ch * 128:(ch + 1) * 128], in_=cacc[:, ch, :], identity=ident32[:]),
                 reads=[("cacc", ch), "ident32"], writes=[bk(B_X)])
        P.op("act", lambda e: e.copy(out=w512[2][:], in_=bank[B_X][:]), writes=[("w512", 2), bk(B_X)])
        x3 = w512[2][:].rearrange("p (h d) -> p h d", d=512)
        layer_norm_free(("w512", 2), x3, 1, 512, lng, lnb, x3, ("w512", 2), "lng", "lnb")
        P.op("act", lambda e: e.activation(out=yt[:, 0:512], in_=w512[2][:], func=AF.Silu), reads=[("w512", 2)], writes=["yt"])
        P.dma(rvo[:], rvod[j * 128:(j + 1) * 128, :], writes=["rvo"])
        for h in range(4):
            P.op("pe", lambda e, h=h: e.matmul(bank[B_X][:, h * 128:(h + 1) * 128], lhsT=kzT[:, h, :], rhs=qpT[:, h, :], start=True, stop=True),
                 reads=["kzT", "qpT"], writes=[bk(B_X)])
        P.op("dve", lambda e: e.tensor_tensor(out=innT[:], in0=bank[B_X][:].rearrange("p (h d) -> p h d", d=128),
                                              in1=tri[:].unsqueeze(1).to_broadcast([128, 4, 128]), op=ALU.mult),
             reads=["tri"], writes=["innT", bk(B_X)])
        for h in range(4):
            P.op("pe", lambda e, h=h: e.matmul(bank[B_O][:, h * 128:(h + 1) * 128], lhsT=innT[:, h, :], rhs=rvo[:, h * 128:(h + 1) * 128],
                                               start=True, stop=False),
                 reads=["innT", "rvo"], writes=[bk(B_O)])
            P.op("pe", lambda e, h=h, j=j: e.matmul(bank[B_O][:, h * 128:(h + 1) * 128], lhsT=qpT[:, h, :], rhs=Tb[:, j, h * 128:(h + 1) * 128],
                                                    start=False, stop=True),
                 reads=["qpT", "Tb"], writes=[bk(B_O)])
        P.op("act", lambda e: e.copy(out=w512[2][:], in_=bank[B_O][:]), writes=[("w512", 2), bk(B_O)])
        x3 = w512[2][:].rearrange("p (h d) -> p h d", d=128)
        layer_norm_free(("w512", 2), x3, 4, 128, gng, gnb, x3, ("w512", 2), "gng", "gnb")
        P.op("act", lambda e: e.activation(out=rg[:], in_=rg[:], func=AF.Silu), writes=["rg"])
        P.op("dve", lambda e: e.tensor_tensor(out=yt[:, 1536:2048], in0=w512[2][:], in1=rg[:], op=ALU.mult),
             reads=[("w512", 2), "rg"], writes=["yt"])
        P.op("act", lambda e: e.activation(out=gsg[:], in_=gt[:], func=AF.Sigmoid), reads=["gt"], writes=["gsg"])
        P.op("dve", lambda e, j=j: e.tensor_scalar(out=mk[:], in0=nt16[:], scalar1=q0c[:, 0, j:j + 1], scalar2=None, op0=ALU.is_le),
             reads=["nt16", "q0c"], writes=["mk"])
        P.op("dve", lambda e, j=j: e.tensor_scalar(out=vld[:], in0=b64[:], scalar1=q0c[:, 0, j:j + 1], scalar2=None, op0=ALU.is_le),
             reads=["b64", "q0c"], writes=["vld"])
        for g in range(2):
            qg = qT[:].rearrange("p h q -> p (h q)")[:, 512 * g:512 * g + 512]

            def score(lhsT, rd):
                sb_ = sbanks[nev[0] % 2]
                P.op("pe", lambda e, sb_=sb_, lhsT=lhsT, qg=qg: e.matmul(bank[sb_][:], lhsT=lhsT, rhs=qg, start=True, stop=True),
                     reads=["qT"] + rd, writes=[bk(sb_)])
                return sb_

            def mask_sb(mask_ap, tags):
                def f(b):
                    P.op("dve", lambda e, b=b: e.tensor_tensor(out=eTm[b][:], in0=eT[b][:].rearrange("p (h q) -> p h q", q=128),
                                                               in1=mask_ap.unsqueeze(1).to_broadcast([128, 4, 128]), op=ALU.mult),
                         reads=[("eT", b)] + tags, writes=[("eTm", b)])
                return f

            for ch in range(NCH):
                sb_ = score(kcT[:, g, ch * 128:(ch + 1) * 128], ["kcT"])
                attend(sb_, mask_sb(mk[:, ch, :], ["mk"]), lambda g=g, ch=ch: vc[:, g, ch, :], ch == 0, ch == NCH - 1, ["vc"])
                b = (nev[0] - 1) % 2
                for h in range(4):
                    P.op("pe", lambda e, b=b, h=h, ch=ch: e.matmul(bank[B_M][:, h * 128:(h + 1) * 128], lhsT=eTm[b][:, h, :], rhs=cover[:, ch, :],
                                                                   start=(ch == 0 and h == 0), stop=(ch == NCH - 1), skip_group_check=True),
                         reads=[("eTm", b), "cover"], writes=[bk(B_M)])
            P.op("dve", lambda e: e.tensor_scalar(out=lrec[:, 0:4], in0=bank[B_L][:, 0:4], scalar1=1e-30, scalar2=None, op0=ALU.max),
                 writes=["lrec", bk(B_L)])
            P.op("dve", lambda e: e.reciprocal(out=lrec[:, 4:8], in_=lrec[:, 0:4]), writes=["lrec"])
            P.op("dve", lambda e: e.tensor_scalar(out=imp[:], in0=bank[B_M][:, 0:128], scalar1=lrec[:, 4:5], scalar2=None, op0=ALU.mult),
                 reads=["lrec"], writes=["imp", bk(B_M)])
            for h in range(1, 4):
                P.op("dve", lambda e, h=h: e.scalar_tensor_tensor(out=imp[:], in0=bank[B_M][:, h * 128:(h + 1) * 128], scalar=lrec[:, 4 + h:5 + h],
                                                                   in1=imp[:], op0=ALU.mult, op1=ALU.add),
                     reads=["lrec"], writes=["imp", bk(B_M)])
            finish_branch(g, 0, True)
            P.op("dve", lambda e: e.tensor_scalar(out=impw[:], in0=vld[:], scalar1=1.0, scalar2=1e30, op0=ALU.subtract, op1=ALU.mult),
                 reads=["vld"], writes=["impw"])
            P.op("dve", lambda e: e.tensor_tensor(out=imp[:], in0=imp[:], in1=vld[:], op=ALU.mult), reads=["vld"], writes=["imp"])
            P.op("dve", lambda e: e.tensor_tensor(out=imp[:], in0=imp[:], in1=impw[:], op=ALU.add), reads=["impw"], writes=["imp"])
            P.op("dve", lambda e, j=j: e.tensor_scalar(out=impw[:], in0=fm[:], scalar1=q0c[:, 1, j:j + 1], scalar2=None, op0=ALU.is_equal),
                 reads=["fm", "q0c"], writes=["impw"])
            P.op("dve", lambda e: e.tensor_tensor(out=impw[:], in0=impw[:], in1=f0[:], op=ALU.max), reads=["f0"], writes=["impw"])
            P.op("dve", lambda e: e.scalar_tensor_tensor(out=imp[:], in0=impw[:], scalar=1e30, in1=imp[:], op0=ALU.mult, op1=ALU.max),
                 reads=["impw"], writes=["imp"])
            P.op("dve", lambda e: e.max(out=mx8[:, 0:8], in_=imp[:]), reads=["imp"], writes=["mx8"])
            P.op("dve", lambda e: e.match_replace(out=impw[:], in_to_replace=mx8[:, 0:8], in_values=imp[:], imm_value=-3.0e38),
                 reads=["imp", "mx8"], writes=["impw"])
            P.op("dve", lambda e: e.max(out=mx8[:, 8:16], in_=impw[:]), reads=["impw"], writes=["mx8"])
            P.op("dve", lambda e: e.tensor_scalar(out=impw[:], in0=imp[:], scalar1=mx8[:, 15:16], scalar2=None, op0=ALU.is_ge),
                 reads=["imp", "mx8"], writes=["impw"])
            P.op("dve", lambda e: e.tensor_tensor(out=impw[:], in0=impw[:], in1=vld[:], op=ALU.mult), reads=["vld"], writes=["impw"])
            P.op("pe", lambda e: e.transpose(out=bank[B_M][:, 0:128], in_=impw[:], identity=ident32[:]), reads=["impw", "ident32"], writes=[bk(B_M)])
            P.op("act", lambda e: e.copy(out=selT[:], in_=bank[B_M][:, 0:128]), writes=["selT", bk(B_M)])
            nkt = 8 * j + 8
            for kt in range(nkt):
                eb = kt % 2
                P.op("pool", lambda e, kt=kt, eb=eb: e.tensor_copy(out=Et[eb][:].rearrange("p (a k) -> p a k", k=64),
                                                                   in_=ident[:, 2 * kt:2 * kt + 2].unsqueeze(2).to_broadcast([128, 2, 64])),
                     reads=["ident"], writes=[("Et", eb)])
                P.op("pe", lambda e, eb=eb: e.matmul(bank[B_M][:, 0:128], lhsT=Et[eb][:], rhs=selT[:], start=True, stop=True),
                     reads=[("Et", eb), "selT"], writes=[bk(B_M)])
                sb_ = score(ksT[:, g, kt * 128:(kt + 1) * 128], ["ksT"])
                if kt < 8 * j:
                    def mf(b):
                        P.op("dve", lambda e, b=b: e.tensor_tensor(out=eTm[b][:], in0=eT[b][:].rearrange("p (h q) -> p h q", q=128),
                                                                   in1=bank[B_M][:, 0:128].unsqueeze(1).to_broadcast([128, 4, 128]), op=ALU.mult),
                             reads=[("eT", b)], writes=[("eTm", b), bk(B_M)])
                else:
                    def mf(b, o=kt - 8 * j):
                        P.op("dve", lambda e: e.tensor_tensor(out=m2[:], in0=bank[B_M][:, 0:128], in1=cmask[:, o, :], op=ALU.mult),
                             reads=["cmask"], writes=["m2", bk(B_M)])
                        P.op("dve", lambda e, b=b: e.tensor_tensor(out=eTm[b][:], in0=eT[b][:].rearrange("p (h q) -> p h q", q=128),
                                                                   in1=m2[:].unsqueeze(1).to_broadcast([128, 4, 128]), op=ALU.mult),
                             reads=[("eT", b), "m2"], writes=[("eTm", b)])
                attend(sb_, mf, lambda g=g, kt=kt: vs[:, kt, g * 128:(g + 1) * 128], kt == 0, kt == nkt - 1, ["vs"])
            finish_branch(g, 1, False)
            for o in range(5):
                sb_ = score(kwT[:, g, o * 128:(o + 1) * 128], ["kwT"])
                attend(sb_, mask_sb(wmask[:, j, o, :], ["wmask"]), lambda g=g, o=o: vw[:, g, o, :], o == 0, o == 4, ["vw"])
            finish_branch(g, 2, False)
            P.op("act", lambda e, g=g: e.copy(out=yt[:, 512 + 512 * g:1024 + 512 * g], in_=ynsa[:].rearrange("p h d -> p (h d)")),
                 reads=["ynsa"], writes=["yt"])
        P.dma(ycat[j * 128:(j + 1) * 128, :], yt[:], reads=["yt"], is_output=True)
    return C.finish()


def prep_k2a(i, NS, p32, p16, lw):
    SU = NS * 1024
    NCMP = (SU - 32) // 16 + 1
    NCH = (NCMP + 127) // 128
    bf = ml_dtypes.bfloat16
    qbs = [8 * j + i for j in range(NS)]
    own = np.concatenate([np.arange(qb * 128, qb * 128 + 128) for qb in qbs])
    m = {}
    m["ident"] = np.eye(128, dtype=np.float32).astype(bf)
    m["ident32"] = np.eye(128, dtype=np.float32)

    def halo(cols):
        a = np.concatenate([np.zeros((30, 512), np.float32), p32[:, cols:cols + 512]], axis=0)
        out = np.zeros((128, 4, NS, 158), np.float32)
        for j, qb in enumerate(qbs):
            blk = a[qb * 128:qb * 128 + 158].T.reshape(4, 128, 158)
            out[:, :, j, :] = blk.transpose(1, 0, 2)
        return out
    m["cvh"] = halo(O_CV)
    m["cgh"] = halo(O_CG)
    dwt = np.concatenate([lw["conv_dw_w"].T, lw["conv_dw_b"][:, None]], axis=1)
    m["dw"] = np.ascontiguousarray(dwt.reshape(4, 128, 32).transpose(1, 0, 2))
    m["lng"] = bc(lw["conv_ln_g"]); m["lnb"] = bc(lw["conv_ln_b"])
    m["kz"] = np.ascontiguousarray(p16[:SU, O_RK:O_RK + 512])
    m["rv"] = np.ascontiguousarray(p16[:SU, O_RV:O_RV + 512])
    m["rvo"] = np.ascontiguousarray(p16[own, O_RV:O_RV + 512])
    m["qpT"] = np.ascontiguousarray(p16[own, O_RQ:O_RQ + 512].reshape(NS, 128, 4, 128).transpose(3, 0, 2, 1))
    m["kzT"] = np.ascontiguousarray(p16[own, O_RK:O_RK + 512].reshape(NS, 128, 4, 128).transpose(3, 0, 2, 1))
    m["rg"] = np.ascontiguousarray(p32[own, O_RG:O_RG + 512])
    m["gng"] = bc(lw["ret_gn_g"]); m["gnb"] = bc(lw["ret_gn_b"])
    lg = ret_consts()
    m["dec"] = bc(np.repeat(np.exp(lg * 128.0), 128).astype(np.float32))
    kk = np.arange(128)
    m["tri"] = (kk[:, None] <= kk[None, :]).astype(np.float32).astype(bf)
    ind = np.zeros((128, 8), np.float32); ind[:, i] = 1.0
    m["ind"] = ind
    m["kcmpT"] = np.ascontiguousarray(np.stack([
        np.stack([p16[:SU, o + hd * 128:o + hd * 128 + 128].T for hd in range(2)]) for o in (O_KC, O_VC)]))
    m["w1"] = np.ascontiguousarray(np.stack([lw["nsa_cmp_k_w1"], lw["nsa_cmp_v_w1"]]))
    m["w2"] = np.ascontiguousarray(np.stack([lw["nsa_cmp_k_w2"], lw["nsa_cmp_v_w2"]]))
    m["peT"] = np.ascontiguousarray(np.stack([lw["nsa_pe_k"].T, lw["nsa_pe_v"].T]))
    m["qT"] = np.ascontiguousarray(p16[own, O_NQ:O_NQ + 1024].reshape(NS, 128, 8, 128).transpose(3, 0, 2, 1))
    m["ksT"] = np.ascontiguousarray(np.stack([p16[:SU, O_KS + g * 128:O_KS + g * 128 + 128].T for g in range(2)]))
    m["vs"] = np.ascontiguousarray(p16[:SU, O_VS:O_VS + 256])
    kwp = np.concatenate([np.zeros((512, 256), bf), p16[:, O_KW:O_KW + 256]], axis=0)
    vwp = np.concatenate([np.zeros((512, 256), bf), p16[:, O_VW:O_VW + 256]], axis=0)
    kwT = np.zeros((128, NS, 2, 640), bf)
    vw = np.zeros((128, NS, 2, 5, 128), bf)
    wmask = np.zeros((128, NS, 5, 128), np.float32)
    for j, qb in enumerate(qbs):
        q0 = qb * 128
        kwT[:, j] = kwp[q0:q0 + 640].reshape(640, 2, 128).transpose(2, 1, 0)
        vw[:, j] = vwp[q0:q0 + 640].reshape(5, 128, 2, 128).transpose(1, 2, 0, 3)
        for o in range(5):
            kp = q0 - 512 + o * 128 + kk[:, None]
            t = q0 + kk[None, :]
            wmask[:, j, o, :] = ((kp >= 0) & (kp <= t) & (kp > t - 512)).astype(np.float32)
    m["kwT"] = kwT; m["vw"] = vw; m["wmask"] = wmask.astype(bf)
    m["gt"] = np.ascontiguousarray(p32[own, O_GT:O_GT + 24])
    n = (np.arange(NCH)[None, :] * 128 + kk[:, None])
    blk = np.arange(128)
    cov = ((16 * n[:, :, None] <= 64 * blk[None, None, :] + 63) & (16 * n[:, :, None] + 31 >= 64 * blk[None, None, :])
           & (n[:, :, None] < NCMP))
    m["cover"] = cov.astype(np.float32).astype(bf)
    m["nt16"] = (16.0 * n[:, :, None] + 31.0 - kk[None, None, :]).astype(np.float32)
    m["b64"] = (64.0 * blk[None, :] - kk[:, None]).astype(np.float32)
    m["fm"] = (blk[None, :] - (kk[:, None] >= 64)).astype(np.float32)
    f0 = np.zeros((128, 128), np.float32); f0[:, 0] = 2.0
    m["f0"] = f0
    q0c = np.zeros((128, 2, NS), np.float32)
    for j, qb in enumerate(qbs):
        q0c[:, 0, j] = qb * 128; q0c[:, 1, j] = 2 * qb
    m["q0c"] = q0c
    cm = np.zeros((128, 8, 128), np.float32)
    for o in range(8):
        if o < i:
            cm[:, o, :] = 1.0
        elif o == i:
            cm[:, o, :] = (kk[:, None] <= kk[None, :])
    m["cmask"] = cm.astype(bf)
    return m, own


def run_k2a(p32, p16, lw):
    nc = build_k2a(8)
    in_maps, owns = [], []
    for i in range(NCORES):
        m, own = prep_k2a(i, 8, p32, p16, lw)
        in_maps.append(m)
        owns.append(own)
    res = run(nc, in_maps)
    y = np.zeros((S, 2048), np.float32)
    for i in range(NCORES):
        y[owns[i]] = res[i]["ycat"]
    return y


def build_k2b(NT=8):
    C = Ctx()
    P = C.P
    HT = 4 if NT >= 4 else NT
    yd = C.din("y", [NT * 128, D])
    xd = C.din("x", [NT * 128, D])
    wd = C.din("w", [D, D])
    gAd = C.din("gateA", [128, D])
    mAd = C.din("modA", [128, D])
    mSd = C.din("modS", [128, D])
    gNd = C.din("gN", [128, D])
    rwd = C.din("rw", [D, 32])
    rbd = C.din("rb", [128, 32])
    identd = C.din("ident", [128, 128], BF16)
    ident32d = C.din("ident32", [128, 128])
    x1d = C.dout("x1", [NT * 128, D])
    hfTd = C.dout("hfT", [128, 16, NT * 128], BF16)
    Gd = C.dout("G", [NT * 128, 32])

    ident = C.sb("ident", [128, 128], BF16)
    ident32 = C.sb("ident32", [128, 128])
    gA = C.sb("gA", [128, D])
    A = C.sb("A", [128, D])
    Sh = C.sb("Sh", [128, D])
    rw = C.sb("rw", [128, 16, 32])
    rb = C.sb("rb", [128, 32])
    P.dma(ident[:], identd, writes=["ident"])
    P.dma(ident32[:], ident32d, writes=["ident32"])
    P.dma(gA[:], gAd, writes=["gA"])
    P.dma(A[:], mAd, writes=["A"])
    P.dma(Sh[:], mSd, writes=["Sh"])
    P.dma(rw[:], rwd.rearrange("(kc p) n -> p kc n", p=128), writes=["rw"])
    P.dma(rb[:], rbd, writes=["rb"])
    sq = C.sb("sq", [128, D])
    P.dma(sq[:], gNd, writes=["sq"])
    P.op("dve", lambda e: e.scalar_tensor_tensor(out=A[:], in0=A[:], scalar=1.0, in1=sq[:], op0=ALU.add, op1=ALU.mult),
         reads=["sq"], writes=["A"])

    xt = [C.sb("xt%d" % i, [128, D]) for i in range(HT)]
    yT = [C.sb("yT%d" % i, [128, 16, 128], BF16) for i in range(HT)]
    yb = C.sb("yb", [128, D], BF16)
    h32 = C.sb("h32", [128, D])
    hb = C.sb("hb", [128, D], BF16)
    hT = C.sb("hT", [128, 16, 128], BF16)
    hT32 = C.sb("hT32", [128, 16, 128])
    st = C.sb("st", [128, 2])
    wst = [C.sb("wst%d" % i, [128, 8, 512]) for i in range(2)]
    wbf = C.sb("wbf", [128, 16, 512], BF16)
    tmp = [C.sb("tmp%d" % i, [128, 512]) for i in range(2)]
    lg = C.sb("lg", [128, 32])
    ex = C.sb("ex", [128, 32])
    mk = C.sb("mk", [128, 32])
    mx = C.sb("mx", [128, 8])
    s1 = C.sb("s1", [128, 2])
    pT = C.ps("pT", [128, 16, 128], BF16)
    pY = [C.ps("pY%d" % i, [128, 512]) for i in range(2)]
    pF = [C.ps("pF%d" % i, [128, 4, 128]) for i in range(2)]
    pL = C.ps("pL", [128, 512])
    wv = wd.rearrange("(kc p) n -> p kc n", p=128)
    nst = 0
    nev = 0
    for half in range(NT // HT):
        for tt in range(HT):
            t = half * HT + tt
            P.dma(xt[tt][:], xd[t * 128:(t + 1) * 128, :], writes=[("xt", tt)])
            P.dma(sq[:], yd[t * 128:(t + 1) * 128, :], writes=["sq"])
            P.op("pool", lambda e: e.tensor_copy(out=yb[:], in_=sq[:]), reads=["sq"], writes=["yb"])
            for kc in range(16):
                P.op("pe", lambda e, kc=kc: e.transpose(out=pT[:, kc, :], in_=yb[:, kc * 128:(kc + 1) * 128], identity=ident[:]),
                     reads=["yb", "ident"], writes=["pT"])
            P.op("act", lambda e, tt=tt: e.copy(out=yT[tt][:], in_=pT[:]), writes=[("yT", tt), "pT"])
        for cc in range(4):
            c0 = cc * 512
            for hf in range(2):
                sbuf = nst % 2
                nst += 1
                P.dma(wst[sbuf][:], wv[:, hf * 8:(hf + 1) * 8, c0:c0 + 512], writes=[("wst", sbuf)])
                P.op("pool", lambda e, sbuf=sbuf, hf=hf: e.tensor_copy(out=wbf[:, hf * 8:(hf + 1) * 8, :], in_=wst[sbuf][:]),
                     reads=[("wst", sbuf)], writes=[("wbf", hf)])
            for tt in range(HT):
                pb = nev % 2
                nev += 1
                for kc in range(16):
                    P.op("pe", lambda e, pb=pb, tt=tt, kc=kc: e.matmul(pY[pb][:], lhsT=yT[tt][:, kc, :], rhs=wbf[:, kc, :],
                                                                       start=(kc == 0), stop=(kc == 15)),
                         reads=[("yT", tt), ("wbf", kc // 8)], writes=[("pY", pb)])
                P.op("dve", lambda e, pb=pb, c0=c0: e.tensor_tensor(out=tmp[pb][:], in0=pY[pb][:], in1=gA[:, c0:c0 + 512], op=ALU.mult),
                     reads=["gA"], writes=[("tmp", pb), ("pY", pb)])
                P.op("pool", lambda e, pb=pb, tt=tt, c0=c0: e.tensor_tensor(out=xt[tt][:, c0:c0 + 512], in0=xt[tt][:, c0:c0 + 512],
                                                                            in1=tmp[pb][:], op=ALU.add),
                     reads=[("tmp", pb)], writes=[("xt", tt)])
        for tt in range(HT):
            t = half * HT + tt
            x1 = xt[tt]
            P.dma(x1d[t * 128:(t + 1) * 128, :], x1[:], reads=[("xt", tt)], is_output=True)
            P.op("act", lambda e, x1=x1: e.activation(out=sq[:], in_=x1[:], func=AF.Square), reads=[("xt", tt)], writes=["sq"])
            P.op("dve", lambda e: e.reduce_sum(out=st[:, 0:1], in_=sq[:], axis=AX.X), reads=["sq"], writes=["st"])
            P.op("act", lambda e: e.activation(out=st[:, 1:2], in_=st[:, 0:1], func=AF.Sqrt, scale=1.0 / D, bias=EPS), writes=["st"])
            P.op("dve", lambda e: e.reciprocal(out=st[:, 0:1], in_=st[:, 1:2]), writes=["st"])
            P.op("dve", lambda e, x1=x1: e.scalar_tensor_tensor(out=sq[:], in0=x1[:], scalar=st[:, 0:1], in1=A[:], op0=ALU.mult, op1=ALU.mult),
                 reads=[("xt", tt), "st", "A"], writes=["sq"])
            P.op("dve", lambda e: e.tensor_tensor(out=h32[:], in0=sq[:], in1=Sh[:], op=ALU.add), reads=["sq", "Sh"], writes=["h32"])
            P.op("pool", lambda e: e.tensor_copy(out=hb[:], in_=h32[:]), reads=["h32"], writes=["hb"])
            for kc in range(16):
                P.op("pe", lambda e, kc=kc: e.transpose(out=pT[:, kc, :], in_=hb[:, kc * 128:(kc + 1) * 128], identity=ident[:]),
                     reads=["hb", "ident"], writes=["pT"])
            P.op("act", lambda e: e.copy(out=hT[:], in_=pT[:]), writes=["hT", "pT"])
            P.dma(hfTd[:, :, t * 128:(t + 1) * 128], hT[:], reads=["hT"], is_output=True)
            for q4 in range(4):
                fb = q4 % 2
                for u in range(4):
                    kc = q4 * 4 + u
                    P.op("pe", lambda e, fb=fb, u=u, kc=kc: e.transpose(out=pF[fb][:, u, :], in_=h32[:, kc * 128:(kc + 1) * 128], identity=ident32[:]),
                         reads=["h32", "ident32"], writes=[("pF", fb)])
                P.op("act", lambda e, fb=fb, q4=q4: e.copy(out=hT32[:, q4 * 4:q4 * 4 + 4, :], in_=pF[fb][:]), writes=[("hT32", q4), ("pF", fb)])
            for kc in range(16):
                P.op("pe", lambda e, kc=kc: e.matmul(pL[:, 0:32], lhsT=hT32[:, kc, :], rhs=rw[:, kc, :], start=(kc == 0), stop=(kc == 15)),
                     reads=[("hT32", kc // 4), "rw"], writes=["pL"])
            P.op("dve", lambda e: e.tensor_tensor(out=lg[:], in0=pL[:, 0:32], in1=rb[:], op=ALU.add), reads=["rb"], writes=["lg", "pL"])
            P.op("dve", lambda e: e.max(out=mx[:], in_=lg[:]), reads=["lg"], writes=["mx"])
            P.op("dve", lambda e: e.tensor_scalar(out=mk[:], in0=lg[:], scalar1=mx[:, 3:4], scalar2=None, op0=ALU.is_ge), reads=["lg", "mx"], writes=["mk"])
            P.op("dve", lambda e: e.tensor_scalar(out=ex[:], in0=lg[:], scalar1=mx[:, 0:1], scalar2=None, op0=ALU.subtract), reads=["lg", "mx"], writes=["ex"])
            P.op("act", lambda e: e.activation(out=ex[:], in_=ex[:], func=AF.Exp), writes=["ex"])
            P.op("dve", lambda e: e.tensor_tensor(out=ex[:], in0=ex[:], in1=mk[:], op=ALU.mult), reads=["mk"], writes=["ex"])
            P.op("dve", lambda e: e.reduce_sum(out=s1[:, 0:1], in_=ex[:], axis=AX.X), reads=["ex"], writes=["s1"])
            P.op("dve", lambda e: e.reciprocal(out=s1[:, 1:2], in_=s1[:, 0:1]), writes=["s1"])
            P.op("dve", lambda e: e.tensor_scalar(out=lg[:], in0=ex[:], scalar1=s1[:, 1:2], scalar2=None, op0=ALU.mult), reads=["ex", "s1"], writes=["lg"])
            P.dma(Gd[t * 128:(t + 1) * 128, :], lg[:], reads=["lg"], is_output=True)
    return C.finish()


def run_k2b(ycat, x2d, w_out_l, mod_l, gffn_l, router_w_l, router_b_l):
    nc = build_k2b(8)
    ident = np.eye(128, dtype=np.float32)
    in_maps = []
    for i in range(NCORES):
        sl = slice(1024 * i, 1024 * (i + 1))
        in_maps.append({"y": np.ascontiguousarray(ycat[sl]), "x": np.ascontiguousarray(x2d[sl]), "w": w_out_l,
                        "gateA": bc(mod_l[2 * D:3 * D]), "modA": bc(mod_l[4 * D:5 * D]), "modS": bc(mod_l[3 * D:4 * D]),
                        "gN": bc(gffn_l), "rw": router_w_l, "rb": bc(router_b_l),
                        "ident": ident.astype(ml_dtypes.bfloat16), "ident32": ident})
    res = run(nc, in_maps)
    x1 = np.concatenate([r["x1"] for r in res], axis=0)
    hfT = np.concatenate([r["hfT"] for r in res], axis=2)
    G = np.concatenate([r["G"] for r in res], axis=0)
    return x1, hfT, G


def build_k3(NTG=16, NE=4):
    C = Ctx()
    P = C.P
    TOK = NTG * 512
    hTd = C.din("hT", [128, 16, TOK], BF16)
    Gbd = C.din("Gb", [NE, 128, TOK])
    wgud = C.din("wgu", [NE, D, 2 * D])
    wdnd = C.din("wdn", [NE, D, D])
    bgud = C.din("bgu", [128, NE, 32])
    bdnd = C.din("bdn", [128, NE, 16])
    outd = C.dout("outT", [128, 16, TOK])

    hT = C.sb("hT", [128, 16, 512], BF16)
    Gb = C.sb("Gb", [128, NE, 512])
    bgu = C.sb("bgu", [128, NE, 32])
    bdn = C.sb("bdn", [128, NE, 16])
    actT = C.sb("actT", [128, NE, 16, 512], BF16)
    NST = 3
    wst = [C.sb("wst%d" % i, [128, 16, 128]) for i in range(NST)]
    wg = [C.sb("wg%d" % i, [128, 16, 128], BF16) for i in range(2)]
    wl = [C.sb("wl%d" % i, [128, 16, 128], BF16) for i in range(2)]
    wdb = [C.sb("wdb%d" % i, [128, 16, 128], BF16) for i in range(2)]
    gtt = [C.sb("gtt%d" % i, [128, 512]) for i in range(2)]
    stt = [C.sb("stt%d" % i, [128, 512]) for i in range(2)]
    ltt = [C.sb("ltt%d" % i, [128, 512]) for i in range(2)]
    ot = [C.sb("ot%d" % i, [128, 512]) for i in range(2)]
    pg = [C.ps("pg%d" % i, [128, 512]) for i in range(2)]
    pl = [C.ps("pl%d" % i, [128, 512]) for i in range(2)]
    po = [C.ps("po%d" % i, [128, 512]) for i in range(2)]
    P.dma(bgu[:], bgud, writes=["bgu"])
    P.dma(bdn[:], bdnd, writes=["bdn"])
    nst = [0]

    def stage(src, dst, dtag, eng):
        s_ = nst[0] % NST
        nst[0] += 1
        P.dma(wst[s_][:], src, writes=[("wst", s_)])
        if eng == "act":
            P.op("act", lambda e, s_=s_: e.copy(out=dst, in_=wst[s_][:]), reads=[("wst", s_)], writes=[dtag])
        else:
            P.op("pool", lambda e, s_=s_: e.tensor_copy(out=dst, in_=wst[s_][:]), reads=[("wst", s_)], writes=[dtag])

    ia = 0
    ib = 0
    for tg in range(NTG):
        t0 = tg * 512
        P.dma(hT[:], hTd[:, :, t0:t0 + 512], writes=["hT"])
        for e_ in range(NE):
            P.dma(Gb[:, e_, :], Gbd[e_][:, t0:t0 + 512], writes=["Gb"])
        for e_ in range(NE):
            wv = wgud[e_].rearrange("(kc p) n -> p kc n", p=128)
            for c in range(16):
                b = ia % 2
                ia += 1
                stage(wv[:, :, c * 128:(c + 1) * 128], wg[b][:], ("wg", b), "act")
                stage(wv[:, :, D + c * 128:D + (c + 1) * 128], wl[b][:], ("wl", b), "pool")
                for kc in range(16):
                    P.op("pe", lambda e, b=b, kc=kc: e.matmul(pg[b][:], lhsT=wg[b][:, kc, :], rhs=hT[:, kc, :], start=(kc == 0), stop=(kc == 15)),
                         reads=[("wg", b), "hT"], writes=[("pg", b)])
                for kc in range(16):
                    P.op("pe", lambda e, b=b, kc=kc: e.matmul(pl[b][:], lhsT=wl[b][:, kc, :], rhs=hT[:, kc, :], start=(kc == 0), stop=(kc == 15)),
                         reads=[("wl", b), "hT"], writes=[("pl", b)])
                P.op("dve", lambda e, b=b, e_=e_, c=c: e.tensor_scalar(out=gtt[b][:], in0=pg[b][:], scalar1=bgu[:, e_, c:c + 1], scalar2=7.0,
                                                                       op0=ALU.add, op1=ALU.min),
                     reads=["bgu"], writes=[("gtt", b), ("pg", b)])
                P.op("act", lambda e, b=b: e.activation(out=stt[b][:], in_=gtt[b][:], func=AF.Sigmoid, scale=1.702),
                     reads=[("gtt", b)], writes=[("stt", b)])
                P.op("dve", lambda e, b=b, e_=e_, c=c: e.tensor_scalar(out=ltt[b][:], in0=pl[b][:], scalar1=bgu[:, e_, 16 + c:17 + c], scalar2=7.0,
                                                                       op0=ALU.add, op1=ALU.min),
                     reads=["bgu"], writes=[("ltt", b), ("pl", b)])
                P.op("dve", lambda e, b=b: e.tensor_scalar(out=ltt[b][:], in0=ltt[b][:], scalar1=-7.0, scalar2=1.0, op0=ALU.max, op1=ALU.add),
                     writes=[("ltt", b)])
                P.op("pool", lambda e, b=b: e.tensor_tensor(out=gtt[b][:], in0=gtt[b][:], in1=stt[b][:], op=ALU.mult),
                     reads=[("stt", b)], writes=[("gtt", b)])
                P.op("pool", lambda e, b=b: e.tensor_tensor(out=gtt[b][:], in0=gtt[b][:], in1=ltt[b][:], op=ALU.mult),
                     reads=[("ltt", b)], writes=[("gtt", b)])
                P.op("dve", lambda e, b=b, e_=e_, c=c: e.tensor_tensor(out=actT[:, e_, c, :], in0=gtt[b][:], in1=Gb[:, e_, :], op=ALU.mult),
                     reads=[("gtt", b), "Gb"], writes=[("actT", e_)])
        for m in range(16):
            pb = m % 2
            for e_ in range(NE):
                b = ib % 2
                ib += 1
                stage(wdnd[e_].rearrange("(c p) n -> p c n", p=128)[:, :, m * 128:(m + 1) * 128], wdb[b][:], ("wdb", b),
                      "act" if e_ % 2 == 0 else "pool")
                for c in range(16):
                    P.op("pe", lambda e, b=b, c=c, e_=e_, pb=pb: e.matmul(po[pb][:], lhsT=wdb[b][:, c, :], rhs=actT[:, e_, c, :],
                                                                          start=(e_ == 0 and c == 0), stop=(e_ == NE - 1 and c == 15)),
                         reads=[("wdb", b), ("actT", e_)], writes=[("po", pb)])
            P.op("dve", lambda e, pb=pb, m=m: e.scalar_tensor_tensor(out=ot[pb][:], in0=Gb[:, 0, :], scalar=bdn[:, 0, m:m + 1], in1=po[pb][:],
                                                                     op0=ALU.mult, op1=ALU.add),
                 reads=["Gb", "bdn"], writes=[("ot", pb), ("po", pb)])
            for e_ in range(1, NE):
                P.op("dve", lambda e, pb=pb, m=m, e_=e_: e.scalar_tensor_tensor(out=ot[pb][:], in0=Gb[:, e_, :], scalar=bdn[:, e_, m:m + 1],
                                                                                in1=ot[pb][:], op0=ALU.mult, op1=ALU.add),
                     reads=["Gb", "bdn"], writes=[("ot", pb)])
            P.dma(outd[:, m, t0:t0 + 512], ot[pb][:], reads=[("ot", pb)], is_output=True)
    return C.finish()


def run_k3(hfT, G, wgu_l, bgu_l, wdn_l, bdn_l):
    nc = build_k3(16, 4)
    in_maps = []
    for i in range(NCORES):
        es = slice(4 * i, 4 * i + 4)
        Gb = np.ascontiguousarray(np.broadcast_to(G[:, es].T[:, None, :], (4, 128, S)))
        in_maps.append({"hT": hfT, "Gb": Gb, "wgu": wgu_l[es], "wdn": wdn_l[es],
                        "bgu": np.ascontiguousarray(bgu_l[es].reshape(4, 32, 128).transpose(2, 0, 1)),
                        "bdn": np.ascontiguousarray(bdn_l[es].reshape(4, 16, 128).transpose(2, 0, 1))})
    res = run(nc, in_maps)
    return [r["outT"] for r in res]


def build_k4(NT=8, final=False):
    C = Ctx()
    P = C.P
    x1d = C.din("x1", [NT * 128, D])
    pd = C.din("parts", [NCORES, NT * 128, D])
    gFd = C.din("gateF", [128, D])
    gfd = C.din("gfin", [128, D])
    od = C.dout("out", [NT * 128, D])
    gF = C.sb("gF", [128, D])
    gfin = C.sb("gfin", [128, D])
    P.dma(gF[:], gFd, writes=["gF"])
    P.dma(gfin[:], gfd, writes=["gfin"])
    xt = [C.sb("xt%d" % i, [128, D]) for i in range(2)]
    acc = [C.sb("acc%d" % i, [128, D]) for i in range(2)]
    pt = [C.sb("pt%d" % i, [128, D]) for i in range(3)]
    sq = C.sb("sq", [128, D])
    st = C.sb("st", [128, 2])
    npt = 0
    for t in range(NT):
        k = t % 2
        P.dma(xt[k][:], x1d[t * 128:(t + 1) * 128, :], writes=[("xt", k)])
        P.dma(acc[k][:], pd[0, t * 128:(t + 1) * 128, :], writes=[("acc", k)])
        for c in range(1, NCORES):
            b = npt % 3
            npt += 1
            P.dma(pt[b][:], pd[c, t * 128:(t + 1) * 128, :], writes=[("pt", b)])
            P.op("dve" if c % 2 else "pool", lambda e, k=k, b=b: e.tensor_tensor(out=acc[k][:], in0=acc[k][:], in1=pt[b][:], op=ALU.add),
                 reads=[("pt", b)], writes=[("acc", k)])
        P.op("dve", lambda e, k=k: e.tensor_tensor(out=acc[k][:], in0=acc[k][:], in1=gF[:], op=ALU.mult), reads=["gF"], writes=[("acc", k)])
        P.op("pool", lambda e, k=k: e.tensor_tensor(out=acc[k][:], in0=acc[k][:], in1=xt[k][:], op=ALU.add), reads=[("xt", k)], writes=[("acc", k)])
        if final:
            P.op("act", lambda e, k=k: e.activation(out=sq[:], in_=acc[k][:], func=AF.Square), reads=[("acc", k)], writes=["sq"])
            P.op("dve", lambda e: e.reduce_sum(out=st[:, 0:1], in_=sq[:], axis=AX.X), reads=["sq"], writes=["st"])
            P.op("act", lambda e: e.activation(out=st[:, 1:2], in_=st[:, 0:1], func=AF.Sqrt, scale=1.0 / D, bias=EPS), writes=["st"])
            P.op("dve", lambda e: e.reciprocal(out=st[:, 0:1], in_=st[:, 1:2]), writes=["st"])
            P.op("dve", lambda e, k=k: e.scalar_tensor_tensor(out=acc[k][:], in0=acc[k][:], scalar=st[:, 0:1], in1=gfin[:], op0=ALU.mult, op1=ALU.mult),
                 reads=["st", "gfin"], writes=[("acc", k)])
        P.dma(od[t * 128:(t + 1) * 128, :], acc[k][:], reads=[("acc", k)], is_output=True)
    return C.finish()


def run_k4(x1, parts, gate_f, gfin, final):
    nc = build_k4(8, final)
    in_maps = []
    for i in range(NCORES):
        sl = slice(1024 * i, 1024 * (i + 1))
        pp = np.stack([np.ascontiguousarray(p[:, :, sl].transpose(2, 1, 0)).reshape(1024, D) for p in parts])
        in_maps.append({"x1": np.ascontiguousarray(x1[sl]), "parts": pp, "gateF": bc(gate_f), "gfin": bc(gfin)})
    res = run(nc, in_maps)
    return np.concatenate([r["out"] for r in res], axis=0)


LAYER_KEYS = ("conv_dw_w", "conv_dw_b", "conv_ln_g", "conv_ln_b", "nsa_pe_k", "nsa_pe_v", "nsa_cmp_k_w1", "nsa_cmp_k_w2",
              "nsa_cmp_v_w1", "nsa_cmp_v_w2", "ret_gn_g", "ret_gn_b")


def kernel(x, c, ada_w, ada_b, norm_mix_g, w_in, conv_dw_w, conv_dw_b, conv_ln_g, conv_ln_b,
           nsa_pe_k, nsa_pe_v, nsa_cmp_k_w1, nsa_cmp_k_w2, nsa_cmp_v_w1, nsa_cmp_v_w2,
           ret_gn_g, ret_gn_b, w_out, norm_ffn_g, router_w, router_b,
           moe_w_gate_up, moe_b_gate_up, moe_w_down, moe_b_down, final_norm_g):
    loc = locals()
    f = lambda a: np.asarray(a, dtype=np.float32)
    xs = f(x)[0]
    mod = run_k0(f(c), f(ada_w), f(ada_b))
    tabs = rot_tables()
    for l in range(DEPTH):
        lw = {k: f(loc[k][l]) for k in LAYER_KEYS}
        p32, p16 = run_k1(xs, f(w_in[l]), mod[l], f(norm_mix_g[l]), tabs)
        ycat = run_k2a(p32, p16, lw)
        del p32, p16
        x1, hfT, G = run_k2b(ycat, xs, f(w_out[l]), mod[l], f(norm_ffn_g[l]), f(router_w[l]), f(router_b[l]))
        parts = run_k3(hfT, G, np.asarray(moe_w_gate_up[l]), f(moe_b_gate_up[l]), np.asarray(moe_w_down[l]), f(moe_b_down[l]))
        xs = run_k4(x1, parts, mod[l][5 * D:6 * D], f(final_norm_g), l == DEPTH - 1)
        del parts
    return xs[None].astype(np.float32)
```

```python
from contextlib import ExitStack

import numpy as np
import ml_dtypes
import concourse.bass as bass
import concourse.mybir as mybir
from concourse.bass_utils import run_bass_kernel_spmd

F32 = mybir.dt.float32
BF16 = mybir.dt.bfloat16
AF = mybir.ActivationFunctionType
ALU = mybir.AluOpType
AX = mybir.AxisListType

NCORES = 8
D = 2048
S = 8192
DEPTH = 2
INW = 5656
EPS = 1e-6

ENGS = ("pe", "act", "dve", "pool", "sp")
N_DMA_SEMS = 8
DMA_QS = ("sp", "act", "pool")


class Prog:
    def __init__(self, nc):
        self.nc = nc
        self.streams = {e: [] for e in ENGS}
        self.count = {e: 0 for e in ENGS}
        self.waited = {e: {} for e in ENGS}
        self.last_w = {}
        self.readers = {}
        self.dma_n = [0] * (N_DMA_SEMS * len(DMA_QS))
        self.dma_rr = [0] * len(DMA_QS)
        self.out_tokens = []

    def _deps(self, reads, writes):
        deps = []
        for b in reads:
            t = self.last_w.get(b)
            if t is not None:
                deps.append(t)
        for b in writes:
            t = self.last_w.get(b)
            if t is not None:
                deps.append(t)
            deps.extend(self.readers.get(b, {}).values())
        return deps

    def _wait(self, eng, tok):
        key, val = tok
        if key == eng == "pe":
            return
        w = self.waited[eng]
        if w.get(key, 0) >= val:
            return
        w[key] = val
        self.streams[eng].append(("wait", key, val))

    def _commit(self, tok, reads, writes):
        for b in writes:
            self.last_w[b] = tok
            self.readers[b] = {}
        for b in reads:
            if b in writes:
                continue
            self.readers.setdefault(b, {})[tok[0]] = tok

    def op(self, eng, fn, reads=(), writes=()):
        for t in self._deps(reads, writes):
            self._wait(eng, t)
        self.count[eng] += 1
        tok = (eng, self.count[eng])
        self.streams[eng].append(("op", fn))
        self._commit(tok, reads, writes)
        return tok

    def dma(self, out, in_, reads=(), writes=(), q="sp", is_output=False, **kw):
        for t in self._deps(reads, writes):
            self._wait(q, t)
        qi = DMA_QS.index(q)
        s = qi * N_DMA_SEMS + self.dma_rr[qi]
        self.dma_rr[qi] = (self.dma_rr[qi] + 1) % N_DMA_SEMS
        key = ("dma", s)
        if self.dma_n[s] > 0:
            self._wait(q, (key, 16 * self.dma_n[s]))
        self.dma_n[s] += 1
        tok = (key, 16 * self.dma_n[s])
        self.streams[q].append(("dma", out, in_, s, kw))
        self._commit(tok, reads, writes)
        if is_output:
            self.out_tokens.append(tok)
        return tok

    def barrier(self):
        toks = [(e, self.count[e]) for e in ENGS if self.count[e] > 0]
        toks += [(("dma", k), 16 * self.dma_n[k]) for k in range(len(self.dma_n)) if self.dma_n[k] > 0]
        for e in ENGS:
            for t in toks:
                self._wait(e, t)

    def emit(self):
        nc = self.nc
        with ExitStack() as st:
            esem = {e: st.enter_context(nc.semaphore("s_" + e)) for e in ENGS}
            dsem = [st.enter_context(nc.semaphore("d%d" % i)) for i in range(N_DMA_SEMS * len(DMA_QS))]
            for t in self.out_tokens:
                self._wait("sp", t)
            block = st.enter_context(nc.Block())

            def semof(key):
                return dsem[key[1]] if isinstance(key, tuple) else esem[key]

            def replay(ename):
                def f(e):
                    for it in self.streams[ename]:
                        if it[0] == "wait":
                            e.wait_ge(semof(it[1]), it[2])
                        elif it[0] == "op":
                            it[1](e).then_inc(esem[ename], 1)
                        else:
                            _, out, in_, s, kw = it
                            e.dma_start(out=out, in_=in_, **kw).then_inc(dsem[s], 16)
                return f

            block.tensor(replay("pe"))
            block.scalar(replay("act"))
            block.vector(replay("dve"))
            block.gpsimd(replay("pool"))
            block.sync(replay("sp"))


class Ctx:
    def __init__(self):
        self.nc = bass.Bass("TRN2", target_bir_lowering=False)
        self.st = ExitStack()
        self.P = Prog(self.nc)

    def din(self, name, shape, dt=F32):
        return self.nc.dram_tensor(name, list(shape), dt, kind="ExternalInput").ap()

    def sbn(self, name, shape, dt=F32):
        return self.sb("s_" + name, shape, dt)

    def dout(self, name, shape, dt=F32):
        return self.nc.dram_tensor(name, list(shape), dt, kind="ExternalOutput").ap()

    def sb(self, name, shape, dt=F32):
        return self.st.enter_context(self.nc.sbuf_tensor("sb_" + name, list(shape), dt))

    def ps(self, name, shape, dt=F32):
        return self.st.enter_context(self.nc.psum_tensor("ps_" + name, list(shape), dt))

    def finish(self):
        self.P.emit()
        self.st.close()
        return self.nc


def run(nc, in_maps):
    res = run_bass_kernel_spmd(nc, in_maps, core_ids=list(range(NCORES)))
    return res.results


def build_k0():
    C = Ctx()
    P = C.P
    c_in = C.din("c", [128, 16])
    w = C.din("w", [DEPTH, D, 1536])
    b = C.din("b", [DEPTH, 1536])
    out = C.dout("out", [DEPTH, 1536])
    craw = C.sb("craw", [128, 16])
    sc = C.sb("sc", [128, 16])
    wt = [C.sb("wt%d" % i, [128, 16, 512]) for i in range(2)]
    bt = [C.sb("bt%d" % i, [1, 512]) for i in range(2)]
    ot = [C.sb("ot%d" % i, [1, 512]) for i in range(2)]
    pp = [C.ps("pp%d" % i, [1, 512]) for i in range(2)]
    P.dma(craw[:], c_in, writes=["craw"])
    P.op("act", lambda e: e.activation(out=sc[:], in_=craw[:], func=AF.Silu), reads=["craw"], writes=["sc"])
    it = 0
    for l in range(DEPTH):
        wl = w[l].rearrange("(kc p) n -> p kc n", p=128)
        for n in range(3):
            k = it % 2
            it += 1
            P.dma(wt[k][:], wl[:, :, n * 512:(n + 1) * 512], writes=[("wt", k)])
            P.dma(bt[k][:], b[l:l + 1, n * 512:(n + 1) * 512], writes=[("bt", k)])
            for kc in range(16):
                P.op("pe", lambda e, k=k, kc=kc: e.matmul(pp[k][:], lhsT=sc[:, kc:kc + 1], rhs=wt[k][:, kc, :],
                                                          start=(kc == 0), stop=(kc == 15)),
                     reads=["sc", ("wt", k)], writes=[("pp", k)])
            P.op("dve", lambda e, k=k: e.tensor_tensor(out=ot[k][:], in0=pp[k][:], in1=bt[k][:], op=ALU.add),
                 reads=[("pp", k), ("bt", k)], writes=[("ot", k)])
            P.dma(out[l:l + 1, n * 512:(n + 1) * 512], ot[k][:], reads=[("ot", k)], is_output=True)
    return C.finish()


def run_k0(c, ada_w, ada_b):
    nc = build_k0()
    cin = np.ascontiguousarray(c.reshape(16, 128).T)
    in_maps = []
    for i in range(NCORES):
        sl = slice(1536 * i, 1536 * (i + 1))
        in_maps.append({"c": cin, "w": np.ascontiguousarray(ada_w[:, :, sl]),
                        "b": np.ascontiguousarray(ada_b[:, sl])})
    res = run(nc, in_maps)
    return np.concatenate([r["out"] for r in res], axis=1)


ORIG_SPLITS = (512, 512, 1024, 256, 256, 256, 256, 256, 256, 24, 512, 512, 512, 512)
ORIG_OFF = np.concatenate([[0], np.cumsum(ORIG_SPLITS)])
NEW_ORDER = (0, 1, 2, 3, 4, 5, 6, 7, 8, 10, 11, 12, 13, 9)
COL_PERM = np.concatenate([np.arange(ORIG_OFF[j], ORIG_OFF[j + 1]) for j in NEW_ORDER])
O_CV, O_CG, O_NQ, O_KC, O_VC, O_KS, O_VS, O_KW, O_VW, O_RQ, O_RK, O_RV, O_RG, O_GT = (
    0, 512, 1024, 2048, 2304, 2560, 2816, 3072, 3328, 3584, 4096, 4608, 5120, 5632)
CH_KIND = ["plain", "plain", "nsa4", "nsa4", "nsa2", "nsa2", "nsa2", "retq", "retk", "plain", "plain", "gate"]


def rot_tables():
    pos = np.arange(S, dtype=np.float32)
    invn = np.power(np.float32(500000.0), -np.arange(16, dtype=np.float32) * np.float32(2.0) / np.float32(32))
    angn = pos[:, None] * invn[None, :].astype(np.float32)
    invr = np.power(np.float32(10000.0), -np.arange(64, dtype=np.float32) * np.float32(2.0) / np.float32(128))
    angr = pos[:, None] * invr[None, :].astype(np.float32)
    return (np.cos(angn).astype(np.float32), np.sin(angn).astype(np.float32),
            np.cos(angr).astype(np.float32), np.sin(angr).astype(np.float32))


def ret_consts():
    lg = np.log(1.0 - 2.0 ** (-5.0 - np.arange(4, dtype=np.float64)))
    return lg


def ret_ztab():
    lg = ret_consts()
    i = (np.arange(S) % 128).astype(np.float64)
    zq = np.exp(lg[None, :] * (i[:, None] - 127.0))
    zk = np.exp(lg[None, :] * (127.0 - i[:, None])) * (128.0 ** -0.5)
    return np.concatenate([zq, zk], axis=1).astype(np.float32)


def emit_norm_T(C, x_src, A, Sh, hT, ident, ntiles, pfx=""):
    P = C.P
    xt = [C.sb(pfx + "nx%d" % i, [128, D]) for i in range(2)]
    sq = C.sb(pfx + "nsq", [128, D])
    hb = [C.sb(pfx + "nhb%d" % i, [128, D], BF16) for i in range(2)]
    st = [C.sb(pfx + "nst%d" % i, [128, 2]) for i in range(2)]
    pT = C.ps(pfx + "npT", [128, 16, 128], BF16)
    for t in range(ntiles):
        k = t % 2
        P.dma(xt[k][:], x_src(t), writes=[(pfx + "nx", k)])
        P.op("act", lambda e, k=k: e.activation(out=sq[:], in_=xt[k][:], func=AF.Square),
             reads=[(pfx + "nx", k)], writes=[pfx + "nsq"])
        P.op("dve", lambda e, k=k: e.reduce_sum(out=st[k][:, 0:1], in_=sq[:], axis=AX.X),
             reads=[pfx + "nsq"], writes=[(pfx + "nst", k)])
        P.op("act", lambda e, k=k: e.activation(out=st[k][:, 1:2], in_=st[k][:, 0:1], func=AF.Sqrt,
                                                scale=1.0 / D, bias=EPS),
             reads=[(pfx + "nst", k)], writes=[(pfx + "nst", k)])
        P.op("dve", lambda e, k=k: e.reciprocal(out=st[k][:, 0:1], in_=st[k][:, 1:2]),
             reads=[(pfx + "nst", k)], writes=[(pfx + "nst", k)])
        P.op("dve", lambda e, k=k: e.scalar_tensor_tensor(out=sq[:], in0=xt[k][:], scalar=st[k][:, 0:1], in1=A[:],
                                                          op0=ALU.mult, op1=ALU.mult),
             reads=[(pfx + "nx", k), (pfx + "nst", k), "A"], writes=[pfx + "nsq"])
        P.op("dve", lambda e, k=k: e.tensor_tensor(out=hb[k][:], in0=sq[:], in1=Sh[:], op=ALU.add),
             reads=[pfx + "nsq", "Sh"], writes=[(pfx + "nhb", k)])
        for kc in range(16):
            P.op("pe", lambda e, k=k, kc=kc: e.transpose(out=pT[:, kc, :], in_=hb[k][:, kc * 128:(kc + 1) * 128],
                                                         identity=ident[:]),
                 reads=[(pfx + "nhb", k), "ident"], writes=[pfx + "npT"])
        P.op("act", lambda e, t=t: e.copy(out=hT[t][:], in_=pT[:]), reads=[pfx + "npT"], writes=[("hT", t)])


def build_k1(NT=8):
    C = Ctx()
    P = C.P
    x = C.din("x", [NT * 128, D])
    w = C.din("w", [D, INW])
    modA = C.din("modA", [128, D])
    modS = C.din("modS", [128, D])
    gN = C.din("gN", [128, D])
    identd = C.din("ident", [128, 128], BF16)
    tabn = C.din("tabn", [2, NT * 128, 16])
    tabr = C.din("tabr", [2, NT * 128, 64])
    ztd = C.din("zt", [NT * 128, 8])
    o32 = C.dout("o32", [NT * 128, INW])
    o16 = C.dout("o16", [NT * 128, INW], BF16)

    ident = C.sb("ident", [128, 128], BF16)
    A = C.sb("A", [128, D])
    Sh = C.sb("Sh", [128, D])
    tn = C.sb("tn", [128, 2, NT, 16])
    tr = C.sb("tr", [128, 2, NT, 64])
    zt = C.sb("zt", [128, NT, 8])
    hT = [C.sb("hT%d" % t, [128, 16, 128], BF16) for t in range(NT)]
    P.dma(ident[:], identd, writes=["ident"])
    for ci in range(2):
        P.dma(tn[:, ci], tabn[ci].rearrange("(t p) f -> p t f", p=128), writes=["tn"])
    P.dma(zt[:], ztd.rearrange("(t p) f -> p t f", p=128), writes=["zt"])
    for ci in range(2):
        P.dma(tr[:, ci], tabr[ci].rearrange("(t p) f -> p t f", p=128), writes=["tr"])
    gtmp = C.sb("gtmp", [128, D])
    P.dma(A[:], modA, writes=["A"])
    P.dma(gtmp[:], gN, writes=["gtmp"])
    P.dma(Sh[:], modS, writes=["Sh"])
    P.op("dve", lambda e: e.scalar_tensor_tensor(out=A[:], in0=A[:], scalar=1.0, in1=gtmp[:], op0=ALU.add, op1=ALU.mult),
         reads=["gtmp"], writes=["A"])
    emit_norm_T(C, lambda t: x[t * 128:(t + 1) * 128, :], A, Sh, hT, ident, NT)

    wst = [C.sb("wst%d" % i, [128, 8, 512]) for i in range(2)]
    wbf = [C.sb("wbf%d" % i, [128, 16, 512], BF16) for i in range(2)]
    ob32 = [C.sb("ob32_%d" % i, [128, 512]) for i in range(2)]
    ob16 = [C.sb("ob16_%d" % i, [128, 512], BF16) for i in range(2)]
    tmp = [C.sb("rt%d" % i, [128, 4, 64]) for i in range(4)]
    pY = [C.ps("pY%d" % i, [128, 512]) for i in range(2)]
    wv = w.rearrange("(kc p) n -> p kc n", p=128)
    nst = 0
    nev = 0
    for cc in range(12):
        kind = CH_KIND[cc]
        ncol = 512 if kind != "gate" else 24
        c0 = cc * 512
        wb = cc % 2
        for half in range(2):
            sbuf = nst % 2
            nst += 1
            P.dma(wst[sbuf][:, :, 0:ncol], wv[:, half * 8:(half + 1) * 8, c0:c0 + ncol], writes=[("wst", sbuf)])
            P.op("pool", lambda e, sbuf=sbuf, wb=wb, half=half, ncol=ncol: e.tensor_copy(
                out=wbf[wb][:, half * 8:(half + 1) * 8, 0:ncol], in_=wst[sbuf][:, :, 0:ncol]),
                reads=[("wst", sbuf)], writes=[("wbf", wb, half)])
        for t in range(NT):
            pb = nev % 2
            nev += 1
            for kc in range(16):
                P.op("pe", lambda e, pb=pb, t=t, kc=kc, wb=wb, ncol=ncol: e.matmul(
                    pY[pb][:, 0:ncol], lhsT=hT[t][:, kc, :], rhs=wbf[wb][:, kc, 0:ncol], start=(kc == 0), stop=(kc == 15)),
                    reads=[("hT", t), ("wbf", wb, kc // 8)], writes=[("pY", pb)])
            ps = pY[pb]
            o3 = ob32[pb]
            o1 = ob16[pb]
            R = []
            PSX = [("pY", pb)]
            W32 = [("ob32", pb)]
            if kind in ("plain", "gate"):
                P.op("act", lambda e, ps=ps, o3=o3, ncol=ncol: e.copy(out=o3[:, 0:ncol], in_=ps[:, 0:ncol]), reads=R, writes=W32 + PSX)
            elif kind in ("nsa4", "nsa2"):
                nh = 4 if kind == "nsa4" else 2
                P.op("act", lambda e, ps=ps, o3=o3: e.copy(out=o3[:], in_=ps[:]), reads=R, writes=W32 + PSX)
                psv = ps[:].rearrange("p (h d) -> p h d", d=128)
                o3v = o3[:].rearrange("p (h d) -> p h d", d=128)
                cs = tn[:, 0, t:t + 1, :].to_broadcast([128, nh, 16])
                sn = tn[:, 1, t:t + 1, :].to_broadcast([128, nh, 16])
                x1 = psv[:, 0:nh, 0:16]
                x2 = psv[:, 0:nh, 16:32]
                tv = [tm[:, 0:nh, 0:16] for tm in tmp]
                for (dst, a, tb) in ((tv[0], x1, cs), (tv[1], x2, sn), (tv[2], x2, cs), (tv[3], x1, sn)):
                    P.op("dve", lambda e, dst=dst, a=a, tb=tb: e.tensor_tensor(out=dst, in0=a, in1=tb, op=ALU.mult),
                         reads=R + ["tn"], writes=["rtmp"] + PSX)
                P.op("dve", lambda e, o3v=o3v, tv=tv, nh=nh: e.tensor_tensor(out=o3v[:, 0:nh, 0:16], in0=tv[0], in1=tv[1], op=ALU.subtract),
                     reads=["rtmp"], writes=W32)
                P.op("dve", lambda e, o3v=o3v, tv=tv, nh=nh: e.tensor_tensor(out=o3v[:, 0:nh, 16:32], in0=tv[2], in1=tv[3], op=ALU.add),
                     reads=["rtmp"], writes=W32)
            else:
                ci = 0
                zo = 0 if kind == "retq" else 4
                psv = ps[:].rearrange("p (h d) -> p h d", d=128)
                o3v = o3[:].rearrange("p (h d) -> p h d", d=128)
                cs = tr[:, ci, t:t + 1, :].to_broadcast([128, 4, 64])
                sn = tr[:, ci + 1, t:t + 1, :].to_broadcast([128, 4, 64])
                x1 = psv[:, :, 0:64]
                x2 = psv[:, :, 64:128]
                tv = [tm[:] for tm in tmp]
                for (dst, a, tb) in ((tv[0], x1, cs), (tv[1], x2, sn), (tv[2], x2, cs), (tv[3], x1, sn)):
                    P.op("dve", lambda e, dst=dst, a=a, tb=tb: e.tensor_tensor(out=dst, in0=a, in1=tb, op=ALU.mult),
                         reads=R + ["tr"], writes=["rtmp"] + PSX)
                P.op("dve", lambda e, o3v=o3v, tv=tv: e.tensor_tensor(out=o3v[:, :, 0:64], in0=tv[0], in1=tv[1], op=ALU.subtract),
                     reads=["rtmp"], writes=W32)
                P.op("dve", lambda e, o3v=o3v, tv=tv: e.tensor_tensor(out=o3v[:, :, 64:128], in0=tv[2], in1=tv[3], op=ALU.add),
                     reads=["rtmp"], writes=W32)
                zb = zt[:, t, zo:zo + 4].unsqueeze(2).to_broadcast([128, 4, 128])
                P.op("dve", lambda e, o3v=o3v, zb=zb: e.tensor_tensor(out=o3v, in0=o3v, in1=zb, op=ALU.mult),
                     reads=["zt"], writes=W32)
            P.op("dve", lambda e, o3=o3, o1=o1, ncol=ncol: e.tensor_copy(out=o1[:, 0:ncol], in_=o3[:, 0:ncol]),
                 reads=W32, writes=[("ob16", pb)])
            P.dma(o32[t * 128:(t + 1) * 128, c0:c0 + ncol], o3[:, 0:ncol], reads=W32, is_output=True)
            P.dma(o16[t * 128:(t + 1) * 128, c0:c0 + ncol], o1[:, 0:ncol], reads=[("ob16", pb)], is_output=True)
    return C.finish()


def bc(v):
    return np.ascontiguousarray(np.broadcast_to(np.asarray(v, np.float32)[None, :], (128, v.shape[0])))


def run_k1(x2d, w_in_l, mod_l, g_l, tabs):
    nc = build_k1()
    cn, sn, cr, sr = tabs
    sc = np.float32(128 ** -0.5)
    wre = np.ascontiguousarray(w_in_l[:, COL_PERM])
    ident = np.eye(128, dtype=np.float32).astype(ml_dtypes.bfloat16)
    in_maps = []
    for i in range(NCORES):
        sl = slice(1024 * i, 1024 * (i + 1))
        in_maps.append({
            "x": np.ascontiguousarray(x2d[sl]), "w": wre,
            "modA": bc(mod_l[D:2 * D]), "modS": bc(mod_l[0:D]), "gN": bc(g_l), "ident": ident,
            "tabn": np.ascontiguousarray(np.stack([cn[sl], sn[sl]])),
            "tabr": np.ascontiguousarray(np.stack([cr[sl], sr[sl]])), "zt": np.ascontiguousarray(ret_ztab()[sl]),
        })
    res = run(nc, in_maps)
    return (np.concatenate([r["o32"] for r in res], axis=0), np.concatenate([r["o16"] for r in res], axis=0))


SCALE = 128 ** -0.5


def build_k2a(NS=8):
    SU = NS * 1024
    NKT = SU // 128
    NCMP = (SU - 32) // 16 + 1
    NCH = (NCMP + 127) // 128
    C = Ctx()
    P = C.P
    identd = C.din("ident", [128, 128], BF16)
    ident32d = C.din("ident32", [128, 128])
    cvh = C.din("cvh", [128, 4, NS, 158])
    cgh = C.din("cgh", [128, 4, NS, 158])
    dwd = C.din("dw", [128, 4, 32])
    lngd = C.din("lng", [128, 512])
    lnbd = C.din("lnb", [128, 512])
    kzd = C.din("kz", [SU, 512], BF16)
    rvd = C.din("rv", [SU, 512], BF16)
    qpTd = C.din("qpT", [128, NS, 4, 128], BF16)
    kzTd = C.din("kzT", [128, NS, 4, 128], BF16)
    rgd = C.din("rg", [NS * 128, 512])
    rvod = C.din("rvo", [NS * 128, 512], BF16)
    gngd = C.din("gng", [128, 512])
    gnbd = C.din("gnb", [128, 512])
    decd = C.din("dec", [128, 512])
    trid = C.din("tri", [128, 128], BF16)
    indd = C.din("ind", [128, 8])
    kcmpTd = C.din("kcmpT", [2, 2, 128, SU], BF16)
    w1d = C.din("w1", [2, 4096, 128])
    w2d = C.din("w2", [2, 128, 128])
    peTd = C.din("peT", [2, 128, 32])
    qTd = C.din("qT", [128, NS, 8, 128], BF16)
    ksTd = C.din("ksT", [2, 128, SU], BF16)
    vsd = C.din("vs", [SU, 256], BF16)
    kwTd = C.din("kwT", [128, NS, 2, 640], BF16)
    vwd = C.din("vw", [128, NS, 2, 5, 128], BF16)
    gtd = C.din("gt", [NS * 128, 24])
    coverd = C.din("cover", [128, NCH, 128], BF16)
    nt16d = C.din("nt16", [128, NCH, 128])
    b64d = C.din("b64", [128, 128])
    fmd = C.din("fm", [128, 128])
    f0d = C.din("f0", [128, 128])
    q0d = C.din("q0c", [128, 2, NS])
    cmaskd = C.din("cmask", [128, 8, 128], BF16)
    wmaskd = C.din("wmask", [128, NS, 5, 128], BF16)
    ycat = C.dout("ycat", [NS * 128, 2048])

    ident = C.sb("ident", [128, 128], BF16)
    ident32 = C.sb("ident32", [128, 128])
    ones = C.sb("ones", [128, 1], BF16)
    P.dma(ident[:], identd, writes=["ident"])
    P.dma(ident32[:], ident32d, writes=["ident32"])
    P.op("pool", lambda e: e.memset(ones[:], 1.0), writes=["ones"])

    NB = 8
    bank = [C.ps("bk%d" % i, [128, 512]) for i in range(NB)]

    def bk(i):
        return ("bk", i)

    kcT = C.sb("kcT", [128, 2, NCH * 128], BF16)
    vc = C.sb("vc", [128, 2, NCH, 128], BF16)
    P.op("pool", lambda e: e.memset(kcT[:], 0.0), writes=["kcT"])
    P.op("pool", lambda e: e.memset(vc[:], 0.0), writes=["vc"])
    with ExitStack() as ph1:
        def sb1(name, shape, dt=F32):
            return ph1.enter_context(C.nc.sbuf_tensor("sb_" + name, list(shape), dt))
        w1s = sb1("w1s", [128, 32, 128])
        w1b = sb1("w1b", [128, 32, 128], BF16)
        w2s = sb1("w2s", [128, 128])
        w2b = sb1("w2b", [128, 128], BF16)
        pes = sb1("pes", [128, 32])
        peb = sb1("peb", [128, 32], BF16)
        bia = sb1("bia", [128, 1])
        xT = sb1("xT", [128, SU], BF16)
        a1 = sb1("a1", [128, NCH * 128], BF16)
        P.op("pool", lambda e: e.memset(a1[:], 0.0), writes=["a1"])
        for kv in range(2):
            P.dma(w1s[:], w1d[kv].rearrange("(l d) o -> d l o", d=128), writes=["w1s"])
            P.dma(w2s[:], w2d[kv], writes=["w2s"])
            P.dma(pes[:], peTd[kv], writes=["pes"])
            P.op("pool", lambda e: e.tensor_copy(out=w1b[:], in_=w1s[:]), reads=["w1s"], writes=["w1b"])
            P.op("pool", lambda e: e.tensor_copy(out=w2b[:], in_=w2s[:]), reads=["w2s"], writes=["w2b"])
            P.op("pool", lambda e: e.tensor_copy(out=peb[:], in_=pes[:]), reads=["pes"], writes=["peb"])
            for l in range(32):
                P.op("pe", lambda e, l=l: e.matmul(bank[0][:, 0:1], lhsT=w1b[:, l, :], rhs=peb[:, l:l + 1],
                                                   start=(l == 0), stop=(l == 31)),
                     reads=["w1b", "peb"], writes=[bk(0)])
            P.op("act", lambda e: e.copy(out=bia[:], in_=bank[0][:, 0:1]), writes=["bia", bk(0)])
            for hd in range(2):
                P.dma(xT[:], kcmpTd[kv, hd], writes=["xT"])
                for l in range(32):
                    P.op("pe", lambda e, l=l: e.matmul(bank[1][:, 0:NCMP], lhsT=w1b[:, l, :],
                                                       rhs=xT[:, l:l + 16 * (NCMP - 1) + 1:16],
                                                       start=(l == 0), stop=(l == 31)),
                         reads=["w1b", "xT"], writes=[bk(1)])
                P.op("act", lambda e: e.activation(out=a1[:, 0:NCMP], in_=bank[1][:, 0:NCMP], func=AF.Silu, bias=bia[:, 0:1]),
                     reads=["bia"], writes=["a1", bk(1)])
                if kv == 0:
                    P.op("pe", lambda e: e.matmul(bank[2][:, 0:NCMP], lhsT=w2b[:], rhs=a1[:, 0:NCMP], start=True, stop=True),
                         reads=["w2b", "a1"], writes=[bk(2)])
                    P.op("act", lambda e, hd=hd: e.copy(out=kcT[:, hd, 0:NCMP], in_=bank[2][:, 0:NCMP]), writes=["kcT", bk(2)])
                else:
                    for ch in range(NCH):
                        P.op("pe", lambda e, ch=ch: e.matmul(bank[2][:, ch * 128:(ch + 1) * 128], lhsT=a1[:, ch * 128:(ch + 1) * 128],
                                                             rhs=w2b[:], start=True, stop=True),
                             reads=["w2b", "a1"], writes=[bk(2)])
                    P.op("act", lambda e, hd=hd: e.copy(out=vc[:, hd, :, :], in_=bank[2][:, 0:NCH * 128].rearrange("p (c d) -> p c d", d=128)),
                         writes=["vc", bk(2)])
        P.barrier()
    Tst = C.sb("Tst", [128, 512])
    Tacc = C.sb("Tacc", [128, NS, 512])
    Tb = C.sb("Tb", [128, NS, 512], BF16)
    dec = C.sb("dec", [128, 512])
    ind = C.sb("ind", [128, 8])
    kzt = [C.sb("kzt%d" % i, [128, 512], BF16) for i in range(2)]
    rvt = [C.sb("rvt%d" % i, [128, 512], BF16) for i in range(2)]
    P.dma(dec[:], decd, writes=["dec"])
    P.dma(ind[:], indd, writes=["ind"])
    P.op("pool", lambda e: e.memset(Tst[:], 0.0), writes=["Tst"])
    P.op("pool", lambda e: e.memset(Tacc[:], 0.0), writes=["Tacc"])
    for m in range(NKT):
        k = m % 2
        j = m // 8
        P.op("dve", lambda e, j=j, m=m: e.scalar_tensor_tensor(out=Tacc[:, j, :], in0=Tst[:], scalar=ind[:, m % 8:m % 8 + 1],
                                                              in1=Tacc[:, j, :], op0=ALU.mult, op1=ALU.add),
             reads=["Tst", "ind"], writes=["Tacc"])
        if m == NKT - 1:
            break
        P.dma(kzt[k][:], kzd[m * 128:(m + 1) * 128, :], writes=[("kzt", k)])
        P.dma(rvt[k][:], rvd[m * 128:(m + 1) * 128, :], writes=[("rvt", k)])
        for h in range(4):
            P.op("pe", lambda e, k=k, h=h: e.matmul(bank[3][:, h * 128:(h + 1) * 128], lhsT=kzt[k][:, h * 128:(h + 1) * 128],
                                                    rhs=rvt[k][:, h * 128:(h + 1) * 128], start=True, stop=True),
                 reads=[("kzt", k), ("rvt", k)], writes=[bk(3)])
        P.op("dve", lambda e: e.tensor_tensor(out=Tst[:], in0=Tst[:], in1=bank[3][:], op=ALU.add), writes=["Tst", bk(3)])
        P.op("dve", lambda e: e.tensor_tensor(out=Tst[:], in0=Tst[:], in1=dec[:], op=ALU.mult), reads=["dec"], writes=["Tst"])
    P.op("act", lambda e: e.copy(out=Tb[:], in_=Tacc[:]), reads=["Tacc"], writes=["Tb"])

    ksT = C.sb("ksT", [128, 2, SU], BF16)
    vs = C.sb("vs", [128, NKT, 256], BF16)
    for g in range(2):
        P.dma(ksT[:, g, :], ksTd[g], writes=["ksT"])
    for c4 in range(0, NKT, 16):
        n4 = min(16, NKT - c4)
        P.dma(vs[:, c4:c4 + n4, :], vsd[c4 * 128:(c4 + n4) * 128, :].rearrange("(t p) f -> p t f", p=128), writes=["vs"])
    cover = C.sb("cover", [128, NCH, 128], BF16)
    nt16 = C.sb("nt16", [128, NCH, 128])
    b64 = C.sb("b64", [128, 128])
    fm = C.sb("fm", [128, 128])
    f0 = C.sb("f0", [128, 128])
    q0c = C.sb("q0c", [128, 2, NS])
    cmask = C.sb("cmask", [128, 8, 128], BF16)
    wmask = C.sb("wmask", [128, NS, 5, 128], BF16)
    tri = C.sb("tri", [128, 128], BF16)
    dw = C.sb("dw", [128, 4, 32])
    lng = C.sb("lng", [128, 512])
    lnb = C.sb("lnb", [128, 512])
    gng = C.sb("gng", [128, 512])
    gnb = C.sb("gnb", [128, 512])
    for (t_, d_, nm) in ((cover, coverd, "cover"), (nt16, nt16d, "nt16"), (b64, b64d, "b64"), (fm, fmd, "fm"), (f0, f0d, "f0"),
                         (q0c, q0d, "q0c"), (cmask, cmaskd, "cmask"), (wmask, wmaskd, "wmask"), (tri, trid, "tri"), (dw, dwd, "dw"),
                         (lng, lngd, "lng"), (lnb, lnbd, "lnb"), (gng, gngd, "gng"), (gnb, gnbd, "gnb")):
        P.dma(t_[:], d_, writes=[nm])

    yt = C.sb("yt", [128, 2048])
    cv = C.sb("cv", [128, 4, 158])
    cg = C.sb("cg", [128, 4, 158])
    cacc = C.sb("cacc", [128, 4, 128])
    w512 = [C.sb("w512_%d" % i, [128, 512]) for i in range(3)]
    st8 = C.sb("st8", [128, 16])
    qpT = C.sb("qpT", [128, 4, 128], BF16)
    kzT = C.sb("kzT", [128, 4, 128], BF16)
    rg = C.sb("rg", [128, 512])
    rvo = C.sb("rvo", [128, 512], BF16)
    innT = C.sb("innT", [128, 4, 128], BF16)
    qT = C.sb("qT", [128, 8, 128], BF16)
    kwT = C.sb("kwT", [128, 2, 640], BF16)
    vw = C.sb("vw", [128, 2, 5, 128], BF16)
    gt = C.sb("gt", [128, 24])
    gsg = C.sb("gsg", [128, 24])
    eT = [C.sb("eT%d" % i, [128, 512]) for i in range(2)]
    eTm = [C.sb("eTm%d" % i, [128, 4, 128], BF16) for i in range(2)]
    mk = C.sb("mk", [128, NCH, 128], BF16)
    m2 = C.sb("m2", [128, 128], BF16)
    Et = [C.sb("Et%d" % i, [128, 128], BF16) for i in range(2)]
    imp = C.sb("imp", [128, 128])
    impw = C.sb("impw", [128, 128])
    vld = C.sb("vld", [128, 128])
    sel = C.sb("sel", [128, 128], BF16)
    selT = C.sb("selT", [128, 128], BF16)
    mx8 = C.sb("mx8", [128, 16])
    lrec = C.sb("lrec", [128, 8])
    ynsa = C.sb("ynsa", [128, 4, 128])
    B_S, B_O, B_L, B_M, B_X = 4, 5, 6, 7, 3
    sbanks = [4, 0]
    nev = [0]

    def attend(spsum_bank, mask_fn, vfn, first, last, extra_reads=()):
        b = nev[0] % 2
        nev[0] += 1
        P.op("act", lambda e, b=b: e.activation(out=eT[b][:], in_=bank[spsum_bank][:], func=AF.Exp, scale=SCALE),
             writes=[("eT", b), bk(spsum_bank)])
        mask_fn(b)
        for h in range(4):
            P.op("pe", lambda e, b=b, h=h: e.matmul(bank[B_O][:, h * 128:(h + 1) * 128], lhsT=eTm[b][:, h, :], rhs=vfn(),
                                                    start=(first and h == 0), stop=last, skip_group_check=True),
                 reads=[("eTm", b)] + list(extra_reads), writes=[bk(B_O)])
        for h in range(4):
            P.op("pe", lambda e, b=b, h=h: e.matmul(bank[B_L][:, h:h + 1], lhsT=eTm[b][:, h, :], rhs=ones[:],
                                                    start=(first and h == 0), stop=last, skip_group_check=True),
                 reads=[("eTm", b), "ones"], writes=[bk(B_L)])

    def finish_branch(g, br, first_branch):
        P.op("dve", lambda e: e.tensor_scalar(out=lrec[:, 0:4], in0=bank[B_L][:, 0:4], scalar1=1e-30, scalar2=None, op0=ALU.max),
             writes=["lrec", bk(B_L)])
        P.op("dve", lambda e: e.reciprocal(out=lrec[:, 4:8], in_=lrec[:, 0:4]), writes=["lrec"])
        P.op("dve", lambda e: e.tensor_tensor(out=lrec[:, 0:4], in0=lrec[:, 4:8], in1=gsg[:, br * 8 + 4 * g:br * 8 + 4 * g + 4], op=ALU.mult),
             reads=["gsg"], writes=["lrec"])
        wb = lrec[:, 0:4].unsqueeze(2).to_broadcast([128, 4, 128])
        ov = bank[B_O][:].rearrange("p (h d) -> p h d", d=128)
        if first_branch:
            P.op("dve", lambda e: e.tensor_tensor(out=ynsa[:], in0=ov, in1=wb, op=ALU.mult), reads=["lrec"], writes=["ynsa", bk(B_O)])
        else:
            tv = w512[0][:].rearrange("p (h d) -> p h d", d=128)
            P.op("dve", lambda e: e.tensor_tensor(out=tv, in0=ov, in1=wb, op=ALU.mult), reads=["lrec"], writes=[("w512", 0), bk(B_O)])
            P.op("dve", lambda e: e.tensor_tensor(out=ynsa[:], in0=ynsa[:], in1=tv, op=ALU.add), reads=[("w512", 0)], writes=["ynsa"])

    def layer_norm_free(src_tag, x3, nh, dd, gam, bet, out3, out_tag, gtag, btag):
        inv = 1.0 / dd
        P.op("dve", lambda e: e.reduce_sum(out=st8[:, 0:nh], in_=x3, axis=AX.X), reads=[src_tag], writes=["st8"])
        P.op("dve", lambda e: e.tensor_scalar(out=st8[:, 0:nh], in0=st8[:, 0:nh], scalar1=-inv, scalar2=None, op0=ALU.mult), writes=["st8"])
        mb = st8[:, 0:nh].unsqueeze(2).to_broadcast([128, nh, dd])
        P.op("dve", lambda e: e.tensor_tensor(out=x3, in0=x3, in1=mb, op=ALU.add), reads=["st8"], writes=[src_tag])
        sq3 = w512[1][:, 0:nh * dd].rearrange("p (h d) -> p h d", d=dd)
        P.op("act", lambda e: e.activation(out=sq3, in_=x3, func=AF.Square), reads=[src_tag], writes=[("w512", 1)])
        P.op("dve", lambda e: e.reduce_sum(out=st8[:, 4:4 + nh], in_=sq3, axis=AX.X), reads=[("w512", 1)], writes=["st8"])
        P.op("act", lambda e: e.activation(out=st8[:, 8:8 + nh], in_=st8[:, 4:4 + nh], func=AF.Sqrt, scale=inv, bias=EPS), writes=["st8"])
        P.op("dve", lambda e: e.reciprocal(out=st8[:, 12:12 + nh], in_=st8[:, 8:8 + nh]), writes=["st8"])
        rb = st8[:, 12:12 + nh].unsqueeze(2).to_broadcast([128, nh, dd])
        P.op("dve", lambda e: e.tensor_tensor(out=x3, in0=x3, in1=rb, op=ALU.mult), reads=["st8"], writes=[src_tag])
        g3 = gam[:, 0:nh * dd].rearrange("p (h d) -> p h d", d=dd)
        b3 = bet[:, 0:nh * dd].rearrange("p (h d) -> p h d", d=dd)
        P.op("dve", lambda e: e.tensor_tensor(out=x3, in0=x3, in1=g3, op=ALU.mult), reads=[gtag], writes=[src_tag])
        P.op("dve", lambda e: e.tensor_tensor(out=out3, in0=x3, in1=b3, op=ALU.add), reads=[src_tag, btag], writes=[out_tag])

    for j in range(NS):
        P.dma(cv[:], cvh[:, :, j, :], writes=["cv"])
        P.dma(cg[:], cgh[:, :, j, :], writes=["cg"])
        P.dma(qpT[:], qpTd[:, j], writes=["qpT"])
        P.dma(kzT[:], kzTd[:, j], writes=["kzT"])
        P.dma(rg[:], rgd[j * 128:(j + 1) * 128, :], writes=["rg"])
        P.dma(qT[:], qTd[:, j], writes=["qT"])
        P.dma(kwT[:], kwTd[:, j], writes=["kwT"])
        P.dma(vw[:], vwd[:, j], writes=["vw"])
        P.dma(gt[:], gtd[j * 128:(j + 1) * 128, :], writes=["gt"])
        P.op("act", lambda e: e.activation(out=cg[:], in_=cg[:], func=AF.Sigmoid), writes=["cg"])
        P.op("dve", lambda e: e.tensor_tensor(out=cv[:], in0=cv[:], in1=cg[:], op=ALU.mult), reads=["cg"], writes=["cv"])
        for ch in range(4):
            en = "dve"
            tg = ("cacc", ch)
            P.op(en, lambda e, ch=ch: e.tensor_scalar(out=cacc[:, ch, :], in0=cv[:, ch, 0:128], scalar1=dw[:, ch, 0:1],
                                                      scalar2=dw[:, ch, 31:32], op0=ALU.mult, op1=ALU.add),
                 reads=["cv", "dw"], writes=[tg])
            for w in range(1, 31):
                P.op(en, lambda e, ch=ch, w=w: e.scalar_tensor_tensor(out=cacc[:, ch, :], in0=cv[:, ch, w:w + 128],
                                                                        scalar=dw[:, ch, w:w + 1], in1=cacc[:, ch, :],
                                                                        op0=ALU.mult, op1=ALU.add),
                     reads=["cv", "dw"], writes=[tg])
        for ch in range(4):
            P.op("pe", lambda e, ch=ch: e.transpose(out=bank[B_X][:, ch * 128:(ch + 1) * 128], in_=cacc[:, ch, :], identity=ident32[:]),
                 reads=[("cacc", ch), "ident32"], writes=[bk(B_X)])
        P.op("act", lambda e: e.copy(out=w512[2][:], in_=bank[B_X][:]), writes=[("w512", 2), bk(B_X)])
        x3 = w512[2][:].rearrange("p (h d) -> p h d", d=512)
        layer_norm_free(("w512", 2), x3, 1, 512, lng, lnb, x3, ("w512", 2), "lng", "lnb")
        P.op("act", lambda e: e.activation(out=yt[:, 0:512], in_=w512[2][:], func=AF.Silu), reads=[("w512", 2)], writes=["yt"])
        P.dma(rvo[:], rvod[j * 128:(j + 1) * 128, :], writes=["rvo"])
        for h in range(4):
            P.op("pe", lambda e, h=h: e.matmul(bank[B_X][:, h * 128:(h + 1) * 128], lhsT=kzT[:, h, :], rhs=qpT[:, h, :], start=True, stop=True),
                 reads=["kzT", "qpT"], writes=[bk(B_X)])
        P.op("dve", lambda e: e.tensor_tensor(out=innT[:], in0=bank[B_X][:].rearrange("p (h d) -> p h d", d=128),
                                              in1=tri[:].unsqueeze(1).to_broadcast([128, 4, 128]), op=ALU.mult),
             reads=["tri"], writes=["innT", bk(B_X)])
        for h in range(4):
            P.op("pe", lambda e, h=h: e.matmul(bank[B_O][:, h * 128:(h + 1) * 128], lhsT=innT[:, h, :], rhs=rvo[:, h * 128:(h + 1) * 128],
                                               start=True, stop=False),
                 reads=["innT", "rvo"], writes=[bk(B_O)])
            P.op("pe", lambda e, h=h, j=j: e.matmul(bank[B_O][:, h * 128:(h + 1) * 128], lhsT=qpT[:, h, :], rhs=Tb[:, j, h * 128:(h + 1) * 128],
                                                    start=False, stop=True),
                 reads=["qpT", "Tb"], writes=[bk(B_O)])
        P.op("act", lambda e: e.copy(out=w512[2][:], in_=bank[B_O][:]), writes=[("w512", 2), bk(B_O)])
        x3 = w512[2][:].rearrange("p (h d) -> p h d", d=128)
        layer_norm_free(("w512", 2), x3, 4, 128, gng, gnb, x3, ("w512", 2), "gng", "gnb")
        P.op("act", lambda e: e.activation(out=rg[:], in_=rg[:], func=AF.Silu), writes=["rg"])
        P.op("dve", lambda e: e.tensor_tensor(out=yt[:, 1536:2048], in0=w512[2][:], in1=rg[:], op=ALU.mult),
             reads=[("w512", 2), "rg"], writes=["yt"])
        P.op("act", lambda e: e.activation(out=gsg[:], in_=gt[:], func=AF.Sigmoid), reads=["gt"], writes=["gsg"])
        P.op("dve", lambda e, j=j: e.tensor_scalar(out=mk[:], in0=nt16[:], scalar1=q0c[:, 0, j:j + 1], scalar2=None, op0=ALU.is_le),
             reads=["nt16", "q0c"], writes=["mk"])
        P.op("dve", lambda e, j=j: e.tensor_scalar(out=vld[:], in0=b64[:], scalar1=q0c[:, 0, j:j + 1], scalar2=None, op0=ALU.is_le),
             reads=["b64", "q0c"], writes=["vld"])
        for g in range(2):
            qg = qT[:].rearrange("p h q -> p (h q)")[:, 512 * g:512 * g + 512]

            def score(lhsT, rd):
                sb_ = sbanks[nev[0] % 2]
                P.op("pe", lambda e, sb_=sb_, lhsT=lhsT, qg=qg: e.matmul(bank[sb_][:], lhsT=lhsT, rhs=qg, start=True, stop=True),
                     reads=["qT"] + rd, writes=[bk(sb_)])
                return sb_

            def mask_sb(mask_ap, tags):
                def f(b):
                    P.op("dve", lambda e, b=b: e.tensor_tensor(out=eTm[b][:], in0=eT[b][:].rearrange("p (h q) -> p h q", q=128),
                                                               in1=mask_ap.unsqueeze(1).to_broadcast([128, 4, 128]), op=ALU.mult),
                         reads=[("eT", b)] + tags, writes=[("eTm", b)])
                return f

            for ch in range(NCH):
                sb_ = score(kcT[:, g, ch * 128:(ch + 1) * 128], ["kcT"])
                attend(sb_, mask_sb(mk[:, ch, :], ["mk"]), lambda g=g, ch=ch: vc[:, g, ch, :], ch == 0, ch == NCH - 1, ["vc"])
                b = (nev[0] - 1) % 2
                for h in range(4):
                    P.op("pe", lambda e, b=b, h=h, ch=ch: e.matmul(bank[B_M][:, h * 128:(h + 1) * 128], lhsT=eTm[b][:, h, :], rhs=cover[:, ch, :],
                                                                   start=(ch == 0 and h == 0), stop=(ch == NCH - 1), skip_group_check=True),
                         reads=[("eTm", b), "cover"], writes=[bk(B_M)])
            P.op("dve", lambda e: e.tensor_scalar(out=lrec[:, 0:4], in0=bank[B_L][:, 0:4], scalar1=1e-30, scalar2=None, op0=ALU.max),
                 writes=["lrec", bk(B_L)])
            P.op("dve", lambda e: e.reciprocal(out=lrec[:, 4:8], in_=lrec[:, 0:4]), writes=["lrec"])
            P.op("dve", lambda e: e.tensor_scalar(out=imp[:], in0=bank[B_M][:, 0:128], scalar1=lrec[:, 4:5], scalar2=None, op0=ALU.mult),
                 reads=["lrec"], writes=["imp", bk(B_M)])
            for h in range(1, 4):
                P.op("dve", lambda e, h=h: e.scalar_tensor_tensor(out=imp[:], in0=bank[B_M][:, h * 128:(h + 1) * 128], scalar=lrec[:, 4 + h:5 + h],
                                                                   in1=imp[:], op0=ALU.mult, op1=ALU.add),
                     reads=["lrec"], writes=["imp", bk(B_M)])
            finish_branch(g, 0, True)
            P.op("dve", lambda e: e.tensor_scalar(out=impw[:], in0=vld[:], scalar1=1.0, scalar2=1e30, op0=ALU.subtract, op1=ALU.mult),
                 reads=["vld"], writes=["impw"])
            P.op("dve", lambda e: e.tensor_tensor(out=imp[:], in0=imp[:], in1=vld[:], op=ALU.mult), reads=["vld"], writes=["imp"])
            P.op("dve", lambda e: e.tensor_tensor(out=imp[:], in0=imp[:], in1=impw[:], op=ALU.add), reads=["impw"], writes=["imp"])
            P.op("dve", lambda e, j=j: e.tensor_scalar(out=impw[:], in0=fm[:], scalar1=q0c[:, 1, j:j + 1], scalar2=None, op0=ALU.is_equal),
                 reads=["fm", "q0c"], writes=["impw"])
            P.op("dve", lambda e: e.tensor_tensor(out=impw[:], in0=impw[:], in1=f0[:], op=ALU.max), reads=["f0"], writes=["impw"])
            P.op("dve", lambda e: e.scalar_tensor_tensor(out=imp[:], in0=impw[:], scalar=1e30, in1=imp[:], op0=ALU.mult, op1=ALU.max),
                 reads=["impw"], writes=["imp"])
            P.op("dve", lambda e: e.max(out=mx8[:, 0:8], in_=imp[:]), reads=["imp"], writes=["mx8"])
            P.op("dve", lambda e: e.match_replace(out=impw[:], in_to_replace=mx8[:, 0:8], in_values=imp[:], imm_value=-3.0e38),
                 reads=["imp", "mx8"], writes=["impw"])
            P.op("dve", lambda e: e.max(out=mx8[:, 8:16], in_=impw[:]), reads=["impw"], writes=["mx8"])
            P.op("dve", lambda e: e.tensor_scalar(out=impw[:], in0=imp[:], scalar1=mx8[:, 15:16], scalar2=None, op0=ALU.is_ge),
                 reads=["imp", "mx8"], writes=["impw"])
            P.op("dve", lambda e: e.tensor_tensor(out=impw[:], in0=impw[:], in1=vld[:], op=ALU.mult), reads=["vld"], writes=["impw"])
            P.op("pe", lambda e: e.transpose(out=bank[B_M][:, 0:128], in_=impw[:], identity=ident32[:]), reads=["impw", "ident32"], writes=[bk(B_M)])
            P.op("act", lambda e: e.copy(out=selT[:], in_=bank[B_M][:, 0:128]), writes=["selT", bk(B_M)])
            nkt = 8 * j + 8
            for kt in range(nkt):
                eb = kt % 2
                P.op("pool", lambda e, kt=kt, eb=eb: e.tensor_copy(out=Et[eb][:].rearrange("p (a k) -> p a k", k=64),
                                                                   in_=ident[:, 2 * kt:2 * kt + 2].unsqueeze(2).to_broadcast([128, 2, 64])),
                     reads=["ident"], writes=[("Et", eb)])
                P.op("pe", lambda e, eb=eb: e.matmul(bank[B_M][:, 0:128], lhsT=Et[eb][:], rhs=selT[:], start=True, stop=True),
                     reads=[("Et", eb), "selT"], writes=[bk(B_M)])
                sb_ = score(ksT[:, g, kt * 128:(kt + 1) * 128], ["ksT"])
                if kt < 8 * j:
                    def mf(b):
                        P.op("dve", lambda e, b=b: e.tensor_tensor(out=eTm[b][:], in0=eT[b][:].rearrange("p (h q) -> p h q", q=128),
                                                                   in1=bank[B_M][:, 0:128].unsqueeze(1).to_broadcast([128, 4, 128]), op=ALU.mult),
                             reads=[("eT", b)], writes=[("eTm", b), bk(B_M)])
                else:
                    def mf(b, o=kt - 8 * j):
                        P.op("dve", lambda e: e.tensor_tensor(out=m2[:], in0=bank[B_M][:, 0:128], in1=cmask[:, o, :], op=ALU.mult),
                             reads=["cmask"], writes=["m2", bk(B_M)])
                        P.op("dve", lambda e, b=b: e.tensor_tensor(out=eTm[b][:], in0=eT[b][:].rearrange("p (h q) -> p h q", q=128),
                                                                   in1=m2[:].unsqueeze(1).to_broadcast([128, 4, 128]), op=ALU.mult),
                             reads=[("eT", b), "m2"], writes=[("eTm", b)])
                attend(sb_, mf, lambda g=g, kt=kt: vs[:, kt, g * 128:(g + 1) * 128], kt == 0, kt == nkt - 1, ["vs"])
            finish_branch(g, 1, False)
            for o in range(5):
                sb_ = score(kwT[:, g, o * 128:(o + 1) * 128], ["kwT"])
                attend(sb_, mask_sb(wmask[:, j, o, :], ["wmask"]), lambda g=g, o=o: vw[:, g, o, :], o == 0, o == 4, ["vw"])
            finish_branch(g, 2, False)
            P.op("act", lambda e, g=g: e.copy(out=yt[:, 512 + 512 * g:1024 + 512 * g], in_=ynsa[:].rearrange("p h d -> p (h d)")),
                 reads=["ynsa"], writes=["yt"])
        P.dma(ycat[j * 128:(j + 1) * 128, :], yt[:], reads=["yt"], is_output=True)
    return C.finish()


def prep_k2a(i, NS, p32, p16, lw):
    SU = NS * 1024
    NCMP = (SU - 32) // 16 + 1
    NCH = (NCMP + 127) // 128
    bf = ml_dtypes.bfloat16
    qbs = [8 * j + i for j in range(NS)]
    own = np.concatenate([np.arange(qb * 128, qb * 128 + 128) for qb in qbs])
    m = {}
    m["ident"] = np.eye(128, dtype=np.float32).astype(bf)
    m["ident32"] = np.eye(128, dtype=np.float32)

    def halo(cols):
        a = np.concatenate([np.zeros((30, 512), np.float32), p32[:, cols:cols + 512]], axis=0)
        out = np.zeros((128, 4, NS, 158), np.float32)
        for j, qb in enumerate(qbs):
            blk = a[qb * 128:qb * 128 + 158].T.reshape(4, 128, 158)
            out[:, :, j, :] = blk.transpose(1, 0, 2)
        return out
    m["cvh"] = halo(O_CV)
    m["cgh"] = halo(O_CG)
    dwt = np.concatenate([lw["conv_dw_w"].T, lw["conv_dw_b"][:, None]], axis=1)
    m["dw"] = np.ascontiguousarray(dwt.reshape(4, 128, 32).transpose(1, 0, 2))
    m["lng"] = bc(lw["conv_ln_g"]); m["lnb"] = bc(lw["conv_ln_b"])
    m["kz"] = np.ascontiguousarray(p16[:SU, O_RK:O_RK + 512])
    m["rv"] = np.ascontiguousarray(p16[:SU, O_RV:O_RV + 512])
    m["rvo"] = np.ascontiguousarray(p16[own, O_RV:O_RV + 512])
    m["qpT"] = np.ascontiguousarray(p16[own, O_RQ:O_RQ + 512].reshape(NS, 128, 4, 128).transpose(3, 0, 2, 1))
    m["kzT"] = np.ascontiguousarray(p16[own, O_RK:O_RK + 512].reshape(NS, 128, 4, 128).transpose(3, 0, 2, 1))
    m["rg"] = np.ascontiguousarray(p32[own, O_RG:O_RG + 512])
    m["gng"] = bc(lw["ret_gn_g"]); m["gnb"] = bc(lw["ret_gn_b"])
    lg = ret_consts()
    m["dec"] = bc(np.repeat(np.exp(lg * 128.0), 128).astype(np.float32))
    kk = np.arange(128)
    m["tri"] = (kk[:, None] <= kk[None, :]).astype(np.float32).astype(bf)
    ind = np.zeros((128, 8), np.float32); ind[:, i] = 1.0
    m["ind"] = ind
    m["kcmpT"] = np.ascontiguousarray(np.stack([
        np.stack([p16[:SU, o + hd * 128:o + hd * 128 + 128].T for hd in range(2)]) for o in (O_KC, O_VC)]))
    m["w1"] = np.ascontiguousarray(np.stack([lw["nsa_cmp_k_w1"], lw["nsa_cmp_v_w1"]]))
    m["w2"] = np.ascontiguousarray(np.stack([lw["nsa_cmp_k_w2"], lw["nsa_cmp_v_w2"]]))
    m["peT"] = np.ascontiguousarray(np.stack([lw["nsa_pe_k"].T, lw["nsa_pe_v"].T]))
    m["qT"] = np.ascontiguousarray(p16[own, O_NQ:O_NQ + 1024].reshape(NS, 128, 8, 128).transpose(3, 0, 2, 1))
    m["ksT"] = np.ascontiguousarray(np.stack([p16[:SU, O_KS + g * 128:O_KS + g * 128 + 128].T for g in range(2)]))
    m["vs"] = np.ascontiguousarray(p16[:SU, O_VS:O_VS + 256])
    kwp = np.concatenate([np.zeros((512, 256), bf), p16[:, O_KW:O_KW + 256]], axis=0)
    vwp = np.concatenate([np.zeros((512, 256), bf), p16[:, O_VW:O_VW + 256]], axis=0)
    kwT = np.zeros((128, NS, 2, 640), bf)
    vw = np.zeros((128, NS, 2, 5, 128), bf)
    wmask = np.zeros((128, NS, 5, 128), np.float32)
    for j, qb in enumerate(qbs):
        q0 = qb * 128
        kwT[:, j] = kwp[q0:q0 + 640].reshape(640, 2, 128).transpose(2, 1, 0)
        vw[:, j] = vwp[q0:q0 + 640].reshape(5, 128, 2, 128).transpose(1, 2, 0, 3)
        for o in range(5):
            kp = q0 - 512 + o * 128 + kk[:, None]
            t = q0 + kk[None, :]
            wmask[:, j, o, :] = ((kp >= 0) & (kp <= t) & (kp > t - 512)).astype(np.float32)
    m["kwT"] = kwT; m["vw"] = vw; m["wmask"] = wmask.astype(bf)
    m["gt"] = np.ascontiguousarray(p32[own, O_GT:O_GT + 24])
    n = (np.arange(NCH)[None, :] * 128 + kk[:, None])
    blk = np.arange(128)
    cov = ((16 * n[:, :, None] <= 64 * blk[None, None, :] + 63) & (16 * n[:, :, None] + 31 >= 64 * blk[None, None, :])
           & (n[:, :, None] < NCMP))
    m["cover"] = cov.astype(np.float32).astype(bf)
    m["nt16"] = (16.0 * n[:, :, None] + 31.0 - kk[None, None, :]).astype(np.float32)
    m["b64"] = (64.0 * blk[None, :] - kk[:, None]).astype(np.float32)
    m["fm"] = (blk[None, :] - (kk[:, None] >= 64)).astype(np.float32)
    f0 = np.zeros((128, 128), np.float32); f0[:, 0] = 2.0
    m["f0"] = f0
    q0c = np.zeros((128, 2, NS), np.float32)
    for j, qb in enumerate(qbs):
        q0c[:, 0, j] = qb * 128; q0c[:, 1, j] = 2 * qb
    m["q0c"] = q0c
    cm = np.zeros((128, 8, 128), np.float32)
    for o in range(8):
        if o < i:
            cm[:, o, :] = 1.0
        elif o == i:
            cm[:, o, :] = (kk[:, None] <= kk[None, :])
    m["cmask"] = cm.astype(bf)
    return m, own


def run_k2a(p32, p16, lw):
    nc = build_k2a(8)
    in_maps, owns = [], []
    for i in range(NCORES):
        m, own = prep_k2a(i, 8, p32, p16, lw)
        in_maps.append(m)
        owns.append(own)
    res = run(nc, in_maps)
    y = np.zeros((S, 2048), np.float32)
    for i in range(NCORES):
        y[owns[i]] = res[i]["ycat"]
    return y


def build_k2b(NT=8):
    C = Ctx()
    P = C.P
    HT = 4 if NT >= 4 else NT
    yd = C.din("y", [NT * 128, D])
    xd = C.din("x", [NT * 128, D])
    wd = C.din("w", [D, D])
    gAd = C.din("gateA", [128, D])
    mAd = C.din("modA", [128, D])
    mSd = C.din("modS", [128, D])
    gNd = C.din("gN", [128, D])
    rwd = C.din("rw", [D, 32])
    rbd = C.din("rb", [128, 32])
    identd = C.din("ident", [128, 128], BF16)
    ident32d = C.din("ident32", [128, 128])
    x1d = C.dout("x1", [NT * 128, D])
    hfTd = C.dout("hfT", [128, 16, NT * 128], BF16)
    Gd = C.dout("G", [NT * 128, 32])

    ident = C.sb("ident", [128, 128], BF16)
    ident32 = C.sb("ident32", [128, 128])
    gA = C.sb("gA", [128, D])
    A = C.sb("A", [128, D])
    Sh = C.sb("Sh", [128, D])
    rw = C.sb("rw", [128, 16, 32])
    rb = C.sb("rb", [128, 32])
    P.dma(ident[:], identd, writes=["ident"])
    P.dma(ident32[:], ident32d, writes=["ident32"])
    P.dma(gA[:], gAd, writes=["gA"])
    P.dma(A[:], mAd, writes=["A"])
    P.dma(Sh[:], mSd, writes=["Sh"])
    P.dma(rw[:], rwd.rearrange("(kc p) n -> p kc n", p=128), writes=["rw"])
    P.dma(rb[:], rbd, writes=["rb"])
    sq = C.sb("sq", [128, D])
    P.dma(sq[:], gNd, writes=["sq"])
    P.op("dve", lambda e: e.scalar_tensor_tensor(out=A[:], in0=A[:], scalar=1.0, in1=sq[:], op0=ALU.add, op1=ALU.mult),
         reads=["sq"], writes=["A"])

    xt = [C.sb("xt%d" % i, [128, D]) for i in range(HT)]
    yT = [C.sb("yT%d" % i, [128, 16, 128], BF16) for i in range(HT)]
    yb = C.sb("yb", [128, D], BF16)
    h32 = C.sb("h32", [128, D])
    hb = C.sb("hb", [128, D], BF16)
    hT = C.sb("hT", [128, 16, 128], BF16)
    hT32 = C.sb("hT32", [128, 16, 128])
    st = C.sb("st", [128, 2])
    wst = [C.sb("wst%d" % i, [128, 8, 512]) for i in range(2)]
    wbf = C.sb("wbf", [128, 16, 512], BF16)
    tmp = [C.sb("tmp%d" % i, [128, 512]) for i in range(2)]
    lg = C.sb("lg", [128, 32])
    ex = C.sb("ex", [128, 32])
    mk = C.sb("mk", [128, 32])
    mx = C.sb("mx", [128, 8])
    s1 = C.sb("s1", [128, 2])
    pT = C.ps("pT", [128, 16, 128], BF16)
    pY = [C.ps("pY%d" % i, [128, 512]) for i in range(2)]
    pF = [C.ps("pF%d" % i, [128, 4, 128]) for i in range(2)]
    pL = C.ps("pL", [128, 512])
    wv = wd.rearrange("(kc p) n -> p kc n", p=128)
    nst = 0
    nev = 0
    for half in range(NT // HT):
        for tt in range(HT):
            t = half * HT + tt
            P.dma(xt[tt][:], xd[t * 128:(t + 1) * 128, :], writes=[("xt", tt)])
            P.dma(sq[:], yd[t * 128:(t + 1) * 128, :], writes=["sq"])
            P.op("pool", lambda e: e.tensor_copy(out=yb[:], in_=sq[:]), reads=["sq"], writes=["yb"])
            for kc in range(16):
                P.op("pe", lambda e, kc=kc: e.transpose(out=pT[:, kc, :], in_=yb[:, kc * 128:(kc + 1) * 128], identity=ident[:]),
                     reads=["yb", "ident"], writes=["pT"])
            P.op("act", lambda e, tt=tt: e.copy(out=yT[tt][:], in_=pT[:]), writes=[("yT", tt), "pT"])
        for cc in range(4):
            c0 = cc * 512
            for hf in range(2):
                sbuf = nst % 2
                nst += 1
                P.dma(wst[sbuf][:], wv[:, hf * 8:(hf + 1) * 8, c0:c0 + 512], writes=[("wst", sbuf)])
                P.op("pool", lambda e, sbuf=sbuf, hf=hf: e.tensor_copy(out=wbf[:, hf * 8:(hf + 1) * 8, :], in_=wst[sbuf][:]),
                     reads=[("wst", sbuf)], writes=[("wbf", hf)])
            for tt in range(HT):
                pb = nev % 2
                nev += 1
                for kc in range(16):
                    P.op("pe", lambda e, pb=pb, tt=tt, kc=kc: e.matmul(pY[pb][:], lhsT=yT[tt][:, kc, :], rhs=wbf[:, kc, :],
                                                                       start=(kc == 0), stop=(kc == 15)),
                         reads=[("yT", tt), ("wbf", kc // 8)], writes=[("pY", pb)])
                P.op("dve", lambda e, pb=pb, c0=c0: e.tensor_tensor(out=tmp[pb][:], in0=pY[pb][:], in1=gA[:, c0:c0 + 512], op=ALU.mult),
                     reads=["gA"], writes=[("tmp", pb), ("pY", pb)])
                P.op("pool", lambda e, pb=pb, tt=tt, c0=c0: e.tensor_tensor(out=xt[tt][:, c0:c0 + 512], in0=xt[tt][:, c0:c0 + 512],
                                                                            in1=tmp[pb][:], op=ALU.add),
                     reads=[("tmp", pb)], writes=[("xt", tt)])
        for tt in range(HT):
            t = half * HT + tt
            x1 = xt[tt]
            P.dma(x1d[t * 128:(t + 1) * 128, :], x1[:], reads=[("xt", tt)], is_output=True)
            P.op("act", lambda e, x1=x1: e.activation(out=sq[:], in_=x1[:], func=AF.Square), reads=[("xt", tt)], writes=["sq"])
            P.op("dve", lambda e: e.reduce_sum(out=st[:, 0:1], in_=sq[:], axis=AX.X), reads=["sq"], writes=["st"])
            P.op("act", lambda e: e.activation(out=st[:, 1:2], in_=st[:, 0:1], func=AF.Sqrt, scale=1.0 / D, bias=EPS), writes=["st"])
            P.op("dve", lambda e: e.reciprocal(out=st[:, 0:1], in_=st[:, 1:2]), writes=["st"])
            P.op("dve", lambda e, x1=x1: e.scalar_tensor_tensor(out=sq[:], in0=x1[:], scalar=st[:, 0:1], in1=A[:], op0=ALU.mult, op1=ALU.mult),
                 reads=[("xt", tt), "st", "A"], writes=["sq"])
            P.op("dve", lambda e: e.tensor_tensor(out=h32[:], in0=sq[:], in1=Sh[:], op=ALU.add), reads=["sq", "Sh"], writes=["h32"])
            P.op("pool", lambda e: e.tensor_copy(out=hb[:], in_=h32[:]), reads=["h32"], writes=["hb"])
            for kc in range(16):
                P.op("pe", lambda e, kc=kc: e.transpose(out=pT[:, kc, :], in_=hb[:, kc * 128:(kc + 1) * 128], identity=ident[:]),
                     reads=["hb", "ident"], writes=["pT"])
            P.op("act", lambda e: e.copy(out=hT[:], in_=pT[:]), writes=["hT", "pT"])
            P.dma(hfTd[:, :, t * 128:(t + 1) * 128], hT[:], reads=["hT"], is_output=True)
            for q4 in range(4):
                fb = q4 % 2
                for u in range(4):
                    kc = q4 * 4 + u
                    P.op("pe", lambda e, fb=fb, u=u, kc=kc: e.transpose(out=pF[fb][:, u, :], in_=h32[:, kc * 128:(kc + 1) * 128], identity=ident32[:]),
                         reads=["h32", "ident32"], writes=[("pF", fb)])
                P.op("act", lambda e, fb=fb, q4=q4: e.copy(out=hT32[:, q4 * 4:q4 * 4 + 4, :], in_=pF[fb][:]), writes=[("hT32", q4), ("pF", fb)])
            for kc in range(16):
                P.op("pe", lambda e, kc=kc: e.matmul(pL[:, 0:32], lhsT=hT32[:, kc, :], rhs=rw[:, kc, :], start=(kc == 0), stop=(kc == 15)),
                     reads=[("hT32", kc // 4), "rw"], writes=["pL"])
            P.op("dve", lambda e: e.tensor_tensor(out=lg[:], in0=pL[:, 0:32], in1=rb[:], op=ALU.add), reads=["rb"], writes=["lg", "pL"])
            P.op("dve", lambda e: e.max(out=mx[:], in_=lg[:]), reads=["lg"], writes=["mx"])
            P.op("dve", lambda e: e.tensor_scalar(out=mk[:], in0=lg[:], scalar1=mx[:, 3:4], scalar2=None, op0=ALU.is_ge), reads=["lg", "mx"], writes=["mk"])
            P.op("dve", lambda e: e.tensor_scalar(out=ex[:], in0=lg[:], scalar1=mx[:, 0:1], scalar2=None, op0=ALU.subtract), reads=["lg", "mx"], writes=["ex"])
            P.op("act", lambda e: e.activation(out=ex[:], in_=ex[:], func=AF.Exp), writes=["ex"])
            P.op("dve", lambda e: e.tensor_tensor(out=ex[:], in0=ex[:], in1=mk[:], op=ALU.mult), reads=["mk"], writes=["ex"])
            P.op("dve", lambda e: e.reduce_sum(out=s1[:, 0:1], in_=ex[:], axis=AX.X), reads=["ex"], writes=["s1"])
            P.op("dve", lambda e: e.reciprocal(out=s1[:, 1:2], in_=s1[:, 0:1]), writes=["s1"])
            P.op("dve", lambda e: e.tensor_scalar(out=lg[:], in0=ex[:], scalar1=s1[:, 1:2], scalar2=None, op0=ALU.mult), reads=["ex", "s1"], writes=["lg"])
            P.dma(Gd[t * 128:(t + 1) * 128, :], lg[:], reads=["lg"], is_output=True)
    return C.finish()


def run_k2b(ycat, x2d, w_out_l, mod_l, gffn_l, router_w_l, router_b_l):
    nc = build_k2b(8)
    ident = np.eye(128, dtype=np.float32)
    in_maps = []
    for i in range(NCORES):
        sl = slice(1024 * i, 1024 * (i + 1))
        in_maps.append({"y": np.ascontiguousarray(ycat[sl]), "x": np.ascontiguousarray(x2d[sl]), "w": w_out_l,
                        "gateA": bc(mod_l[2 * D:3 * D]), "modA": bc(mod_l[4 * D:5 * D]), "modS": bc(mod_l[3 * D:4 * D]),
                        "gN": bc(gffn_l), "rw": router_w_l, "rb": bc(router_b_l),
                        "ident": ident.astype(ml_dtypes.bfloat16), "ident32": ident})
    res = run(nc, in_maps)
    x1 = np.concatenate([r["x1"] for r in res], axis=0)
    hfT = np.concatenate([r["hfT"] for r in res], axis=2)
    G = np.concatenate([r["G"] for r in res], axis=0)
    return x1, hfT, G


def build_k3(NTG=16, NE=4):
    C = Ctx()
    P = C.P
    TOK = NTG * 512
    hTd = C.din("hT", [128, 16, TOK], BF16)
    Gbd = C.din("Gb", [NE, 128, TOK])
    wgud = C.din("wgu", [NE, D, 2 * D])
    wdnd = C.din("wdn", [NE, D, D])
    bgud = C.din("bgu", [128, NE, 32])
    bdnd = C.din("bdn", [128, NE, 16])
    outd = C.dout("outT", [128, 16, TOK])
    sgu = C.nc.dram_tensor("sgu", [NE, 32, 128, 2048], BF16).ap()
    sdn = C.nc.dram_tensor("sdn", [NE, 16, 128, 2048], BF16).ap()

    hT = C.sb("hT", [128, 16, 512], BF16)
    Gb = [C.sb("Gb%d" % i, [128, NE, 512]) for i in range(2)]
    bgu = C.sb("bgu", [128, NE, 32])
    bdn = C.sb("bdn", [128, NE, 16])
    actT = C.sb("actT", [128, NE, 16, 512], BF16)
    NST = 3
    wst = [C.sb("wst%d" % i, [128, 16, 128]) for i in range(NST)]
    NWB = 3
    wg = [C.sb("wg%d" % i, [128, 16, 128], BF16) for i in range(NWB)]
    wl = [C.sb("wl%d" % i, [128, 16, 128], BF16) for i in range(NWB)]
    gtt = [C.sb("gtt%d" % i, [128, 512]) for i in range(2)]
    stt = [C.sb("stt%d" % i, [128, 512]) for i in range(2)]
    ltt = [C.sb("ltt%d" % i, [128, 512]) for i in range(2)]
    ot = [C.sb("ot%d" % i, [128, 512]) for i in range(2)]
    pg = [C.ps("pg%d" % i, [128, 512]) for i in range(2)]
    pl = [C.ps("pl%d" % i, [128, 512]) for i in range(2)]
    po = [C.ps("po%d" % i, [128, 512]) for i in range(2)]
    P.dma(bgu[:], bgud, writes=["bgu"])
    P.dma(bdn[:], bdnd, writes=["bdn"])

    blocks = []
    for e_ in range(NE):
        wv = wgud[e_].rearrange("(kc p) n -> p kc n", p=128)
        for cb in range(32):
            blocks.append((wv[:, :, cb * 128:(cb + 1) * 128], sgu[e_, cb], ("sgu", e_, cb)))
        wv2 = wdnd[e_].rearrange("(c p) n -> p c n", p=128)
        for m in range(16):
            blocks.append((wv2[:, :, m * 128:(m + 1) * 128], sdn[e_, m], ("sdn", e_, m)))
    NBk = len(blocks)
    for n in range(NBk + 2):
        if n < NBk:
            s_ = n % NST
            P.dma(wst[s_][:], blocks[n][0], writes=[("wst", s_)])
        if n >= 2:
            k = n - 2
            s_ = k % NST
            b = k % NWB
            if k % 2 == 0:
                P.op("act", lambda e, s_=s_, b=b: e.copy(out=wg[b][:], in_=wst[s_][:]), reads=[("wst", s_)], writes=[("wg", b)])
            else:
                P.op("pool", lambda e, s_=s_, b=b: e.tensor_copy(out=wg[b][:], in_=wst[s_][:]), reads=[("wst", s_)], writes=[("wg", b)])
            P.dma(blocks[k][1].rearrange("p (a j) -> p a j", j=128), wg[b][:], reads=[("wg", b)], writes=[blocks[k][2]])

    ia = 0
    ib = 0
    for tg in range(NTG):
        t0 = tg * 512
        gb = Gb[tg % 2]
        gtag = ("Gb", tg % 2)
        P.dma(hT[:], hTd[:, :, t0:t0 + 512], writes=["hT"])
        for e_ in range(NE):
            P.dma(gb[:, e_, :], Gbd[e_][:, t0:t0 + 512], writes=[gtag])
        for e_ in range(NE):
            for c in range(16):
                b = ia % NWB
                pb = ia % 2
                ia += 1
                P.dma(wg[b][:], sgu[e_, c].rearrange("p (a j) -> p a j", j=128), reads=[("sgu", e_, c)], writes=[("wg", b)])
                P.dma(wl[b][:], sgu[e_, 16 + c].rearrange("p (a j) -> p a j", j=128), reads=[("sgu", e_, 16 + c)], writes=[("wl", b)])
                for kc in range(16):
                    P.op("pe", lambda e, b=b, pb=pb, kc=kc: e.matmul(pg[pb][:], lhsT=wg[b][:, kc, :], rhs=hT[:, kc, :], start=(kc == 0), stop=(kc == 15)),
                         reads=[("wg", b), "hT"], writes=[("pg", pb)])
                for kc in range(16):
                    P.op("pe", lambda e, b=b, pb=pb, kc=kc: e.matmul(pl[pb][:], lhsT=wl[b][:, kc, :], rhs=hT[:, kc, :], start=(kc == 0), stop=(kc == 15)),
                         reads=[("wl", b), "hT"], writes=[("pl", pb)])
                b = pb
                P.op("dve", lambda e, b=b, e_=e_, c=c: e.tensor_scalar(out=gtt[b][:], in0=pg[b][:], scalar1=bgu[:, e_, c:c + 1], scalar2=7.0,
                                                                       op0=ALU.add, op1=ALU.min),
                     reads=["bgu"], writes=[("gtt", b), ("pg", b)])
                P.op("act", lambda e, b=b: e.activation(out=stt[b][:], in_=gtt[b][:], func=AF.Sigmoid, scale=1.702),
                     reads=[("gtt", b)], writes=[("stt", b)])
                P.op("dve", lambda e, b=b, e_=e_, c=c: e.tensor_scalar(out=ltt[b][:], in0=pl[b][:], scalar1=bgu[:, e_, 16 + c:17 + c], scalar2=7.0,
                                                                       op0=ALU.add, op1=ALU.min),
                     reads=["bgu"], writes=[("ltt", b), ("pl", b)])
                P.op("dve", lambda e, b=b: e.tensor_scalar(out=ltt[b][:], in0=ltt[b][:], scalar1=-7.0, scalar2=1.0, op0=ALU.max, op1=ALU.add),
                     writes=[("ltt", b)])
                P.op("pool", lambda e, b=b: e.tensor_tensor(out=gtt[b][:], in0=gtt[b][:], in1=stt[b][:], op=ALU.mult),
                     reads=[("stt", b)], writes=[("gtt", b)])
                P.op("pool", lambda e, b=b: e.tensor_tensor(out=gtt[b][:], in0=gtt[b][:], in1=ltt[b][:], op=ALU.mult),
                     reads=[("ltt", b)], writes=[("gtt", b)])
                P.op("dve", lambda e, b=b, e_=e_, c=c, gb=gb: e.tensor_tensor(out=actT[:, e_, c, :], in0=gtt[b][:], in1=gb[:, e_, :], op=ALU.mult),
                     reads=[("gtt", b), gtag], writes=[("actT", e_)])
        for m in range(16):
            pb = m % 2
            for e_ in range(NE):
                b = ib % NWB
                ib += 1
                wt_, wtag = (wg[b], ("wg", b)) if ib % 2 else (wl[b], ("wl", b))
                P.dma(wt_[:], sdn[e_, m].rearrange("p (a j) -> p a j", j=128), reads=[("sdn", e_, m)], writes=[wtag])
                for c in range(16):
                    P.op("pe", lambda e, wt_=wt_, c=c, e_=e_, pb=pb: e.matmul(po[pb][:], lhsT=wt_[:, c, :], rhs=actT[:, e_, c, :],
                                                                              start=(e_ == 0 and c == 0), stop=(e_ == NE - 1 and c == 15)),
                         reads=[wtag, ("actT", e_)], writes=[("po", pb)])
            P.op("dve", lambda e, pb=pb, m=m, gb=gb: e.scalar_tensor_tensor(out=ot[pb][:], in0=gb[:, 0, :], scalar=bdn[:, 0, m:m + 1], in1=po[pb][:],
                                                                            op0=ALU.mult, op1=ALU.add),
                 reads=[gtag, "bdn"], writes=[("ot", pb), ("po", pb)])
            for e_ in range(1, NE):
                P.op("dve", lambda e, pb=pb, m=m, e_=e_, gb=gb: e.scalar_tensor_tensor(out=ot[pb][:], in0=gb[:, e_, :], scalar=bdn[:, e_, m:m + 1],
                                                                                       in1=ot[pb][:], op0=ALU.mult, op1=ALU.add),
                     reads=[gtag, "bdn"], writes=[("ot", pb)])
            P.dma(outd[:, m, t0:t0 + 512], ot[pb][:], reads=[("ot", pb)], is_output=True, q="act")
    return C.finish()


def run_k3(hfT, G, wgu_l, bgu_l, wdn_l, bdn_l):
    nc = build_k3(16, 4)
    in_maps = []
    for i in range(NCORES):
        es = slice(4 * i, 4 * i + 4)
        Gb = np.ascontiguousarray(np.broadcast_to(G[:, es].T[:, None, :], (4, 128, S)))
        in_maps.append({"hT": hfT, "Gb": Gb, "wgu": wgu_l[es], "wdn": wdn_l[es],
                        "bgu": np.ascontiguousarray(bgu_l[es].reshape(4, 32, 128).transpose(2, 0, 1)),
                        "bdn": np.ascontiguousarray(bdn_l[es].reshape(4, 16, 128).transpose(2, 0, 1))})
    res = run(nc, in_maps)
    return [r["outT"] for r in res]


def build_k4(NT=8, final=False):
    C = Ctx()
    P = C.P
    x1d = C.din("x1", [NT * 128, D])
    pd = C.din("parts", [NCORES, NT * 128, D])
    gFd = C.din("gateF", [128, D])
    gfd = C.din("gfin", [128, D])
    od = C.dout("out", [NT * 128, D])
    gF = C.sb("gF", [128, D])
    gfin = C.sb("gfin", [128, D])
    P.dma(gF[:], gFd, writes=["gF"])
    P.dma(gfin[:], gfd, writes=["gfin"])
    xt = [C.sb("xt%d" % i, [128, D]) for i in range(2)]
    acc = [C.sb("acc%d" % i, [128, D]) for i in range(2)]
    pt = [C.sb("pt%d" % i, [128, D]) for i in range(3)]
    sq = C.sb("sq", [128, D])
    st = C.sb("st", [128, 2])
    npt = 0
    for t in range(NT):
        k = t % 2
        P.dma(xt[k][:], x1d[t * 128:(t + 1) * 128, :], writes=[("xt", k)])
        P.dma(acc[k][:], pd[0, t * 128:(t + 1) * 128, :], writes=[("acc", k)])
        for c in range(1, NCORES):
            b = npt % 3
            npt += 1
            P.dma(pt[b][:], pd[c, t * 128:(t + 1) * 128, :], writes=[("pt", b)])
            P.op("dve" if c % 2 else "pool", lambda e, k=k, b=b: e.tensor_tensor(out=acc[k][:], in0=acc[k][:], in1=pt[b][:], op=ALU.add),
                 reads=[("pt", b)], writes=[("acc", k)])
        P.op("dve", lambda e, k=k: e.tensor_tensor(out=acc[k][:], in0=acc[k][:], in1=gF[:], op=ALU.mult), reads=["gF"], writes=[("acc", k)])
        P.op("pool", lambda e, k=k: e.tensor_tensor(out=acc[k][:], in0=acc[k][:], in1=xt[k][:], op=ALU.add), reads=[("xt", k)], writes=[("acc", k)])
        if final:
            P.op("act", lambda e, k=k: e.activation(out=sq[:], in_=acc[k][:], func=AF.Square), reads=[("acc", k)], writes=["sq"])
            P.op("dve", lambda e: e.reduce_sum(out=st[:, 0:1], in_=sq[:], axis=AX.X), reads=["sq"], writes=["st"])
            P.op("act", lambda e: e.activation(out=st[:, 1:2], in_=st[:, 0:1], func=AF.Sqrt, scale=1.0 / D, bias=EPS), writes=["st"])
            P.op("dve", lambda e: e.reciprocal(out=st[:, 0:1], in_=st[:, 1:2]), writes=["st"])
            P.op("dve", lambda e, k=k: e.scalar_tensor_tensor(out=acc[k][:], in0=acc[k][:], scalar=st[:, 0:1], in1=gfin[:], op0=ALU.mult, op1=ALU.mult),
                 reads=["st", "gfin"], writes=[("acc", k)])
        P.dma(od[t * 128:(t + 1) * 128, :], acc[k][:], reads=[("acc", k)], is_output=True)
    return C.finish()


def run_k4(x1, parts, gate_f, gfin, final):
    nc = build_k4(8, final)
    in_maps = []
    for i in range(NCORES):
        sl = slice(1024 * i, 1024 * (i + 1))
        pp = np.stack([np.ascontiguousarray(p[:, :, sl].transpose(2, 1, 0)).reshape(1024, D) for p in parts])
        in_maps.append({"x1": np.ascontiguousarray(x1[sl]), "parts": pp, "gateF": bc(gate_f), "gfin": bc(gfin)})
    res = run(nc, in_maps)
    return np.concatenate([r["out"] for r in res], axis=0)


LAYER_KEYS = ("conv_dw_w", "conv_dw_b", "conv_ln_g", "conv_ln_b", "nsa_pe_k", "nsa_pe_v", "nsa_cmp_k_w1", "nsa_cmp_k_w2",
              "nsa_cmp_v_w1", "nsa_cmp_v_w2", "ret_gn_g", "ret_gn_b")


def kernel(x, c, ada_w, ada_b, norm_mix_g, w_in, conv_dw_w, conv_dw_b, conv_ln_g, conv_ln_b,
           nsa_pe_k, nsa_pe_v, nsa_cmp_k_w1, nsa_cmp_k_w2, nsa_cmp_v_w1, nsa_cmp_v_w2,
           ret_gn_g, ret_gn_b, w_out, norm_ffn_g, router_w, router_b,
           moe_w_gate_up, moe_b_gate_up, moe_w_down, moe_b_down, final_norm_g):
    loc = locals()
    f = lambda a: np.asarray(a, dtype=np.float32)
    xs = f(x)[0]
    mod = run_k0(f(c), f(ada_w), f(ada_b))
    tabs = rot_tables()
    for l in range(DEPTH):
        lw = {k: f(loc[k][l]) for k in LAYER_KEYS}
        p32, p16 = run_k1(xs, f(w_in[l]), mod[l], f(norm_mix_g[l]), tabs)
        ycat = run_k2a(p32, p16, lw)
        del p32, p16
        x1, hfT, G = run_k2b(ycat, xs, f(w_out[l]), mod[l], f(norm_ffn_g[l]), f(router_w[l]), f(router_b[l]))
        parts = run_k3(hfT, G, np.asarray(moe_w_gate_up[l]), f(moe_b_gate_up[l]), np.asarray(moe_w_down[l]), f(moe_b_down[l]))
        xs = run_k4(x1, parts, mod[l][5 * D:6 * D], f(final_norm_g), l == DEPTH - 1)
        del parts
    return xs[None].astype(np.float32)
```

```python
from contextlib import ExitStack

import numpy as np
import ml_dtypes
import concourse.bass as bass
import concourse.mybir as mybir
from concourse.bass_utils import run_bass_kernel_spmd

F32 = mybir.dt.float32
BF16 = mybir.dt.bfloat16
AF = mybir.ActivationFunctionType
ALU = mybir.AluOpType
AX = mybir.AxisListType

NCORES = 8
D = 2048
S = 8192
DEPTH = 2
INW = 5656
EPS = 1e-6

ENGS = ("pe", "act", "dve", "pool", "sp")
N_DMA_SEMS = 8
DMA_QS = ("sp", "act", "pool")


class Prog:
    def __init__(self, nc):
        self.nc = nc
        self.streams = {e: [] for e in ENGS}
        self.count = {e: 0 for e in ENGS}
        self.waited = {e: {} for e in ENGS}
        self.last_w = {}
        self.readers = {}
        self.dma_n = [0] * (N_DMA_SEMS * len(DMA_QS))
        self.dma_rr = [0] * len(DMA_QS)
        self.out_tokens = []

    def _deps(self, reads, writes):
        deps = []
        for b in reads:
            t = self.last_w.get(b)
            if t is not None:
                deps.append(t)
        for b in writes:
            t = self.last_w.get(b)
            if t is not None:
                deps.append(t)
            deps.extend(self.readers.get(b, {}).values())
        return deps

    def _wait(self, eng, tok):
        key, val = tok
        if key == eng == "pe":
            return
        w = self.waited[eng]
        if w.get(key, 0) >= val:
            return
        w[key] = val
        self.streams[eng].append(("wait", key, val))

    def _commit(self, tok, reads, writes):
        for b in writes:
            self.last_w[b] = tok
            self.readers[b] = {}
        for b in reads:
            if b in writes:
                continue
            self.readers.setdefault(b, {})[tok[0]] = tok

    def op(self, eng, fn, reads=(), writes=()):
        for t in self._deps(reads, writes):
            self._wait(eng, t)
        self.count[eng] += 1
        tok = (eng, self.count[eng])
        self.streams[eng].append(("op", fn))
        self._commit(tok, reads, writes)
        return tok

    def dma(self, out, in_, reads=(), writes=(), q="sp", is_output=False, **kw):
        for t in self._deps(reads, writes):
            self._wait(q, t)
        qi = DMA_QS.index(q)
        s = qi * N_DMA_SEMS + self.dma_rr[qi]
        self.dma_rr[qi] = (self.dma_rr[qi] + 1) % N_DMA_SEMS
        key = ("dma", s)
        if self.dma_n[s] > 0:
            self._wait(q, (key, 16 * self.dma_n[s]))
        self.dma_n[s] += 1
        tok = (key, 16 * self.dma_n[s])
        self.streams[q].append(("dma", out, in_, s, kw))
        self._commit(tok, reads, writes)
        if is_output:
            self.out_tokens.append(tok)
        return tok

    def barrier(self):
        toks = [(e, self.count[e]) for e in ENGS if self.count[e] > 0]
        toks += [(("dma", k), 16 * self.dma_n[k]) for k in range(len(self.dma_n)) if self.dma_n[k] > 0]
        for e in ENGS:
            for t in toks:
                self._wait(e, t)

    def emit(self):
        nc = self.nc
        with ExitStack() as st:
            esem = {e: st.enter_context(nc.semaphore("s_" + e)) for e in ENGS}
            dsem = [st.enter_context(nc.semaphore("d%d" % i)) for i in range(N_DMA_SEMS * len(DMA_QS))]
            for t in self.out_tokens:
                self._wait("sp", t)
            block = st.enter_context(nc.Block())

            def semof(key):
                return dsem[key[1]] if isinstance(key, tuple) else esem[key]

            def replay(ename):
                def f(e):
                    for it in self.streams[ename]:
                        if it[0] == "wait":
                            e.wait_ge(semof(it[1]), it[2])
                        elif it[0] == "op":
                            it[1](e).then_inc(esem[ename], 1)
                        else:
                            _, out, in_, s, kw = it
                            e.dma_start(out=out, in_=in_, **kw).then_inc(dsem[s], 16)
                return f

            block.tensor(replay("pe"))
            block.scalar(replay("act"))
            block.vector(replay("dve"))
            block.gpsimd(replay("pool"))
            block.sync(replay("sp"))


class Ctx:
    def __init__(self):
        self.nc = bass.Bass("TRN2", target_bir_lowering=False)
        self.st = ExitStack()
        self.P = Prog(self.nc)

    def din(self, name, shape, dt=F32):
        return self.nc.dram_tensor(name, list(shape), dt, kind="ExternalInput").ap()

    def sbn(self, name, shape, dt=F32):
        return self.sb("s_" + name, shape, dt)

    def dout(self, name, shape, dt=F32):
        return self.nc.dram_tensor(name, list(shape), dt, kind="ExternalOutput").ap()

    def sb(self, name, shape, dt=F32):
        return self.st.enter_context(self.nc.sbuf_tensor("sb_" + name, list(shape), dt))

    def ps(self, name, shape, dt=F32):
        return self.st.enter_context(self.nc.psum_tensor("ps_" + name, list(shape), dt))

    def finish(self):
        self.P.emit()
        self.st.close()
        return self.nc


def run(nc, in_maps):
    res = run_bass_kernel_spmd(nc, in_maps, core_ids=list(range(NCORES)))
    return res.results


def build_k0():
    C = Ctx()
    P = C.P
    c_in = C.din("c", [128, 16])
    w = C.din("w", [DEPTH, D, 1536])
    b = C.din("b", [DEPTH, 1536])
    out = C.dout("out", [DEPTH, 1536])
    craw = C.sb("craw", [128, 16])
    sc = C.sb("sc", [128, 16])
    wt = [C.sb("wt%d" % i, [128, 16, 512]) for i in range(2)]
    bt = [C.sb("bt%d" % i, [1, 512]) for i in range(2)]
    ot = [C.sb("ot%d" % i, [1, 512]) for i in range(2)]
    pp = [C.ps("pp%d" % i, [1, 512]) for i in range(2)]
    P.dma(craw[:], c_in, writes=["craw"])
    P.op("act", lambda e: e.activation(out=sc[:], in_=craw[:], func=AF.Silu), reads=["craw"], writes=["sc"])
    it = 0
    for l in range(DEPTH):
        wl = w[l].rearrange("(kc p) n -> p kc n", p=128)
        for n in range(3):
            k = it % 2
            it += 1
            P.dma(wt[k][:], wl[:, :, n * 512:(n + 1) * 512], writes=[("wt", k)])
            P.dma(bt[k][:], b[l:l + 1, n * 512:(n + 1) * 512], writes=[("bt", k)])
            for kc in range(16):
                P.op("pe", lambda e, k=k, kc=kc: e.matmul(pp[k][:], lhsT=sc[:, kc:kc + 1], rhs=wt[k][:, kc, :],
                                                          start=(kc == 0), stop=(kc == 15)),
                     reads=["sc", ("wt", k)], writes=[("pp", k)])
            P.op("dve", lambda e, k=k: e.tensor_tensor(out=ot[k][:], in0=pp[k][:], in1=bt[k][:], op=ALU.add),
                 reads=[("pp", k), ("bt", k)], writes=[("ot", k)])
            P.dma(out[l:l + 1, n * 512:(n + 1) * 512], ot[k][:], reads=[("ot", k)], is_output=True)
    return C.finish()


def run_k0(c, ada_w, ada_b):
    nc = build_k0()
    cin = np.ascontiguousarray(c.reshape(16, 128).T)
    in_maps = []
    for i in range(NCORES):
        sl = slice(1536 * i, 1536 * (i + 1))
        in_maps.append({"c": cin, "w": np.ascontiguousarray(ada_w[:, :, sl]),
                        "b": np.ascontiguousarray(ada_b[:, sl])})
    res = run(nc, in_maps)
    return np.concatenate([r["out"] for r in res], axis=1)


ORIG_SPLITS = (512, 512, 1024, 256, 256, 256, 256, 256, 256, 24, 512, 512, 512, 512)
ORIG_OFF = np.concatenate([[0], np.cumsum(ORIG_SPLITS)])
NEW_ORDER = (0, 1, 2, 3, 4, 5, 6, 7, 8, 10, 11, 12, 13, 9)
COL_PERM = np.concatenate([np.arange(ORIG_OFF[j], ORIG_OFF[j + 1]) for j in NEW_ORDER])
O_CV, O_CG, O_NQ, O_KC, O_VC, O_KS, O_VS, O_KW, O_VW, O_RQ, O_RK, O_RV, O_RG, O_GT = (
    0, 512, 1024, 2048, 2304, 2560, 2816, 3072, 3328, 3584, 4096, 4608, 5120, 5632)
CH_KIND = ["plain", "plain", "nsa4", "nsa4", "nsa2", "nsa2", "nsa2", "retq", "retk", "plain", "plain", "gate"]


def rot_tables():
    pos = np.arange(S, dtype=np.float32)
    invn = np.power(np.float32(500000.0), -np.arange(16, dtype=np.float32) * np.float32(2.0) / np.float32(32))
    angn = pos[:, None] * invn[None, :].astype(np.float32)
    invr = np.power(np.float32(10000.0), -np.arange(64, dtype=np.float32) * np.float32(2.0) / np.float32(128))
    angr = pos[:, None] * invr[None, :].astype(np.float32)
    return (np.cos(angn).astype(np.float32), np.sin(angn).astype(np.float32),
            np.cos(angr).astype(np.float32), np.sin(angr).astype(np.float32))


def ret_consts():
    lg = np.log(1.0 - 2.0 ** (-5.0 - np.arange(4, dtype=np.float64)))
    return lg


def ret_ztab():
    lg = ret_consts()
    i = (np.arange(S) % 128).astype(np.float64)
    zq = np.exp(lg[None, :] * (i[:, None] - 127.0))
    zk = np.exp(lg[None, :] * (127.0 - i[:, None])) * (128.0 ** -0.5)
    return np.concatenate([zq, zk], axis=1).astype(np.float32)


def emit_norm_T(C, x_src, A, Sh, hT, ident, ntiles, pfx=""):
    P = C.P
    xt = [C.sb(pfx + "nx%d" % i, [128, D]) for i in range(2)]
    sq = C.sb(pfx + "nsq", [128, D])
    hb = [C.sb(pfx + "nhb%d" % i, [128, D], BF16) for i in range(2)]
    st = [C.sb(pfx + "nst%d" % i, [128, 2]) for i in range(2)]
    pT = C.ps(pfx + "npT", [128, 16, 128], BF16)
    for t in range(ntiles):
        k = t % 2
        P.dma(xt[k][:], x_src(t), writes=[(pfx + "nx", k)])
        P.op("act", lambda e, k=k: e.activation(out=sq[:], in_=xt[k][:], func=AF.Square),
             reads=[(pfx + "nx", k)], writes=[pfx + "nsq"])
        P.op("dve", lambda e, k=k: e.reduce_sum(out=st[k][:, 0:1], in_=sq[:], axis=AX.X),
             reads=[pfx + "nsq"], writes=[(pfx + "nst", k)])
        P.op("act", lambda e, k=k: e.activation(out=st[k][:, 1:2], in_=st[k][:, 0:1], func=AF.Sqrt,
                                                scale=1.0 / D, bias=EPS),
             reads=[(pfx + "nst", k)], writes=[(pfx + "nst", k)])
        P.op("dve", lambda e, k=k: e.reciprocal(out=st[k][:, 0:1], in_=st[k][:, 1:2]),
             reads=[(pfx + "nst", k)], writes=[(pfx + "nst", k)])
        P.op("dve", lambda e, k=k: e.scalar_tensor_tensor(out=sq[:], in0=xt[k][:], scalar=st[k][:, 0:1], in1=A[:],
                                                          op0=ALU.mult, op1=ALU.mult),
             reads=[(pfx + "nx", k), (pfx + "nst", k), "A"], writes=[pfx + "nsq"])
        P.op("dve", lambda e, k=k: e.tensor_tensor(out=hb[k][:], in0=sq[:], in1=Sh[:], op=ALU.add),
             reads=[pfx + "nsq", "Sh"], writes=[(pfx + "nhb", k)])
        for kc in range(16):
            P.op("pe", lambda e, k=k, kc=kc: e.transpose(out=pT[:, kc, :], in_=hb[k][:, kc * 128:(kc + 1) * 128],
                                                         identity=ident[:]),
                 reads=[(pfx + "nhb", k), "ident"], writes=[pfx + "npT"])
        P.op("act", lambda e, t=t: e.copy(out=hT[t][:], in_=pT[:]), reads=[pfx + "npT"], writes=[("hT", t)])


def build_k1(NT=8):
    C = Ctx()
    P = C.P
    x = C.din("x", [NT * 128, D])
    w = C.din("w", [D, INW])
    modA = C.din("modA", [128, D])
    modS = C.din("modS", [128, D])
    gN = C.din("gN", [128, D])
    identd = C.din("ident", [128, 128], BF16)
    tabn = C.din("tabn", [2, NT * 128, 16])
    tabr = C.din("tabr", [2, NT * 128, 64])
    ztd = C.din("zt", [NT * 128, 8])
    o32 = C.dout("o32", [NT * 128, INW])
    o16 = C.dout("o16", [NT * 128, INW], BF16)

    ident = C.sb("ident", [128, 128], BF16)
    A = C.sb("A", [128, D])
    Sh = C.sb("Sh", [128, D])
    tn = C.sb("tn", [128, 2, NT, 16])
    tr = C.sb("tr", [128, 2, NT, 64])
    zt = C.sb("zt", [128, NT, 8])
    hT = [C.sb("hT%d" % t, [128, 16, 128], BF16) for t in range(NT)]
    P.dma(ident[:], identd, writes=["ident"])
    for ci in range(2):
        P.dma(tn[:, ci], tabn[ci].rearrange("(t p) f -> p t f", p=128), writes=["tn"])
    P.dma(zt[:], ztd.rearrange("(t p) f -> p t f", p=128), writes=["zt"])
    for ci in range(2):
        P.dma(tr[:, ci], tabr[ci].rearrange("(t p) f -> p t f", p=128), writes=["tr"])
    gtmp = C.sb("gtmp", [128, D])
    P.dma(A[:], modA, writes=["A"])
    P.dma(gtmp[:], gN, writes=["gtmp"])
    P.dma(Sh[:], modS, writes=["Sh"])
    P.op("dve", lambda e: e.scalar_tensor_tensor(out=A[:], in0=A[:], scalar=1.0, in1=gtmp[:], op0=ALU.add, op1=ALU.mult),
         reads=["gtmp"], writes=["A"])
    emit_norm_T(C, lambda t: x[t * 128:(t + 1) * 128, :], A, Sh, hT, ident, NT)

    wst = [C.sb("wst%d" % i, [128, 8, 512]) for i in range(2)]
    wbf = [C.sb("wbf%d" % i, [128, 16, 512], BF16) for i in range(2)]
    ob32 = [C.sb("ob32_%d" % i, [128, 512]) for i in range(2)]
    ob16 = [C.sb("ob16_%d" % i, [128, 512], BF16) for i in range(2)]
    tmp = [C.sb("rt%d" % i, [128, 4, 64]) for i in range(4)]
    pY = [C.ps("pY%d" % i, [128, 512]) for i in range(2)]
    wv = w.rearrange("(kc p) n -> p kc n", p=128)
    nst = 0
    nev = 0
    for cc in range(12):
        kind = CH_KIND[cc]
        ncol = 512 if kind != "gate" else 24
        c0 = cc * 512
        wb = cc % 2
        for half in range(2):
            sbuf = nst % 2
            nst += 1
            P.dma(wst[sbuf][:, :, 0:ncol], wv[:, half * 8:(half + 1) * 8, c0:c0 + ncol], writes=[("wst", sbuf)])
            P.op("pool", lambda e, sbuf=sbuf, wb=wb, half=half, ncol=ncol: e.tensor_copy(
                out=wbf[wb][:, half * 8:(half + 1) * 8, 0:ncol], in_=wst[sbuf][:, :, 0:ncol]),
                reads=[("wst", sbuf)], writes=[("wbf", wb, half)])
        for t in range(NT):
            pb = nev % 2
            nev += 1
            for kc in range(16):
                P.op("pe", lambda e, pb=pb, t=t, kc=kc, wb=wb, ncol=ncol: e.matmul(
                    pY[pb][:, 0:ncol], lhsT=hT[t][:, kc, :], rhs=wbf[wb][:, kc, 0:ncol], start=(kc == 0), stop=(kc == 15)),
                    reads=[("hT", t), ("wbf", wb, kc // 8)], writes=[("pY", pb)])
            ps = pY[pb]
            o3 = ob32[pb]
            o1 = ob16[pb]
            R = []
            PSX = [("pY", pb)]
            W32 = [("ob32", pb)]
            if kind in ("plain", "gate"):
                P.op("act", lambda e, ps=ps, o3=o3, ncol=ncol: e.copy(out=o3[:, 0:ncol], in_=ps[:, 0:ncol]), reads=R, writes=W32 + PSX)
            elif kind in ("nsa4", "nsa2"):
                nh = 4 if kind == "nsa4" else 2
                P.op("act", lambda e, ps=ps, o3=o3: e.copy(out=o3[:], in_=ps[:]), reads=R, writes=W32 + PSX)
                psv = ps[:].rearrange("p (h d) -> p h d", d=128)
                o3v = o3[:].rearrange("p (h d) -> p h d", d=128)
                cs = tn[:, 0, t:t + 1, :].to_broadcast([128, nh, 16])
                sn = tn[:, 1, t:t + 1, :].to_broadcast([128, nh, 16])
                x1 = psv[:, 0:nh, 0:16]
                x2 = psv[:, 0:nh, 16:32]
                tv = [tm[:, 0:nh, 0:16] for tm in tmp]
                for (dst, a, tb) in ((tv[0], x1, cs), (tv[1], x2, sn), (tv[2], x2, cs), (tv[3], x1, sn)):
                    P.op("dve", lambda e, dst=dst, a=a, tb=tb: e.tensor_tensor(out=dst, in0=a, in1=tb, op=ALU.mult),
                         reads=R + ["tn"], writes=["rtmp"] + PSX)
                P.op("dve", lambda e, o3v=o3v, tv=tv, nh=nh: e.tensor_tensor(out=o3v[:, 0:nh, 0:16], in0=tv[0], in1=tv[1], op=ALU.subtract),
                     reads=["rtmp"], writes=W32)
                P.op("dve", lambda e, o3v=o3v, tv=tv, nh=nh: e.tensor_tensor(out=o3v[:, 0:nh, 16:32], in0=tv[2], in1=tv[3], op=ALU.add),
                     reads=["rtmp"], writes=W32)
            else:
                ci = 0
                zo = 0 if kind == "retq" else 4
                psv = ps[:].rearrange("p (h d) -> p h d", d=128)
                o3v = o3[:].rearrange("p (h d) -> p h d", d=128)
                cs = tr[:, ci, t:t + 1, :].to_broadcast([128, 4, 64])
                sn = tr[:, ci + 1, t:t + 1, :].to_broadcast([128, 4, 64])
                x1 = psv[:, :, 0:64]
                x2 = psv[:, :, 64:128]
                tv = [tm[:] for tm in tmp]
                for (dst, a, tb) in ((tv[0], x1, cs), (tv[1], x2, sn), (tv[2], x2, cs), (tv[3], x1, sn)):
                    P.op("dve", lambda e, dst=dst, a=a, tb=tb: e.tensor_tensor(out=dst, in0=a, in1=tb, op=ALU.mult),
                         reads=R + ["tr"], writes=["rtmp"] + PSX)
                P.op("dve", lambda e, o3v=o3v, tv=tv: e.tensor_tensor(out=o3v[:, :, 0:64], in0=tv[0], in1=tv[1], op=ALU.subtract),
                     reads=["rtmp"], writes=W32)
                P.op("dve", lambda e, o3v=o3v, tv=tv: e.tensor_tensor(out=o3v[:, :, 64:128], in0=tv[2], in1=tv[3], op=ALU.add),
                     reads=["rtmp"], writes=W32)
                zb = zt[:, t, zo:zo + 4].unsqueeze(2).to_broadcast([128, 4, 128])
                P.op("dve", lambda e, o3v=o3v, zb=zb: e.tensor_tensor(out=o3v, in0=o3v, in1=zb, op=ALU.mult),
                     reads=["zt"], writes=W32)
            P.op("dve", lambda e, o3=o3, o1=o1, ncol=ncol: e.tensor_copy(out=o1[:, 0:ncol], in_=o3[:, 0:ncol]),
                 reads=W32, writes=[("ob16", pb)])
            P.dma(o32[t * 128:(t + 1) * 128, c0:c0 + ncol], o3[:, 0:ncol], reads=W32, is_output=True)
            P.dma(o16[t * 128:(t + 1) * 128, c0:c0 + ncol], o1[:, 0:ncol], reads=[("ob16", pb)], is_output=True)
    return C.finish()


def bc(v):
    return np.ascontiguousarray(np.broadcast_to(np.asarray(v, np.float32)[None, :], (128, v.shape[0])))


def run_k1(x2d, w_in_l, mod_l, g_l, tabs):
    nc = build_k1()
    cn, sn, cr, sr = tabs
    sc = np.float32(128 ** -0.5)
    wre = np.ascontiguousarray(w_in_l[:, COL_PERM])
    ident = np.eye(128, dtype=np.float32).astype(ml_dtypes.bfloat16)
    in_maps = []
    for i in range(NCORES):
        sl = slice(1024 * i, 1024 * (i + 1))
        in_maps.append({
            "x": np.ascontiguousarray(x2d[sl]), "w": wre,
            "modA": bc(mod_l[D:2 * D]), "modS": bc(mod_l[0:D]), "gN": bc(g_l), "ident": ident,
            "tabn": np.ascontiguousarray(np.stack([cn[sl], sn[sl]])),
            "tabr": np.ascontiguousarray(np.stack([cr[sl], sr[sl]])), "zt": np.ascontiguousarray(ret_ztab()[sl]),
        })
    res = run(nc, in_maps)
    return (np.concatenate([r["o32"] for r in res], axis=0), np.concatenate([r["o16"] for r in res], axis=0))


SCALE = 128 ** -0.5


def build_k2a(NS=8):
    SU = NS * 1024
    NKT = SU // 128
    NCMP = (SU - 32) // 16 + 1
    NCH = (NCMP + 127) // 128
    C = Ctx()
    P = C.P
    identd = C.din("ident", [128, 128], BF16)
    ident32d = C.din("ident32", [128, 128])
    cvh = C.din("cvh", [128, 4, NS, 158])
    cgh = C.din("cgh", [128, 4, NS, 158])
    dwd = C.din("dw", [128, 4, 32])
    lngd = C.din("lng", [128, 512])
    lnbd = C.din("lnb", [128, 512])
    kzd = C.din("kz", [SU, 512], BF16)
    rvd = C.din("rv", [SU, 512], BF16)
    qpTd = C.din("qpT", [128, NS, 4, 128], BF16)
    kzTd = C.din("kzT", [128, NS, 4, 128], BF16)
    rgd = C.din("rg", [NS * 128, 512])
    rvod = C.din("rvo", [NS * 128, 512], BF16)
    gngd = C.din("gng", [128, 512])
    gnbd = C.din("gnb", [128, 512])
    decd = C.din("dec", [128, 512])
    trid = C.din("tri", [128, 128], BF16)
    indd = C.din("ind", [128, 8])
    kcmpTd = C.din("kcmpT", [2, 2, 128, SU], BF16)
    w1d = C.din("w1", [2, 4096, 128])
    w2d = C.din("w2", [2, 128, 128])
    peTd = C.din("peT", [2, 128, 32])
    qTd = C.din("qT", [128, NS, 8, 128], BF16)
    ksTd = C.din("ksT", [2, 128, SU], BF16)
    vsd = C.din("vs", [SU, 256], BF16)
    kwTd = C.din("kwT", [128, NS, 2, 640], BF16)
    vwd = C.din("vw", [128, NS, 2, 5, 128], BF16)
    gtd = C.din("gt", [NS * 128, 24])
    coverd = C.din("cover", [128, NCH, 128], BF16)
    nt16d = C.din("nt16", [128, NCH, 128])
    b64d = C.din("b64", [128, 128])
    fmd = C.din("fm", [128, 128])
    f0d = C.din("f0", [128, 128])
    q0d = C.din("q0c", [128, 2, NS])
    cmaskd = C.din("cmask", [128, 8, 128], BF16)
    wmaskd = C.din("wmask", [128, NS, 5, 128], BF16)
    ycat = C.dout("ycat", [NS * 128, 2048])

    ident = C.sb("ident", [128, 128], BF16)
    ident32 = C.sb("ident32", [128, 128])
    ones = C.sb("ones", [128, 1], BF16)
    P.dma(ident[:], identd, writes=["ident"])
    P.dma(ident32[:], ident32d, writes=["ident32"])
    P.op("pool", lambda e: e.memset(ones[:], 1.0), writes=["ones"])

    NB = 8
    bank = [C.ps("bk%d" % i, [128, 512]) for i in range(NB)]

    def bk(i):
        return ("bk", i)

    kcT = C.sb("kcT", [128, 2, NCH * 128], BF16)
    vc = C.sb("vc", [128, 2, NCH, 128], BF16)
    P.op("pool", lambda e: e.memset(kcT[:], 0.0), writes=["kcT"])
    P.op("pool", lambda e: e.memset(vc[:], 0.0), writes=["vc"])
    with ExitStack() as ph1:
        def sb1(name, shape, dt=F32):
            return ph1.enter_context(C.nc.sbuf_tensor("sb_" + name, list(shape), dt))
        w1s = sb1("w1s", [128, 32, 128])
        w1b = sb1("w1b", [128, 32, 128], BF16)
        w2s = sb1("w2s", [128, 128])
        w2b = sb1("w2b", [128, 128], BF16)
        pes = sb1("pes", [128, 32])
        peb = sb1("peb", [128, 32], BF16)
        bia = sb1("bia", [128, 1])
        xT = sb1("xT", [128, SU], BF16)
        a1 = sb1("a1", [128, NCH * 128], BF16)
        P.op("pool", lambda e: e.memset(a1[:], 0.0), writes=["a1"])
        for kv in range(2):
            P.dma(w1s[:], w1d[kv].rearrange("(l d) o -> d l o", d=128), writes=["w1s"])
            P.dma(w2s[:], w2d[kv], writes=["w2s"])
            P.dma(pes[:], peTd[kv], writes=["pes"])
            P.op("pool", lambda e: e.tensor_copy(out=w1b[:], in_=w1s[:]), reads=["w1s"], writes=["w1b"])
            P.op("pool", lambda e: e.tensor_copy(out=w2b[:], in_=w2s[:]), reads=["w2s"], writes=["w2b"])
            P.op("pool", lambda e: e.tensor_copy(out=peb[:], in_=pes[:]), reads=["pes"], writes=["peb"])
            for l in range(32):
                P.op("pe", lambda e, l=l: e.matmul(bank[0][:, 0:1], lhsT=w1b[:, l, :], rhs=peb[:, l:l + 1],
                                                   start=(l == 0), stop=(l == 31)),
                     reads=["w1b", "peb"], writes=[bk(0)])
            P.op("act", lambda e: e.copy(out=bia[:], in_=bank[0][:, 0:1]), writes=["bia", bk(0)])
            for hd in range(2):
                P.dma(xT[:], kcmpTd[kv, hd], writes=["xT"])
                for l in range(32):
                    P.op("pe", lambda e, l=l: e.matmul(bank[1][:, 0:NCMP], lhsT=w1b[:, l, :],
                                                       rhs=xT[:, l:l + 16 * (NCMP - 1) + 1:16],
                                                       start=(l == 0), stop=(l == 31)),
                         reads=["w1b", "xT"], writes=[bk(1)])
                P.op("act", lambda e: e.activation(out=a1[:, 0:NCMP], in_=bank[1][:, 0:NCMP], func=AF.Silu, bias=bia[:, 0:1]),
                     reads=["bia"], writes=["a1", bk(1)])
                if kv == 0:
                    P.op("pe", lambda e: e.matmul(bank[2][:, 0:NCMP], lhsT=w2b[:], rhs=a1[:, 0:NCMP], start=True, stop=True),
                         reads=["w2b", "a1"], writes=[bk(2)])
                    P.op("act", lambda e, hd=hd: e.copy(out=kcT[:, hd, 0:NCMP], in_=bank[2][:, 0:NCMP]), writes=["kcT", bk(2)])
                else:
                    for ch in range(NCH):
                        P.op("pe", lambda e, ch=ch: e.matmul(bank[2][:, ch * 128:(ch + 1) * 128], lhsT=a1[:, ch * 128:(ch + 1) * 128],
                                                             rhs=w2b[:], start=True, stop=True),
                             reads=["w2b", "a1"], writes=[bk(2)])
                    P.op("act", lambda e, hd=hd: e.copy(out=vc[:, hd, :, :], in_=bank[2][:, 0:NCH * 128].rearrange("p (c d) -> p c d", d=128)),
                         writes=["vc", bk(2)])
        P.barrier()
    Tst = C.sb("Tst", [128, 512])
    Tacc = C.sb("Tacc", [128, NS, 512])
    Tb = C.sb("Tb", [128, NS, 512], BF16)
    dec = C.sb("dec", [128, 512])
    ind = C.sb("ind", [128, 8])
    kzt = [C.sb("kzt%d" % i, [128, 512], BF16) for i in range(2)]
    rvt = [C.sb("rvt%d" % i, [128, 512], BF16) for i in range(2)]
    P.dma(dec[:], decd, writes=["dec"])
    P.dma(ind[:], indd, writes=["ind"])
    P.op("pool", lambda e: e.memset(Tst[:], 0.0), writes=["Tst"])
    P.op("pool", lambda e: e.memset(Tacc[:], 0.0), writes=["Tacc"])
    for m in range(NKT):
        k = m % 2
        j = m // 8
        P.op("dve", lambda e, j=j, m=m: e.scalar_tensor_tensor(out=Tacc[:, j, :], in0=Tst[:], scalar=ind[:, m % 8:m % 8 + 1],
                                                              in1=Tacc[:, j, :], op0=ALU.mult, op1=ALU.add),
             reads=["Tst", "ind"], writes=["Tacc"])
        if m == NKT - 1:
            break
        P.dma(kzt[k][:], kzd[m * 128:(m + 1) * 128, :], writes=[("kzt", k)])
        P.dma(rvt[k][:], rvd[m * 128:(m + 1) * 128, :], writes=[("rvt", k)])
        for h in range(4):
            P.op("pe", lambda e, k=k, h=h: e.matmul(bank[3][:, h * 128:(h + 1) * 128], lhsT=kzt[k][:, h * 128:(h + 1) * 128],
                                                    rhs=rvt[k][:, h * 128:(h + 1) * 128], start=True, stop=True),
                 reads=[("kzt", k), ("rvt", k)], writes=[bk(3)])
        P.op("dve", lambda e: e.tensor_tensor(out=Tst[:], in0=Tst[:], in1=bank[3][:], op=ALU.add), writes=["Tst", bk(3)])
        P.op("dve", lambda e: e.tensor_tensor(out=Tst[:], in0=Tst[:], in1=dec[:], op=ALU.mult), reads=["dec"], writes=["Tst"])
    P.op("act", lambda e: e.copy(out=Tb[:], in_=Tacc[:]), reads=["Tacc"], writes=["Tb"])

    ksT = C.sb("ksT", [128, 2, SU], BF16)
    vs = C.sb("vs", [128, NKT, 256], BF16)
    for g in range(2):
        P.dma(ksT[:, g, :], ksTd[g], writes=["ksT"])
    for c4 in range(0, NKT, 16):
        n4 = min(16, NKT - c4)
        P.dma(vs[:, c4:c4 + n4, :], vsd[c4 * 128:(c4 + n4) * 128, :].rearrange("(t p) f -> p t f", p=128), writes=["vs"])
    cover = C.sb("cover", [128, NCH, 128], BF16)
    nt16 = C.sb("nt16", [128, NCH, 128])
    b64 = C.sb("b64", [128, 128])
    fm = C.sb("fm", [128, 128])
    f0 = C.sb("f0", [128, 128])
    q0c = C.sb("q0c", [128, 2, NS])
    cmask = C.sb("cmask", [128, 8, 128], BF16)
    wmask = C.sb("wmask", [128, NS, 5, 128], BF16)
    tri = C.sb("tri", [128, 128], BF16)
    dw = C.sb("dw", [128, 4, 32])
    lng = C.sb("lng", [128, 512])
    lnb = C.sb("lnb", [128, 512])
    gng = C.sb("gng", [128, 512])
    gnb = C.sb("gnb", [128, 512])
    for (t_, d_, nm) in ((cover, coverd, "cover"), (nt16, nt16d, "nt16"), (b64, b64d, "b64"), (fm, fmd, "fm"), (f0, f0d, "f0"),
                         (q0c, q0d, "q0c"), (cmask, cmaskd, "cmask"), (wmask, wmaskd, "wmask"), (tri, trid, "tri"), (dw, dwd, "dw"),
                         (lng, lngd, "lng"), (lnb, lnbd, "lnb"), (gng, gngd, "gng"), (gnb, gnbd, "gnb")):
        P.dma(t_[:], d_, writes=[nm])

    yt = C.sb("yt", [128, 2048])
    cv = C.sb("cv", [128, 4, 158])
    cg = C.sb("cg", [128, 4, 158])
    cacc = C.sb("cacc", [128, 4, 128])
    w512 = [C.sb("w512_%d" % i, [128, 512]) for i in range(3)]
    st8 = C.sb("st8", [128, 16])
    qpT = C.sb("qpT", [128, 4, 128], BF16)
    kzT = C.sb("kzT", [128, 4, 128], BF16)
    rg = C.sb("rg", [128, 512])
    rvo = C.sb("rvo", [128, 512], BF16)
    innT = C.sb("innT", [128, 4, 128], BF16)
    qT = C.sb("qT", [128, 8, 128], BF16)
    kwT = C.sb("kwT", [128, 2, 640], BF16)
    vw = C.sb("vw", [128, 2, 5, 128], BF16)
    gt = C.sb("gt", [128, 24])
    gsg = C.sb("gsg", [128, 24])
    eT = [C.sb("eT%d" % i, [128, 512]) for i in range(2)]
    eTm = [C.sb("eTm%d" % i, [128, 4, 128], BF16) for i in range(2)]
    mk = C.sb("mk", [128, NCH, 128], BF16)
    m2 = C.sb("m2", [128, 128], BF16)
    Et = [C.sb("Et%d" % i, [128, 128], BF16) for i in range(2)]
    imp = C.sb("imp", [128, 128])
    impw = C.sb("impw", [128, 128])
    vld = C.sb("vld", [128, 128])
    sel = C.sb("sel", [128, 128], BF16)
    selT = C.sb("selT", [128, 128], BF16)
    mx8 = C.sb("mx8", [128, 16])
    lrec = C.sb("lrec", [128, 8])
    ynsa = C.sb("ynsa", [128, 4, 128])
    B_S, B_O, B_L, B_M, B_X = 4, 5, 6, 7, 3
    sbanks = [4, 0]
    nev = [0]

    mbanks = [B_M, 1]

    def stage1(it):
        b = nev[0] % 2
        nev[0] += 1
        if it.get("pre") is not None:
            it["pre"](b)
        sb_ = sbanks[b]
        P.op("pe", lambda e, sb_=sb_, lhsT=it["lhsT"], qg=it["qg"]: e.matmul(bank[sb_][:], lhsT=lhsT, rhs=qg, start=True, stop=True),
             reads=["qT"] + it["rd"], writes=[bk(sb_)])
        P.op("act", lambda e, b=b, sb_=sb_: e.activation(out=eT[b][:], in_=bank[sb_][:], func=AF.Exp, scale=SCALE),
             writes=[("eT", b), bk(sb_)])
        it["mask_fn"](b)
        return b

    def stage2(it, b, first, last):
        vfn = it["vfn"]
        for h in range(4):
            P.op("pe", lambda e, b=b, h=h, vfn=vfn: e.matmul(bank[B_O][:, h * 128:(h + 1) * 128], lhsT=eTm[b][:, h, :], rhs=vfn(),
                                                             start=(first and h == 0), stop=last, skip_group_check=True),
                 reads=[("eTm", b)] + it["extra"], writes=[bk(B_O)])
        for h in range(4):
            P.op("pe", lambda e, b=b, h=h: e.matmul(bank[B_L][:, h:h + 1], lhsT=eTm[b][:, h, :], rhs=ones[:],
                                                    start=(first and h == 0), stop=last, skip_group_check=True),
                 reads=[("eTm", b), "ones"], writes=[bk(B_L)])
        if it.get("post") is not None:
            it["post"](b, first, last)

    def pipeline(items):
        n = len(items)
        bs = [None] * n
        bs[0] = stage1(items[0])
        for i_ in range(n):
            if i_ + 1 < n:
                bs[i_ + 1] = stage1(items[i_ + 1])
            stage2(items[i_], bs[i_], i_ == 0, i_ == n - 1)

    def finish_branch(g, br, first_branch):
        P.op("dve", lambda e: e.tensor_scalar(out=lrec[:, 0:4], in0=bank[B_L][:, 0:4], scalar1=1e-30, scalar2=None, op0=ALU.max),
             writes=["lrec", bk(B_L)])
        P.op("dve", lambda e: e.reciprocal(out=lrec[:, 4:8], in_=lrec[:, 0:4]), writes=["lrec"])
        P.op("dve", lambda e: e.tensor_tensor(out=lrec[:, 0:4], in0=lrec[:, 4:8], in1=gsg[:, br * 8 + 4 * g:br * 8 + 4 * g + 4], op=ALU.mult),
             reads=["gsg"], writes=["lrec"])
        wb = lrec[:, 0:4].unsqueeze(2).to_broadcast([128, 4, 128])
        ov = bank[B_O][:].rearrange("p (h d) -> p h d", d=128)
        if first_branch:
            P.op("dve", lambda e: e.tensor_tensor(out=ynsa[:], in0=ov, in1=wb, op=ALU.mult), reads=["lrec"], writes=["ynsa", bk(B_O)])
        else:
            tv = w512[0][:].rearrange("p (h d) -> p h d", d=128)
            P.op("dve", lambda e: e.tensor_tensor(out=tv, in0=ov, in1=wb, op=ALU.mult), reads=["lrec"], writes=[("w512", 0), bk(B_O)])
            P.op("dve", lambda e: e.tensor_tensor(out=ynsa[:], in0=ynsa[:], in1=tv, op=ALU.add), reads=[("w512", 0)], writes=["ynsa"])

    def layer_norm_free(src_tag, x3, nh, dd, gam, bet, out3, out_tag, gtag, btag):
        inv = 1.0 / dd
        P.op("dve", lambda e: e.reduce_sum(out=st8[:, 0:nh], in_=x3, axis=AX.X), reads=[src_tag], writes=["st8"])
        P.op("dve", lambda e: e.tensor_scalar(out=st8[:, 0:nh], in0=st8[:, 0:nh], scalar1=-inv, scalar2=None, op0=ALU.mult), writes=["st8"])
        mb = st8[:, 0:nh].unsqueeze(2).to_broadcast([128, nh, dd])
        P.op("dve", lambda e: e.tensor_tensor(out=x3, in0=x3, in1=mb, op=ALU.add), reads=["st8"], writes=[src_tag])
        sq3 = w512[1][:, 0:nh * dd].rearrange("p (h d) -> p h d", d=dd)
        P.op("act", lambda e: e.activation(out=sq3, in_=x3, func=AF.Square), reads=[src_tag], writes=[("w512", 1)])
        P.op("dve", lambda e: e.reduce_sum(out=st8[:, 4:4 + nh], in_=sq3, axis=AX.X), reads=[("w512", 1)], writes=["st8"])
        P.op("act", lambda e: e.activation(out=st8[:, 8:8 + nh], in_=st8[:, 4:4 + nh], func=AF.Sqrt, scale=inv, bias=EPS), writes=["st8"])
        P.op("dve", lambda e: e.reciprocal(out=st8[:, 12:12 + nh], in_=st8[:, 8:8 + nh]), writes=["st8"])
        rb = st8[:, 12:12 + nh].unsqueeze(2).to_broadcast([128, nh, dd])
        P.op("dve", lambda e: e.tensor_tensor(out=x3, in0=x3, in1=rb, op=ALU.mult), reads=["st8"], writes=[src_tag])
        g3 = gam[:, 0:nh * dd].rearrange("p (h d) -> p h d", d=dd)
        b3 = bet[:, 0:nh * dd].rearrange("p (h d) -> p h d", d=dd)
        P.op("dve", lambda e: e.tensor_tensor(out=x3, in0=x3, in1=g3, op=ALU.mult), reads=[gtag], writes=[src_tag])
        P.op("dve", lambda e: e.tensor_tensor(out=out3, in0=x3, in1=b3, op=ALU.add), reads=[src_tag, btag], writes=[out_tag])

    for j in range(NS):
        P.dma(cv[:], cvh[:, :, j, :], writes=["cv"])
        P.dma(cg[:], cgh[:, :, j, :], writes=["cg"])
        P.dma(qpT[:], qpTd[:, j], writes=["qpT"])
        P.dma(kzT[:], kzTd[:, j], writes=["kzT"])
        P.dma(rg[:], rgd[j * 128:(j + 1) * 128, :], writes=["rg"])
        P.dma(qT[:], qTd[:, j], writes=["qT"])
        P.dma(kwT[:], kwTd[:, j], writes=["kwT"])
        P.dma(vw[:], vwd[:, j], writes=["vw"])
        P.dma(gt[:], gtd[j * 128:(j + 1) * 128, :], writes=["gt"])
        P.op("act", lambda e: e.activation(out=cg[:], in_=cg[:], func=AF.Sigmoid), writes=["cg"])
        P.op("dve", lambda e: e.tensor_tensor(out=cv[:], in0=cv[:], in1=cg[:], op=ALU.mult), reads=["cg"], writes=["cv"])
        for ch in range(4):
            en = "dve"
            tg = ("cacc", ch)
            P.op(en, lambda e, ch=ch: e.tensor_scalar(out=cacc[:, ch, :], in0=cv[:, ch, 0:128], scalar1=dw[:, ch, 0:1],
                                                      scalar2=dw[:, ch, 31:32], op0=ALU.mult, op1=ALU.add),
                 reads=["cv", "dw"], writes=[tg])
            for w in range(1, 31):
                P.op(en, lambda e, ch=ch, w=w: e.scalar_tensor_tensor(out=cacc[:, ch, :], in0=cv[:, ch, w:w + 128],
                                                                        scalar=dw[:, ch, w:w + 1], in1=cacc[:, ch, :],
                                                                        op0=ALU.mult, op1=ALU.add),
                     reads=["cv", "dw"], writes=[tg])
        for ch in range(4):
            P.op("pe", lambda e, ch=ch: e.transpose(out=bank[B_X][:, ch * 128:(ch + 1) * 128], in_=cacc[:, ch, :], identity=ident32[:]),
                 reads=[("cacc", ch), "ident32"], writes=[bk(B_X)])
        P.op("act", lambda e: e.copy(out=w512[2][:], in_=bank[B_X][:]), writes=[("w512", 2), bk(B_X)])
        x3 = w512[2][:].rearrange("p (h d) -> p h d", d=512)
        layer_norm_free(("w512", 2), x3, 1, 512, lng, lnb, x3, ("w512", 2), "lng", "lnb")
        P.op("act", lambda e: e.activation(out=yt[:, 0:512], in_=w512[2][:], func=AF.Silu), reads=[("w512", 2)], writes=["yt"])
        P.dma(rvo[:], rvod[j * 128:(j + 1) * 128, :], writes=["rvo"])
        for h in range(4):
            P.op("pe", lambda e, h=h: e.matmul(bank[B_X][:, h * 128:(h + 1) * 128], lhsT=kzT[:, h, :], rhs=qpT[:, h, :], start=True, stop=True),
                 reads=["kzT", "qpT"], writes=[bk(B_X)])
        P.op("dve", lambda e: e.tensor_tensor(out=innT[:], in0=bank[B_X][:].rearrange("p (h d) -> p h d", d=128),
                                              in1=tri[:].unsqueeze(1).to_broadcast([128, 4, 128]), op=ALU.mult),
             reads=["tri"], writes=["innT", bk(B_X)])
        for h in range(4):
            P.op("pe", lambda e, h=h: e.matmul(bank[B_O][:, h * 128:(h + 1) * 128], lhsT=innT[:, h, :], rhs=rvo[:, h * 128:(h + 1) * 128],
                                               start=True, stop=False),
                 reads=["innT", "rvo"], writes=[bk(B_O)])
            P.op("pe", lambda e, h=h, j=j: e.matmul(bank[B_O][:, h * 128:(h + 1) * 128], lhsT=qpT[:, h, :], rhs=Tb[:, j, h * 128:(h + 1) * 128],
                                                    start=False, stop=True),
                 reads=["qpT", "Tb"], writes=[bk(B_O)])
        P.op("act", lambda e: e.copy(out=w512[2][:], in_=bank[B_O][:]), writes=[("w512", 2), bk(B_O)])
        x3 = w512[2][:].rearrange("p (h d) -> p h d", d=128)
        layer_norm_free(("w512", 2), x3, 4, 128, gng, gnb, x3, ("w512", 2), "gng", "gnb")
        P.op("act", lambda e: e.activation(out=rg[:], in_=rg[:], func=AF.Silu), writes=["rg"])
        P.op("dve", lambda e: e.tensor_tensor(out=yt[:, 1536:2048], in0=w512[2][:], in1=rg[:], op=ALU.mult),
             reads=[("w512", 2), "rg"], writes=["yt"])
        P.op("act", lambda e: e.activation(out=gsg[:], in_=gt[:], func=AF.Sigmoid), reads=["gt"], writes=["gsg"])
        P.op("dve", lambda e, j=j: e.tensor_scalar(out=mk[:], in0=nt16[:], scalar1=q0c[:, 0, j:j + 1], scalar2=None, op0=ALU.is_le),
             reads=["nt16", "q0c"], writes=["mk"])
        P.op("dve", lambda e, j=j: e.tensor_scalar(out=vld[:], in0=b64[:], scalar1=q0c[:, 0, j:j + 1], scalar2=None, op0=ALU.is_le),
             reads=["b64", "q0c"], writes=["vld"])
        for g in range(2):
            qg = qT[:].rearrange("p h q -> p (h q)")[:, 512 * g:512 * g + 512]

            def mask_sb(mask_ap, tags):
                def f(b):
                    P.op("dve", lambda e, b=b: e.tensor_tensor(out=eTm[b][:], in0=eT[b][:].rearrange("p (h q) -> p h q", q=128),
                                                               in1=mask_ap.unsqueeze(1).to_broadcast([128, 4, 128]), op=ALU.mult),
                         reads=[("eT", b)] + tags, writes=[("eTm", b)])
                return f

            def imp_post(ch):
                def f(b, first, last):
                    for h in range(4):
                        P.op("pe", lambda e, b=b, h=h: e.matmul(bank[B_M][:, h * 128:(h + 1) * 128], lhsT=eTm[b][:, h, :], rhs=cover[:, ch, :],
                                                                start=(first and h == 0), stop=last, skip_group_check=True),
                             reads=[("eTm", b), "cover"], writes=[bk(B_M)])
                return f
            pipeline([dict(lhsT=kcT[:, g, ch * 128:(ch + 1) * 128], qg=qg, rd=["kcT"], mask_fn=mask_sb(mk[:, ch, :], ["mk"]),
                           vfn=(lambda g=g, ch=ch: vc[:, g, ch, :]), extra=["vc"], post=imp_post(ch)) for ch in range(NCH)])
            P.op("dve", lambda e: e.tensor_scalar(out=lrec[:, 0:4], in0=bank[B_L][:, 0:4], scalar1=1e-30, scalar2=None, op0=ALU.max),
                 writes=["lrec", bk(B_L)])
            P.op("dve", lambda e: e.reciprocal(out=lrec[:, 4:8], in_=lrec[:, 0:4]), writes=["lrec"])
            P.op("dve", lambda e: e.tensor_scalar(out=imp[:], in0=bank[B_M][:, 0:128], scalar1=lrec[:, 4:5], scalar2=None, op0=ALU.mult),
                 reads=["lrec"], writes=["imp", bk(B_M)])
            for h in range(1, 4):
                P.op("dve", lambda e, h=h: e.scalar_tensor_tensor(out=imp[:], in0=bank[B_M][:, h * 128:(h + 1) * 128], scalar=lrec[:, 4 + h:5 + h],
                                                                   in1=imp[:], op0=ALU.mult, op1=ALU.add),
                     reads=["lrec"], writes=["imp", bk(B_M)])
            finish_branch(g, 0, True)
            P.op("dve", lambda e: e.tensor_scalar(out=impw[:], in0=vld[:], scalar1=1.0, scalar2=1e30, op0=ALU.subtract, op1=ALU.mult),
                 reads=["vld"], writes=["impw"])
            P.op("dve", lambda e: e.tensor_tensor(out=imp[:], in0=imp[:], in1=vld[:], op=ALU.mult), reads=["vld"], writes=["imp"])
            P.op("dve", lambda e: e.tensor_tensor(out=imp[:], in0=imp[:], in1=impw[:], op=ALU.add), reads=["impw"], writes=["imp"])
            P.op("dve", lambda e, j=j: e.tensor_scalar(out=impw[:], in0=fm[:], scalar1=q0c[:, 1, j:j + 1], scalar2=None, op0=ALU.is_equal),
                 reads=["fm", "q0c"], writes=["impw"])
            P.op("dve", lambda e: e.tensor_tensor(out=impw[:], in0=impw[:], in1=f0[:], op=ALU.max), reads=["f0"], writes=["impw"])
            P.op("dve", lambda e: e.scalar_tensor_tensor(out=imp[:], in0=impw[:], scalar=1e30, in1=imp[:], op0=ALU.mult, op1=ALU.max),
                 reads=["impw"], writes=["imp"])
            P.op("dve", lambda e: e.max(out=mx8[:, 0:8], in_=imp[:]), reads=["imp"], writes=["mx8"])
            P.op("dve", lambda e: e.match_replace(out=impw[:], in_to_replace=mx8[:, 0:8], in_values=imp[:], imm_value=-3.0e38),
                 reads=["imp", "mx8"], writes=["impw"])
            P.op("dve", lambda e: e.max(out=mx8[:, 8:16], in_=impw[:]), reads=["impw"], writes=["mx8"])
            P.op("dve", lambda e: e.tensor_scalar(out=impw[:], in0=imp[:], scalar1=mx8[:, 15:16], scalar2=None, op0=ALU.is_ge),
                 reads=["imp", "mx8"], writes=["impw"])
            P.op("dve", lambda e: e.tensor_tensor(out=impw[:], in0=impw[:], in1=vld[:], op=ALU.mult), reads=["vld"], writes=["impw"])
            P.op("pe", lambda e: e.transpose(out=bank[B_M][:, 0:128], in_=impw[:], identity=ident32[:]), reads=["impw", "ident32"], writes=[bk(B_M)])
            P.op("act", lambda e: e.copy(out=selT[:], in_=bank[B_M][:, 0:128]), writes=["selT", bk(B_M)])
            nkt = 8 * j + 8

            def sel_pre(kt):
                def f(b):
                    mb = mbanks[b]
                    P.op("pool", lambda e, b=b: e.tensor_copy(out=Et[b][:].rearrange("p (a k) -> p a k", k=64),
                                                              in_=ident[:, 2 * kt:2 * kt + 2].unsqueeze(2).to_broadcast([128, 2, 64])),
                         reads=["ident"], writes=[("Et", b)])
                    P.op("pe", lambda e, b=b, mb=mb: e.matmul(bank[mb][:, 0:128], lhsT=Et[b][:], rhs=selT[:], start=True, stop=True),
                         reads=[("Et", b), "selT"], writes=[bk(mb)])
                return f

            def sel_mask(kt):
                if kt < 8 * j:
                    def mf(b):
                        mb = mbanks[b]
                        P.op("dve", lambda e, b=b, mb=mb: e.tensor_tensor(out=eTm[b][:], in0=eT[b][:].rearrange("p (h q) -> p h q", q=128),
                                                                          in1=bank[mb][:, 0:128].unsqueeze(1).to_broadcast([128, 4, 128]), op=ALU.mult),
                             reads=[("eT", b)], writes=[("eTm", b), bk(mb)])
                else:
                    def mf(b, o=kt - 8 * j):
                        mb = mbanks[b]
                        P.op("dve", lambda e, mb=mb: e.tensor_tensor(out=m2[:], in0=bank[mb][:, 0:128], in1=cmask[:, o, :], op=ALU.mult),
                             reads=["cmask"], writes=["m2", bk(mb)])
                        P.op("dve", lambda e, b=b: e.tensor_tensor(out=eTm[b][:], in0=eT[b][:].rearrange("p (h q) -> p h q", q=128),
                                                                   in1=m2[:].unsqueeze(1).to_broadcast([128, 4, 128]), op=ALU.mult),
                             reads=[("eT", b), "m2"], writes=[("eTm", b)])
                return mf
            pipeline([dict(lhsT=ksT[:, g, kt * 128:(kt + 1) * 128], qg=qg, rd=["ksT"], pre=sel_pre(kt), mask_fn=sel_mask(kt),
                           vfn=(lambda g=g, kt=kt: vs[:, kt, g * 128:(g + 1) * 128]), extra=["vs"]) for kt in range(nkt)])
            finish_branch(g, 1, False)
            pipeline([dict(lhsT=kwT[:, g, o * 128:(o + 1) * 128], qg=qg, rd=["kwT"], mask_fn=mask_sb(wmask[:, j, o, :], ["wmask"]),
                           vfn=(lambda g=g, o=o: vw[:, g, o, :]), extra=["vw"]) for o in range(5)])
            finish_branch(g, 2, False)
            P.op("act", lambda e, g=g: e.copy(out=yt[:, 512 + 512 * g:1024 + 512 * g], in_=ynsa[:].rearrange("p h d -> p (h d)")),
                 reads=["ynsa"], writes=["yt"])
        P.dma(ycat[j * 128:(j + 1) * 128, :], yt[:], reads=["yt"], is_output=True)
    return C.finish()


def prep_k2a(i, NS, p32, p16, lw):
    SU = NS * 1024
    NCMP = (SU - 32) // 16 + 1
    NCH = (NCMP + 127) // 128
    bf = ml_dtypes.bfloat16
    qbs = [8 * j + i for j in range(NS)]
    own = np.concatenate([np.arange(qb * 128, qb * 128 + 128) for qb in qbs])
    m = {}
    m["ident"] = np.eye(128, dtype=np.float32).astype(bf)
    m["ident32"] = np.eye(128, dtype=np.float32)

    def halo(cols):
        a = np.concatenate([np.zeros((30, 512), np.float32), p32[:, cols:cols + 512]], axis=0)
        out = np.zeros((128, 4, NS, 158), np.float32)
        for j, qb in enumerate(qbs):
            blk = a[qb * 128:qb * 128 + 158].T.reshape(4, 128, 158)
            out[:, :, j, :] = blk.transpose(1, 0, 2)
        return out
    m["cvh"] = halo(O_CV)
    m["cgh"] = halo(O_CG)
    dwt = np.concatenate([lw["conv_dw_w"].T, lw["conv_dw_b"][:, None]], axis=1)
    m["dw"] = np.ascontiguousarray(dwt.reshape(4, 128, 32).transpose(1, 0, 2))
    m["lng"] = bc(lw["conv_ln_g"]); m["lnb"] = bc(lw["conv_ln_b"])
    m["kz"] = np.ascontiguousarray(p16[:SU, O_RK:O_RK + 512])
    m["rv"] = np.ascontiguousarray(p16[:SU, O_RV:O_RV + 512])
    m["rvo"] = np.ascontiguousarray(p16[own, O_RV:O_RV + 512])
    m["qpT"] = np.ascontiguousarray(p16[own, O_RQ:O_RQ + 512].reshape(NS, 128, 4, 128).transpose(3, 0, 2, 1))
    m["kzT"] = np.ascontiguousarray(p16[own, O_RK:O_RK + 512].reshape(NS, 128, 4, 128).transpose(3, 0, 2, 1))
    m["rg"] = np.ascontiguousarray(p32[own, O_RG:O_RG + 512])
    m["gng"] = bc(lw["ret_gn_g"]); m["gnb"] = bc(lw["ret_gn_b"])
    lg = ret_consts()
    m["dec"] = bc(np.repeat(np.exp(lg * 128.0), 128).astype(np.float32))
    kk = np.arange(128)
    m["tri"] = (kk[:, None] <= kk[None, :]).astype(np.float32).astype(bf)
    ind = np.zeros((128, 8), np.float32); ind[:, i] = 1.0
    m["ind"] = ind
    m["kcmpT"] = np.ascontiguousarray(np.stack([
        np.stack([p16[:SU, o + hd * 128:o + hd * 128 + 128].T for hd in range(2)]) for o in (O_KC, O_VC)]))
    m["w1"] = np.ascontiguousarray(np.stack([lw["nsa_cmp_k_w1"], lw["nsa_cmp_v_w1"]]))
    m["w2"] = np.ascontiguousarray(np.stack([lw["nsa_cmp_k_w2"], lw["nsa_cmp_v_w2"]]))
    m["peT"] = np.ascontiguousarray(np.stack([lw["nsa_pe_k"].T, lw["nsa_pe_v"].T]))
    m["qT"] = np.ascontiguousarray(p16[own, O_NQ:O_NQ + 1024].reshape(NS, 128, 8, 128).transpose(3, 0, 2, 1))
    m["ksT"] = np.ascontiguousarray(np.stack([p16[:SU, O_KS + g * 128:O_KS + g * 128 + 128].T for g in range(2)]))
    m["vs"] = np.ascontiguousarray(p16[:SU, O_VS:O_VS + 256])
    kwp = np.concatenate([np.zeros((512, 256), bf), p16[:, O_KW:O_KW + 256]], axis=0)
    vwp = np.concatenate([np.zeros((512, 256), bf), p16[:, O_VW:O_VW + 256]], axis=0)
    kwT = np.zeros((128, NS, 2, 640), bf)
    vw = np.zeros((128, NS, 2, 5, 128), bf)
    wmask = np.zeros((128, NS, 5, 128), np.float32)
    for j, qb in enumerate(qbs):
        q0 = qb * 128
        kwT[:, j] = kwp[q0:q0 + 640].reshape(640, 2, 128).transpose(2, 1, 0)
        vw[:, j] = vwp[q0:q0 + 640].reshape(5, 128, 2, 128).transpose(1, 2, 0, 3)
        for o in range(5):
            kp = q0 - 512 + o * 128 + kk[:, None]
            t = q0 + kk[None, :]
            wmask[:, j, o, :] = ((kp >= 0) & (kp <= t) & (kp > t - 512)).astype(np.float32)
    m["kwT"] = kwT; m["vw"] = vw; m["wmask"] = wmask.astype(bf)
    m["gt"] = np.ascontiguousarray(p32[own, O_GT:O_GT + 24])
    n = (np.arange(NCH)[None, :] * 128 + kk[:, None])
    blk = np.arange(128)
    cov = ((16 * n[:, :, None] <= 64 * blk[None, None, :] + 63) & (16 * n[:, :, None] + 31 >= 64 * blk[None, None, :])
           & (n[:, :, None] < NCMP))
    m["cover"] = cov.astype(np.float32).astype(bf)
    m["nt16"] = (16.0 * n[:, :, None] + 31.0 - kk[None, None, :]).astype(np.float32)
    m["b64"] = (64.0 * blk[None, :] - kk[:, None]).astype(np.float32)
    m["fm"] = (blk[None, :] - (kk[:, None] >= 64)).astype(np.float32)
    f0 = np.zeros((128, 128), np.float32); f0[:, 0] = 2.0
    m["f0"] = f0
    q0c = np.zeros((128, 2, NS), np.float32)
    for j, qb in enumerate(qbs):
        q0c[:, 0, j] = qb * 128; q0c[:, 1, j] = 2 * qb
    m["q0c"] = q0c
    cm = np.zeros((128, 8, 128), np.float32)
    for o in range(8):
        if o < i:
            cm[:, o, :] = 1.0
        elif o == i:
            cm[:, o, :] = (kk[:, None] <= kk[None, :])
    m["cmask"] = cm.astype(bf)
    return m, own


def run_k2a(p32, p16, lw):
    nc = build_k2a(8)
    in_maps, owns = [], []
    for i in range(NCORES):
        m, own = prep_k2a(i, 8, p32, p16, lw)
        in_maps.append(m)
        owns.append(own)
    res = run(nc, in_maps)
    y = np.zeros((S, 2048), np.float32)
    for i in range(NCORES):
        y[owns[i]] = res[i]["ycat"]
    return y


def build_k2b(NT=8):
    C = Ctx()
    P = C.P
    HT = 4 if NT >= 4 else NT
    yd = C.din("y", [NT * 128, D])
    xd = C.din("x", [NT * 128, D])
    wd = C.din("w", [D, D])
    gAd = C.din("gateA", [128, D])
    mAd = C.din("modA", [128, D])
    mSd = C.din("modS", [128, D])
    gNd = C.din("gN", [128, D])
    rwd = C.din("rw", [D, 32])
    rbd = C.din("rb", [128, 32])
    identd = C.din("ident", [128, 128], BF16)
    ident32d = C.din("ident32", [128, 128])
    x1d = C.dout("x1", [NT * 128, D])
    hfTd = C.dout("hfT", [128, 16, NT * 128], BF16)
    Gd = C.dout("G", [NT * 128, 32])

    ident = C.sb("ident", [128, 128], BF16)
    ident32 = C.sb("ident32", [128, 128])
    gA = C.sb("gA", [128, D])
    A = C.sb("A", [128, D])
    Sh = C.sb("Sh", [128, D])
    rw = C.sb("rw", [128, 16, 32])
    rb = C.sb("rb", [128, 32])
    P.dma(ident[:], identd, writes=["ident"])
    P.dma(ident32[:], ident32d, writes=["ident32"])
    P.dma(gA[:], gAd, writes=["gA"])
    P.dma(A[:], mAd, writes=["A"])
    P.dma(Sh[:], mSd, writes=["Sh"])
    P.dma(rw[:], rwd.rearrange("(kc p) n -> p kc n", p=128), writes=["rw"])
    P.dma(rb[:], rbd, writes=["rb"])
    sq = C.sb("sq", [128, D])
    P.dma(sq[:], gNd, writes=["sq"])
    P.op("dve", lambda e: e.scalar_tensor_tensor(out=A[:], in0=A[:], scalar=1.0, in1=sq[:], op0=ALU.add, op1=ALU.mult),
         reads=["sq"], writes=["A"])

    xt = [C.sb("xt%d" % i, [128, D]) for i in range(HT)]
    yT = [C.sb("yT%d" % i, [128, 16, 128], BF16) for i in range(HT)]
    yb = C.sb("yb", [128, D], BF16)
    h32 = C.sb("h32", [128, D])
    hb = C.sb("hb", [128, D], BF16)
    hT = C.sb("hT", [128, 16, 128], BF16)
    hT32 = C.sb("hT32", [128, 16, 128])
    st = C.sb("st", [128, 2])
    wst = [C.sb("wst%d" % i, [128, 8, 512]) for i in range(2)]
    wbf = C.sb("wbf", [128, 16, 512], BF16)
    tmp = [C.sb("tmp%d" % i, [128, 512]) for i in range(2)]
    lg = C.sb("lg", [128, 32])
    ex = C.sb("ex", [128, 32])
    mk = C.sb("mk", [128, 32])
    mx = C.sb("mx", [128, 8])
    s1 = C.sb("s1", [128, 2])
    pT = C.ps("pT", [128, 16, 128], BF16)
    pY = [C.ps("pY%d" % i, [128, 512]) for i in range(2)]
    pF = [C.ps("pF%d" % i, [128, 4, 128]) for i in range(2)]
    pL = C.ps("pL", [128, 512])
    wv = wd.rearrange("(kc p) n -> p kc n", p=128)
    nst = 0
    nev = 0
    for half in range(NT // HT):
        for tt in range(HT):
            t = half * HT + tt
            P.dma(xt[tt][:], xd[t * 128:(t + 1) * 128, :], writes=[("xt", tt)])
            P.dma(sq[:], yd[t * 128:(t + 1) * 128, :], writes=["sq"])
            P.op("pool", lambda e: e.tensor_copy(out=yb[:], in_=sq[:]), reads=["sq"], writes=["yb"])
            for kc in range(16):
                P.op("pe", lambda e, kc=kc: e.transpose(out=pT[:, kc, :], in_=yb[:, kc * 128:(kc + 1) * 128], identity=ident[:]),
                     reads=["yb", "ident"], writes=["pT"])
            P.op("act", lambda e, tt=tt: e.copy(out=yT[tt][:], in_=pT[:]), writes=[("yT", tt), "pT"])
        for cc in range(4):
            c0 = cc * 512
            for hf in range(2):
                sbuf = nst % 2
                nst += 1
                P.dma(wst[sbuf][:], wv[:, hf * 8:(hf + 1) * 8, c0:c0 + 512], writes=[("wst", sbuf)])
                P.op("pool", lambda e, sbuf=sbuf, hf=hf: e.tensor_copy(out=wbf[:, hf * 8:(hf + 1) * 8, :], in_=wst[sbuf][:]),
                     reads=[("wst", sbuf)], writes=[("wbf", hf)])
            for tt in range(HT):
                pb = nev % 2
                nev += 1
                for kc in range(16):
                    P.op("pe", lambda e, pb=pb, tt=tt, kc=kc: e.matmul(pY[pb][:], lhsT=yT[tt][:, kc, :], rhs=wbf[:, kc, :],
                                                                       start=(kc == 0), stop=(kc == 15)),
                         reads=[("yT", tt), ("wbf", kc // 8)], writes=[("pY", pb)])
                P.op("dve", lambda e, pb=pb, c0=c0: e.tensor_tensor(out=tmp[pb][:], in0=pY[pb][:], in1=gA[:, c0:c0 + 512], op=ALU.mult),
                     reads=["gA"], writes=[("tmp", pb), ("pY", pb)])
                P.op("pool", lambda e, pb=pb, tt=tt, c0=c0: e.tensor_tensor(out=xt[tt][:, c0:c0 + 512], in0=xt[tt][:, c0:c0 + 512],
                                                                            in1=tmp[pb][:], op=ALU.add),
                     reads=[("tmp", pb)], writes=[("xt", tt)])
        for tt in range(HT):
            t = half * HT + tt
            x1 = xt[tt]
            P.dma(x1d[t * 128:(t + 1) * 128, :], x1[:], reads=[("xt", tt)], is_output=True)
            P.op("act", lambda e, x1=x1: e.activation(out=sq[:], in_=x1[:], func=AF.Square), reads=[("xt", tt)], writes=["sq"])
            P.op("dve", lambda e: e.reduce_sum(out=st[:, 0:1], in_=sq[:], axis=AX.X), reads=["sq"], writes=["st"])
            P.op("act", lambda e: e.activation(out=st[:, 1:2], in_=st[:, 0:1], func=AF.Sqrt, scale=1.0 / D, bias=EPS), writes=["st"])
            P.op("dve", lambda e: e.reciprocal(out=st[:, 0:1], in_=st[:, 1:2]), writes=["st"])
            P.op("dve", lambda e, x1=x1: e.scalar_tensor_tensor(out=sq[:], in0=x1[:], scalar=st[:, 0:1], in1=A[:], op0=ALU.mult, op1=ALU.mult),
                 reads=[("xt", tt), "st", "A"], writes=["sq"])
            P.op("dve", lambda e: e.tensor_tensor(out=h32[:], in0=sq[:], in1=Sh[:], op=ALU.add), reads=["sq", "Sh"], writes=["h32"])
            P.op("pool", lambda e: e.tensor_copy(out=hb[:], in_=h32[:]), reads=["h32"], writes=["hb"])
            for kc in range(16):
                P.op("pe", lambda e, kc=kc: e.transpose(out=pT[:, kc, :], in_=hb[:, kc * 128:(kc + 1) * 128], identity=ident[:]),
                     reads=["hb", "ident"], writes=["pT"])
            P.op("act", lambda e: e.copy(out=hT[:], in_=pT[:]), writes=["hT", "pT"])
            P.dma(hfTd[:, :, t * 128:(t + 1) * 128], hT[:], reads=["hT"], is_output=True)
            for q4 in range(4):
                fb = q4 % 2
                for u in range(4):
                    kc = q4 * 4 + u
                    P.op("pe", lambda e, fb=fb, u=u, kc=kc: e.transpose(out=pF[fb][:, u, :], in_=h32[:, kc * 128:(kc + 1) * 128], identity=ident32[:]),
                         reads=["h32", "ident32"], writes=[("pF", fb)])
                P.op("act", lambda e, fb=fb, q4=q4: e.copy(out=hT32[:, q4 * 4:q4 * 4 + 4, :], in_=pF[fb][:]), writes=[("hT32", q4), ("pF", fb)])
            for kc in range(16):
                P.op("pe", lambda e, kc=kc: e.matmul(pL[:, 0:32], lhsT=hT32[:, kc, :], rhs=rw[:, kc, :], start=(kc == 0), stop=(kc == 15)),
                     reads=[("hT32", kc // 4), "rw"], writes=["pL"])
            P.op("dve", lambda e: e.tensor_tensor(out=lg[:], in0=pL[:, 0:32], in1=rb[:], op=ALU.add), reads=["rb"], writes=["lg", "pL"])
            P.op("dve", lambda e: e.max(out=mx[:], in_=lg[:]), reads=["lg"], writes=["mx"])
            P.op("dve", lambda e: e.tensor_scalar(out=mk[:], in0=lg[:], scalar1=mx[:, 3:4], scalar2=None, op0=ALU.is_ge), reads=["lg", "mx"], writes=["mk"])
            P.op("dve", lambda e: e.tensor_scalar(out=ex[:], in0=lg[:], scalar1=mx[:, 0:1], scalar2=None, op0=ALU.subtract), reads=["lg", "mx"], writes=["ex"])
            P.op("act", lambda e: e.activation(out=ex[:], in_=ex[:], func=AF.Exp), writes=["ex"])
            P.op("dve", lambda e: e.tensor_tensor(out=ex[:], in0=ex[:], in1=mk[:], op=ALU.mult), reads=["mk"], writes=["ex"])
            P.op("dve", lambda e: e.reduce_sum(out=s1[:, 0:1], in_=ex[:], axis=AX.X), reads=["ex"], writes=["s1"])
            P.op("dve", lambda e: e.reciprocal(out=s1[:, 1:2], in_=s1[:, 0:1]), writes=["s1"])
            P.op("dve", lambda e: e.tensor_scalar(out=lg[:], in0=ex[:], scalar1=s1[:, 1:2], scalar2=None, op0=ALU.mult), reads=["ex", "s1"], writes=["lg"])
            P.dma(Gd[t * 128:(t + 1) * 128, :], lg[:], reads=["lg"], is_output=True)
    return C.finish()


def run_k2b(ycat, x2d, w_out_l, mod_l, gffn_l, router_w_l, router_b_l):
    nc = build_k2b(8)
    ident = np.eye(128, dtype=np.float32)
    in_maps = []
    for i in range(NCORES):
        sl = slice(1024 * i, 1024 * (i + 1))
        in_maps.append({"y": np.ascontiguousarray(ycat[sl]), "x": np.ascontiguousarray(x2d[sl]), "w": w_out_l,
                        "gateA": bc(mod_l[2 * D:3 * D]), "modA": bc(mod_l[4 * D:5 * D]), "modS": bc(mod_l[3 * D:4 * D]),
                        "gN": bc(gffn_l), "rw": router_w_l, "rb": bc(router_b_l),
                        "ident": ident.astype(ml_dtypes.bfloat16), "ident32": ident})
    res = run(nc, in_maps)
    x1 = np.concatenate([r["x1"] for r in res], axis=0)
    hfT = np.concatenate([r["hfT"] for r in res], axis=2)
    G = np.concatenate([r["G"] for r in res], axis=0)
    return x1, hfT, G


def build_k3(NTG=16, NE=4):
    C = Ctx()
    P = C.P
    TOK = NTG * 512
    hTd = C.din("hT", [128, 16, TOK], BF16)
    Gbd = C.din("Gb", [NE, 128, TOK])
    wgud = C.din("wgu", [NE, D, 2 * D])
    wdnd = C.din("wdn", [NE, D, D])
    bgud = C.din("bgu", [128, NE, 32])
    bdnd = C.din("bdn", [128, NE, 16])
    outd = C.dout("outT", [128, 16, TOK])
    sgu = C.nc.dram_tensor("sgu", [NE, 32, 128, 2048], BF16).ap()
    sdn = C.nc.dram_tensor("sdn", [NE, 16, 128, 2048], BF16).ap()

    hT = C.sb("hT", [128, 16, 512], BF16)
    Gb = [C.sb("Gb%d" % i, [128, NE, 512]) for i in range(2)]
    bgu = C.sb("bgu", [128, NE, 32])
    bdn = C.sb("bdn", [128, NE, 16])
    actT = C.sb("actT", [128, NE, 16, 512], BF16)
    NST = 3
    wst = [C.sb("wst%d" % i, [128, 16, 128]) for i in range(NST)]
    NWB = 3
    wg = [C.sb("wg%d" % i, [128, 16, 128], BF16) for i in range(NWB)]
    wl = [C.sb("wl%d" % i, [128, 16, 128], BF16) for i in range(NWB)]
    gtt = [C.sb("gtt%d" % i, [128, 512]) for i in range(2)]
    stt = [C.sb("stt%d" % i, [128, 512]) for i in range(2)]
    ltt = [C.sb("ltt%d" % i, [128, 512]) for i in range(2)]
    ot = [C.sb("ot%d" % i, [128, 512]) for i in range(2)]
    pg = [C.ps("pg%d" % i, [128, 512]) for i in range(2)]
    pl = [C.ps("pl%d" % i, [128, 512]) for i in range(2)]
    po = [C.ps("po%d" % i, [128, 512]) for i in range(2)]
    P.dma(bgu[:], bgud, writes=["bgu"])
    P.dma(bdn[:], bdnd, writes=["bdn"])

    nst = [0]
    pend = []

    def fresh(src, dst, dtag, scr, stag, eng):
        s_ = nst[0] % NST
        nst[0] += 1
        P.dma(wst[s_][:], src, writes=[("wst", s_)])
        if eng == "act":
            P.op("act", lambda e, s_=s_: e.copy(out=dst, in_=wst[s_][:]), reads=[("wst", s_)], writes=[dtag])
        else:
            P.op("pool", lambda e, s_=s_: e.tensor_copy(out=dst, in_=wst[s_][:]), reads=[("wst", s_)], writes=[dtag])
        pend.append((scr, dst, dtag, stag))

    def flush(keep):
        while len(pend) > keep:
            scr, dst, dtag, stag = pend.pop(0)
            P.dma(scr.rearrange("p (a j) -> p a j", j=128), dst, reads=[dtag], writes=[stag])

    ia = 0
    ib = 0
    for tg in range(NTG):
        t0 = tg * 512
        gb = Gb[tg % 2]
        gtag = ("Gb", tg % 2)

        def load_group(tg_):
            P.dma(hT[:], hTd[:, :, tg_ * 512:(tg_ + 1) * 512], writes=["hT"])
            for e_ in range(NE):
                P.dma(Gb[tg_ % 2][:, e_, :], Gbd[e_][:, tg_ * 512:(tg_ + 1) * 512], writes=[("Gb", tg_ % 2)])
        if tg == 0:
            load_group(0)
        for e_ in range(NE):
            for c in range(16):
                b = ia % NWB
                pb = ia % 2
                ia += 1
                if tg == 0:
                    wv = wgud[e_].rearrange("(kc p) n -> p kc n", p=128)
                    fresh(wv[:, :, c * 128:(c + 1) * 128], wg[b][:], ("wg", b), sgu[e_, c], ("sgu", e_, c), "act")
                    fresh(wv[:, :, D + c * 128:D + (c + 1) * 128], wl[b][:], ("wl", b), sgu[e_, 16 + c], ("sgu", e_, 16 + c), "pool")
                    flush(2)
                else:
                    P.dma(wg[b][:], sgu[e_, c].rearrange("p (a j) -> p a j", j=128), reads=[("sgu", e_, c)], writes=[("wg", b)])
                    P.dma(wl[b][:], sgu[e_, 16 + c].rearrange("p (a j) -> p a j", j=128), reads=[("sgu", e_, 16 + c)], writes=[("wl", b)])
                for kc in range(16):
                    P.op("pe", lambda e, b=b, pb=pb, kc=kc: e.matmul(pg[pb][:], lhsT=wg[b][:, kc, :], rhs=hT[:, kc, :], start=(kc == 0), stop=(kc == 15)),
                         reads=[("wg", b), "hT"], writes=[("pg", pb)])
                for kc in range(16):
                    P.op("pe", lambda e, b=b, pb=pb, kc=kc: e.matmul(pl[pb][:], lhsT=wl[b][:, kc, :], rhs=hT[:, kc, :], start=(kc == 0), stop=(kc == 15)),
                         reads=[("wl", b), "hT"], writes=[("pl", pb)])
                b = pb
                P.op("dve", lambda e, b=b, e_=e_, c=c: e.tensor_scalar(out=gtt[b][:], in0=pg[b][:], scalar1=bgu[:, e_, c:c + 1], scalar2=7.0,
                                                                       op0=ALU.add, op1=ALU.min),
                     reads=["bgu"], writes=[("gtt", b), ("pg", b)])
                P.op("act", lambda e, b=b: e.activation(out=stt[b][:], in_=gtt[b][:], func=AF.Sigmoid, scale=1.702),
                     reads=[("gtt", b)], writes=[("stt", b)])
                P.op("dve", lambda e, b=b, e_=e_, c=c: e.tensor_scalar(out=ltt[b][:], in0=pl[b][:], scalar1=bgu[:, e_, 16 + c:17 + c], scalar2=7.0,
                                                                       op0=ALU.add, op1=ALU.min),
                     reads=["bgu"], writes=[("ltt", b), ("pl", b)])
                P.op("dve", lambda e, b=b: e.tensor_scalar(out=ltt[b][:], in0=ltt[b][:], scalar1=-7.0, scalar2=1.0, op0=ALU.max, op1=ALU.add),
                     writes=[("ltt", b)])
                P.op("pool", lambda e, b=b: e.tensor_tensor(out=gtt[b][:], in0=gtt[b][:], in1=stt[b][:], op=ALU.mult),
                     reads=[("stt", b)], writes=[("gtt", b)])
                P.op("pool", lambda e, b=b: e.tensor_tensor(out=gtt[b][:], in0=gtt[b][:], in1=ltt[b][:], op=ALU.mult),
                     reads=[("ltt", b)], writes=[("gtt", b)])
                P.op("dve", lambda e, b=b, e_=e_, c=c, gb=gb: e.tensor_tensor(out=actT[:, e_, c, :], in0=gtt[b][:], in1=gb[:, e_, :], op=ALU.mult),
                     reads=[("gtt", b), gtag], writes=[("actT", e_)])
        flush(0)
        if tg + 1 < NTG:
            load_group(tg + 1)
        for m in range(16):
            pb = m % 2
            for e_ in range(NE):
                b = ib % NWB
                ib += 1
                wt_, wtag = (wg[b], ("wg", b)) if ib % 2 else (wl[b], ("wl", b))
                if tg == 0:
                    fresh(wdnd[e_].rearrange("(c p) n -> p c n", p=128)[:, :, m * 128:(m + 1) * 128], wt_[:], wtag, sdn[e_, m], ("sdn", e_, m),
                          "act" if ib % 2 else "pool")
                    flush(1)
                else:
                    P.dma(wt_[:], sdn[e_, m].rearrange("p (a j) -> p a j", j=128), reads=[("sdn", e_, m)], writes=[wtag])
                for c in range(16):
                    P.op("pe", lambda e, wt_=wt_, c=c, e_=e_, pb=pb: e.matmul(po[pb][:], lhsT=wt_[:, c, :], rhs=actT[:, e_, c, :],
                                                                              start=(e_ == 0 and c == 0), stop=(e_ == NE - 1 and c == 15)),
                         reads=[wtag, ("actT", e_)], writes=[("po", pb)])
            P.op("dve", lambda e, pb=pb, m=m, gb=gb: e.scalar_tensor_tensor(out=ot[pb][:], in0=gb[:, 0, :], scalar=bdn[:, 0, m:m + 1], in1=po[pb][:],
                                                                            op0=ALU.mult, op1=ALU.add),
                 reads=[gtag, "bdn"], writes=[("ot", pb), ("po", pb)])
            for e_ in range(1, NE):
                P.op("dve", lambda e, pb=pb, m=m, e_=e_, gb=gb: e.scalar_tensor_tensor(out=ot[pb][:], in0=gb[:, e_, :], scalar=bdn[:, e_, m:m + 1],
                                                                                       in1=ot[pb][:], op0=ALU.mult, op1=ALU.add),
                     reads=[gtag, "bdn"], writes=[("ot", pb)])
            P.dma(outd[:, m, t0:t0 + 512], ot[pb][:], reads=[("ot", pb)], is_output=True, q="act")
        flush(0)
    return C.finish()


def run_k3(hfT, G, wgu_l, bgu_l, wdn_l, bdn_l):
    nc = build_k3(16, 4)
    in_maps = []
    for i in range(NCORES):
        es = slice(4 * i, 4 * i + 4)
        Gb = np.ascontiguousarray(np.broadcast_to(G[:, es].T[:, None, :], (4, 128, S)))
        in_maps.append({"hT": hfT, "Gb": Gb, "wgu": wgu_l[es], "wdn": wdn_l[es],
                        "bgu": np.ascontiguousarray(bgu_l[es].reshape(4, 32, 128).transpose(2, 0, 1)),
                        "bdn": np.ascontiguousarray(bdn_l[es].reshape(4, 16, 128).transpose(2, 0, 1))})
    res = run(nc, in_maps)
    return [r["outT"] for r in res]


def build_k4(NT=8, final=False):
    C = Ctx()
    P = C.P
    x1d = C.din("x1", [NT * 128, D])
    pd = C.din("parts", [NCORES, NT * 128, D])
    gFd = C.din("gateF", [128, D])
    gfd = C.din("gfin", [128, D])
    od = C.dout("out", [NT * 128, D])
    gF = C.sb("gF", [128, D])
    gfin = C.sb("gfin", [128, D])
    P.dma(gF[:], gFd, writes=["gF"])
    P.dma(gfin[:], gfd, writes=["gfin"])
    xt = [C.sb("xt%d" % i, [128, D]) for i in range(2)]
    acc = [C.sb("acc%d" % i, [128, D]) for i in range(2)]
    pt = [C.sb("pt%d" % i, [128, D]) for i in range(3)]
    sq = C.sb("sq", [128, D])
    st = C.sb("st", [128, 2])
    npt = 0
    for t in range(NT):
        k = t % 2
        P.dma(xt[k][:], x1d[t * 128:(t + 1) * 128, :], writes=[("xt", k)])
        P.dma(acc[k][:], pd[0, t * 128:(t + 1) * 128, :], writes=[("acc", k)])
        for c in range(1, NCORES):
            b = npt % 3
            npt += 1
            P.dma(pt[b][:], pd[c, t * 128:(t + 1) * 128, :], writes=[("pt", b)])
            P.op("dve" if c % 2 else "pool", lambda e, k=k, b=b: e.tensor_tensor(out=acc[k][:], in0=acc[k][:], in1=pt[b][:], op=ALU.add),
                 reads=[("pt", b)], writes=[("acc", k)])
        P.op("dve", lambda e, k=k: e.tensor_tensor(out=acc[k][:], in0=acc[k][:], in1=gF[:], op=ALU.mult), reads=["gF"], writes=[("acc", k)])
        P.op("pool", lambda e, k=k: e.tensor_tensor(out=acc[k][:], in0=acc[k][:], in1=xt[k][:], op=ALU.add), reads=[("xt", k)], writes=[("acc", k)])
        if final:
            P.op("act", lambda e, k=k: e.activation(out=sq[:], in_=acc[k][:], func=AF.Square), reads=[("acc", k)], writes=["sq"])
            P.op("dve", lambda e: e.reduce_sum(out=st[:, 0:1], in_=sq[:], axis=AX.X), reads=["sq"], writes=["st"])
            P.op("act", lambda e: e.activation(out=st[:, 1:2], in_=st[:, 0:1], func=AF.Sqrt, scale=1.0 / D, bias=EPS), writes=["st"])
            P.op("dve", lambda e: e.reciprocal(out=st[:, 0:1], in_=st[:, 1:2]), writes=["st"])
            P.op("dve", lambda e, k=k: e.scalar_tensor_tensor(out=acc[k][:], in0=acc[k][:], scalar=st[:, 0:1], in1=gfin[:], op0=ALU.mult, op1=ALU.mult),
                 reads=["st", "gfin"], writes=[("acc", k)])
        P.dma(od[t * 128:(t + 1) * 128, :], acc[k][:], reads=[("acc", k)], is_output=True)
    return C.finish()


def run_k4(x1, parts, gate_f, gfin, final):
    nc = build_k4(8, final)
    in_maps = []
    for i in range(NCORES):
        sl = slice(1024 * i, 1024 * (i + 1))
        pp = np.stack([np.ascontiguousarray(p[:, :, sl].transpose(2, 1, 0)).reshape(1024, D) for p in parts])
        in_maps.append({"x1": np.ascontiguousarray(x1[sl]), "parts": pp, "gateF": bc(gate_f), "gfin": bc(gfin)})
    res = run(nc, in_maps)
    return np.concatenate([r["out"] for r in res], axis=0)


LAYER_KEYS = ("conv_dw_w", "conv_dw_b", "conv_ln_g", "conv_ln_b", "nsa_pe_k", "nsa_pe_v", "nsa_cmp_k_w1", "nsa_cmp_k_w2",
              "nsa_cmp_v_w1", "nsa_cmp_v_w2", "ret_gn_g", "ret_gn_b")


def kernel(x, c, ada_w, ada_b, norm_mix_g, w_in, conv_dw_w, conv_dw_b, conv_ln_g, conv_ln_b,
           nsa_pe_k, nsa_pe_v, nsa_cmp_k_w1, nsa_cmp_k_w2, nsa_cmp_v_w1, nsa_cmp_v_w2,
           ret_gn_g, ret_gn_b, w_out, norm_ffn_g, router_w, router_b,
           moe_w_gate_up, moe_b_gate_up, moe_w_down, moe_b_down, final_norm_g):
    loc = locals()
    f = lambda a: np.asarray(a, dtype=np.float32)
    xs = f(x)[0]
    mod = run_k0(f(c), f(ada_w), f(ada_b))
    tabs = rot_tables()
    for l in range(DEPTH):
        lw = {k: f(loc[k][l]) for k in LAYER_KEYS}
        p32, p16 = run_k1(xs, f(w_in[l]), mod[l], f(norm_mix_g[l]), tabs)
        ycat = run_k2a(p32, p16, lw)
        del p32, p16
        x1, hfT, G = run_k2b(ycat, xs, f(w_out[l]), mod[l], f(norm_ffn_g[l]), f(router_w[l]), f(router_b[l]))
        parts = run_k3(hfT, G, np.asarray(moe_w_gate_up[l]), f(moe_b_gate_up[l]), np.asarray(moe_w_down[l]), f(moe_b_down[l]))
        xs = run_k4(x1, parts, mod[l][5 * D:6 * D], f(final_norm_g), l == DEPTH - 1)
        del parts
    return xs[None].astype(np.float32)
```

```python
from contextlib import ExitStack

import numpy as np
import ml_dtypes
import concourse.bass as bass
import concourse.mybir as mybir
from concourse.bass_utils import run_bass_kernel_spmd

F32 = mybir.dt.float32
BF16 = mybir.dt.bfloat16
AF = mybir.ActivationFunctionType
ALU = mybir.AluOpType
AX = mybir.AxisListType

NCORES = 8
D = 2048
S = 8192
DEPTH = 2
INW = 5656
EPS = 1e-6

ENGS = ("pe", "act", "dve", "pool", "sp")
N_DMA_SEMS = 8
DMA_QS = ("sp", "act", "pool")


class Prog:
    def __init__(self, nc):
        self.nc = nc
        self.streams = {e: [] for e in ENGS}
        self.count = {e: 0 for e in ENGS}
        self.waited = {e: {} for e in ENGS}
        self.last_w = {}
        self.readers = {}
        self.dma_n = [0] * (N_DMA_SEMS * len(DMA_QS))
        self.dma_rr = [0] * len(DMA_QS)
        self.out_tokens = []

    def _deps(self, reads, writes):
        deps = []
        for b in reads:
            t = self.last_w.get(b)
            if t is not None:
                deps.append(t)
        for b in writes:
            t = self.last_w.get(b)
            if t is not None:
                deps.append(t)
            deps.extend(self.readers.get(b, {}).values())
        return deps

    def _wait(self, eng, tok):
        key, val = tok
        if key == eng == "pe":
            return
        w = self.waited[eng]
        if w.get(key, 0) >= val:
            return
        w[key] = val
        self.streams[eng].append(("wait", key, val))

    def _commit(self, tok, reads, writes):
        for b in writes:
            self.last_w[b] = tok
            self.readers[b] = {}
        for b in reads:
            if b in writes:
                continue
            self.readers.setdefault(b, {})[tok[0]] = tok

    def op(self, eng, fn, reads=(), writes=()):
        for t in self._deps(reads, writes):
            self._wait(eng, t)
        self.count[eng] += 1
        tok = (eng, self.count[eng])
        self.streams[eng].append(("op", fn))
        self._commit(tok, reads, writes)
        return tok

    def dma(self, out, in_, reads=(), writes=(), q="sp", is_output=False, **kw):
        for t in self._deps(reads, writes):
            self._wait(q, t)
        qi = DMA_QS.index(q)
        s = qi * N_DMA_SEMS + self.dma_rr[qi]
        self.dma_rr[qi] = (self.dma_rr[qi] + 1) % N_DMA_SEMS
        key = ("dma", s)
        if self.dma_n[s] > 0:
            self._wait(q, (key, 16 * self.dma_n[s]))
        self.dma_n[s] += 1
        tok = (key, 16 * self.dma_n[s])
        self.streams[q].append(("dma", out, in_, s, kw))
        self._commit(tok, reads, writes)
        if is_output:
            self.out_tokens.append(tok)
        return tok

    def barrier(self):
        toks = [(e, self.count[e]) for e in ENGS if self.count[e] > 0]
        toks += [(("dma", k), 16 * self.dma_n[k]) for k in range(len(self.dma_n)) if self.dma_n[k] > 0]
        for e in ENGS:
            for t in toks:
                self._wait(e, t)

    def emit(self):
        nc = self.nc
        with ExitStack() as st:
            esem = {e: st.enter_context(nc.semaphore("s_" + e)) for e in ENGS}
            dsem = [st.enter_context(nc.semaphore("d%d" % i)) for i in range(N_DMA_SEMS * len(DMA_QS))]
            for t in self.out_tokens:
                self._wait("sp", t)
            block = st.enter_context(nc.Block())

            def semof(key):
                return dsem[key[1]] if isinstance(key, tuple) else esem[key]

            def replay(ename):
                def f(e):
                    for it in self.streams[ename]:
                        if it[0] == "wait":
                            e.wait_ge(semof(it[1]), it[2])
                        elif it[0] == "op":
                            it[1](e).then_inc(esem[ename], 1)
                        else:
                            _, out, in_, s, kw = it
                            e.dma_start(out=out, in_=in_, **kw).then_inc(dsem[s], 16)
                return f

            block.tensor(replay("pe"))
            block.scalar(replay("act"))
            block.vector(replay("dve"))
            block.gpsimd(replay("pool"))
            block.sync(replay("sp"))


class Ctx:
    def __init__(self):
        self.nc = bass.Bass("TRN2", target_bir_lowering=False)
        self.st = ExitStack()
        self.P = Prog(self.nc)

    def din(self, name, shape, dt=F32):
        return self.nc.dram_tensor(name, list(shape), dt, kind="ExternalInput").ap()

    def sbn(self, name, shape, dt=F32):
        return self.sb("s_" + name, shape, dt)

    def dout(self, name, shape, dt=F32):
        return self.nc.dram_tensor(name, list(shape), dt, kind="ExternalOutput").ap()

    def sb(self, name, shape, dt=F32):
        return self.st.enter_context(self.nc.sbuf_tensor("sb_" + name, list(shape), dt))

    def ps(self, name, shape, dt=F32):
        return self.st.enter_context(self.nc.psum_tensor("ps_" + name, list(shape), dt))

    def finish(self):
        self.P.emit()
        self.st.close()
        return self.nc


def run(nc, in_maps):
    res = run_bass_kernel_spmd(nc, in_maps, core_ids=list(range(NCORES)))
    return res.results


def build_k0():
    C = Ctx()
    P = C.P
    c_in = C.din("c", [128, 16])
    w = C.din("w", [DEPTH, D, 1536])
    b = C.din("b", [DEPTH, 1536])
    out = C.dout("out", [DEPTH, 1536])
    craw = C.sb("craw", [128, 16])
    sc = C.sb("sc", [128, 16])
    wt = [C.sb("wt%d" % i, [128, 16, 512]) for i in range(2)]
    bt = [C.sb("bt%d" % i, [1, 512]) for i in range(2)]
    ot = [C.sb("ot%d" % i, [1, 512]) for i in range(2)]
    pp = [C.ps("pp%d" % i, [1, 512]) for i in range(2)]
    P.dma(craw[:], c_in, writes=["craw"])
    P.op("act", lambda e: e.activation(out=sc[:], in_=craw[:], func=AF.Silu), reads=["craw"], writes=["sc"])
    it = 0
    for l in range(DEPTH):
        wl = w[l].rearrange("(kc p) n -> p kc n", p=128)
        for n in range(3):
            k = it % 2
            it += 1
            P.dma(wt[k][:], wl[:, :, n * 512:(n + 1) * 512], writes=[("wt", k)])
            P.dma(bt[k][:], b[l:l + 1, n * 512:(n + 1) * 512], writes=[("bt", k)])
            for kc in range(16):
                P.op("pe", lambda e, k=k, kc=kc: e.matmul(pp[k][:], lhsT=sc[:, kc:kc + 1], rhs=wt[k][:, kc, :],
                                                          start=(kc == 0), stop=(kc == 15)),
                     reads=["sc", ("wt", k)], writes=[("pp", k)])
            P.op("dve", lambda e, k=k: e.tensor_tensor(out=ot[k][:], in0=pp[k][:], in1=bt[k][:], op=ALU.add),
                 reads=[("pp", k), ("bt", k)], writes=[("ot", k)])
            P.dma(out[l:l + 1, n * 512:(n + 1) * 512], ot[k][:], reads=[("ot", k)], is_output=True)
    return C.finish()


def run_k0(c, ada_w, ada_b):
    nc = build_k0()
    cin = np.ascontiguousarray(c.reshape(16, 128).T)
    in_maps = []
    for i in range(NCORES):
        sl = slice(1536 * i, 1536 * (i + 1))
        in_maps.append({"c": cin, "w": np.ascontiguousarray(ada_w[:, :, sl]),
                        "b": np.ascontiguousarray(ada_b[:, sl])})
    res = run(nc, in_maps)
    return np.concatenate([r["out"] for r in res], axis=1)


ORIG_SPLITS = (512, 512, 1024, 256, 256, 256, 256, 256, 256, 24, 512, 512, 512, 512)
ORIG_OFF = np.concatenate([[0], np.cumsum(ORIG_SPLITS)])
NEW_ORDER = (0, 1, 2, 3, 4, 5, 6, 7, 8, 10, 11, 12, 13, 9)
COL_PERM = np.concatenate([np.arange(ORIG_OFF[j], ORIG_OFF[j + 1]) for j in NEW_ORDER])
O_CV, O_CG, O_NQ, O_KC, O_VC, O_KS, O_VS, O_KW, O_VW, O_RQ, O_RK, O_RV, O_RG, O_GT = (
    0, 512, 1024, 2048, 2304, 2560, 2816, 3072, 3328, 3584, 4096, 4608, 5120, 5632)
CH_KIND = ["plain", "plain", "nsa4", "nsa4", "nsa2", "nsa2", "nsa2", "retq", "retk", "plain", "plain", "gate"]


def rot_tables():
    pos = np.arange(S, dtype=np.float32)
    invn = np.power(np.float32(500000.0), -np.arange(16, dtype=np.float32) * np.float32(2.0) / np.float32(32))
    angn = pos[:, None] * invn[None, :].astype(np.float32)
    invr = np.power(np.float32(10000.0), -np.arange(64, dtype=np.float32) * np.float32(2.0) / np.float32(128))
    angr = pos[:, None] * invr[None, :].astype(np.float32)
    return (np.cos(angn).astype(np.float32), np.sin(angn).astype(np.float32),
            np.cos(angr).astype(np.float32), np.sin(angr).astype(np.float32))


def ret_consts():
    lg = np.log(1.0 - 2.0 ** (-5.0 - np.arange(4, dtype=np.float64)))
    return lg


def ret_ztab():
    lg = ret_consts()
    i = (np.arange(S) % 128).astype(np.float64)
    zq = np.exp(lg[None, :] * (i[:, None] - 127.0))
    zk = np.exp(lg[None, :] * (127.0 - i[:, None])) * (128.0 ** -0.5)
    return np.concatenate([zq, zk], axis=1).astype(np.float32)


def emit_norm_T(C, x_src, A, Sh, hT, ident, ntiles, pfx=""):
    P = C.P
    xt = [C.sb(pfx + "nx%d" % i, [128, D]) for i in range(2)]
    sq = C.sb(pfx + "nsq", [128, D])
    hb = [C.sb(pfx + "nhb%d" % i, [128, D], BF16) for i in range(2)]
    st = [C.sb(pfx + "nst%d" % i, [128, 2]) for i in range(2)]
    pT = C.ps(pfx + "npT", [128, 16, 128], BF16)
    for t in range(ntiles):
        k = t % 2
        P.dma(xt[k][:], x_src(t), writes=[(pfx + "nx", k)])
        P.op("act", lambda e, k=k: e.activation(out=sq[:], in_=xt[k][:], func=AF.Square),
             reads=[(pfx + "nx", k)], writes=[pfx + "nsq"])
        P.op("dve", lambda e, k=k: e.reduce_sum(out=st[k][:, 0:1], in_=sq[:], axis=AX.X),
             reads=[pfx + "nsq"], writes=[(pfx + "nst", k)])
        P.op("act", lambda e, k=k: e.activation(out=st[k][:, 1:2], in_=st[k][:, 0:1], func=AF.Sqrt,
                                                scale=1.0 / D, bias=EPS),
             reads=[(pfx + "nst", k)], writes=[(pfx + "nst", k)])
        P.op("dve", lambda e, k=k: e.reciprocal(out=st[k][:, 0:1], in_=st[k][:, 1:2]),
             reads=[(pfx + "nst", k)], writes=[(pfx + "nst", k)])
        P.op("dve", lambda e, k=k: e.scalar_tensor_tensor(out=sq[:], in0=xt[k][:], scalar=st[k][:, 0:1], in1=A[:],
                                                          op0=ALU.mult, op1=ALU.mult),
             reads=[(pfx + "nx", k), (pfx + "nst", k), "A"], writes=[pfx + "nsq"])
        P.op("dve", lambda e, k=k: e.tensor_tensor(out=hb[k][:], in0=sq[:], in1=Sh[:], op=ALU.add),
             reads=[pfx + "nsq", "Sh"], writes=[(pfx + "nhb", k)])
        for kc in range(16):
            P.op("pe", lambda e, k=k, kc=kc: e.transpose(out=pT[:, kc, :], in_=hb[k][:, kc * 128:(kc + 1) * 128],
                                                         identity=ident[:]),
                 reads=[(pfx + "nhb", k), "ident"], writes=[pfx + "npT"])
        P.op("act", lambda e, t=t: e.copy(out=hT[t][:], in_=pT[:]), reads=[pfx + "npT"], writes=[("hT", t)])


def build_k1(NT=8):
    C = Ctx()
    P = C.P
    x = C.din("x", [NT * 128, D])
    w = C.din("w", [D, INW])
    modA = C.din("modA", [128, D])
    modS = C.din("modS", [128, D])
    gN = C.din("gN", [128, D])
    identd = C.din("ident", [128, 128], BF16)
    tabn = C.din("tabn", [2, NT * 128, 16])
    tabr = C.din("tabr", [2, NT * 128, 64])
    ztd = C.din("zt", [NT * 128, 8])
    o32 = C.dout("o32", [NT * 128, INW])
    o16 = C.dout("o16", [NT * 128, INW], BF16)

    ident = C.sb("ident", [128, 128], BF16)
    A = C.sb("A", [128, D])
    Sh = C.sb("Sh", [128, D])
    tn = C.sb("tn", [128, 2, NT, 16])
    tr = C.sb("tr", [128, 2, NT, 64])
    zt = C.sb("zt", [128, NT, 8])
    hT = [C.sb("hT%d" % t, [128, 16, 128], BF16) for t in range(NT)]
    P.dma(ident[:], identd, writes=["ident"])
    for ci in range(2):
        P.dma(tn[:, ci], tabn[ci].rearrange("(t p) f -> p t f", p=128), writes=["tn"])
    P.dma(zt[:], ztd.rearrange("(t p) f -> p t f", p=128), writes=["zt"])
    for ci in range(2):
        P.dma(tr[:, ci], tabr[ci].rearrange("(t p) f -> p t f", p=128), writes=["tr"])
    gtmp = C.sb("gtmp", [128, D])
    P.dma(A[:], modA, writes=["A"])
    P.dma(gtmp[:], gN, writes=["gtmp"])
    P.dma(Sh[:], modS, writes=["Sh"])
    P.op("dve", lambda e: e.scalar_tensor_tensor(out=A[:], in0=A[:], scalar=1.0, in1=gtmp[:], op0=ALU.add, op1=ALU.mult),
         reads=["gtmp"], writes=["A"])
    emit_norm_T(C, lambda t: x[t * 128:(t + 1) * 128, :], A, Sh, hT, ident, NT)

    wst = [C.sb("wst%d" % i, [128, 8, 512]) for i in range(2)]
    wbf = [C.sb("wbf%d" % i, [128, 16, 512], BF16) for i in range(2)]
    ob32 = [C.sb("ob32_%d" % i, [128, 512]) for i in range(2)]
    ob16 = [C.sb("ob16_%d" % i, [128, 512], BF16) for i in range(2)]
    tmp = [C.sb("rt%d" % i, [128, 4, 64]) for i in range(4)]
    pY = [C.ps("pY%d" % i, [128, 512]) for i in range(2)]
    wv = w.rearrange("(kc p) n -> p kc n", p=128)
    nst = 0
    nev = 0
    def load_w(cc_):
        nonlocal nst
        ncol_ = 512 if CH_KIND[cc_] != "gate" else 24
        wb_ = cc_ % 2
        for half in range(2):
            sbuf = nst % 2
            nst += 1
            P.dma(wst[sbuf][:, :, 0:ncol_], wv[:, half * 8:(half + 1) * 8, cc_ * 512:cc_ * 512 + ncol_], writes=[("wst", sbuf)])
            P.op("pool", lambda e, sbuf=sbuf, wb_=wb_, half=half, ncol_=ncol_: e.tensor_copy(
                out=wbf[wb_][:, half * 8:(half + 1) * 8, 0:ncol_], in_=wst[sbuf][:, :, 0:ncol_]),
                reads=[("wst", sbuf)], writes=[("wbf", wb_, half)])

    load_w(0)
    for cc in range(12):
        kind = CH_KIND[cc]
        ncol = 512 if kind != "gate" else 24
        c0 = cc * 512
        wb = cc % 2
        if cc + 1 < 12:
            load_w(cc + 1)
        for t in range(NT):
            pb = nev % 2
            nev += 1
            for kc in range(16):
                P.op("pe", lambda e, pb=pb, t=t, kc=kc, wb=wb, ncol=ncol: e.matmul(
                    pY[pb][:, 0:ncol], lhsT=hT[t][:, kc, :], rhs=wbf[wb][:, kc, 0:ncol], start=(kc == 0), stop=(kc == 15)),
                    reads=[("hT", t), ("wbf", wb, kc // 8)], writes=[("pY", pb)])
            ps = pY[pb]
            o3 = ob32[pb]
            o1 = ob16[pb]
            R = []
            PSX = [("pY", pb)]
            W32 = [("ob32", pb)]
            if kind in ("plain", "gate"):
                P.op("act", lambda e, ps=ps, o3=o3, ncol=ncol: e.copy(out=o3[:, 0:ncol], in_=ps[:, 0:ncol]), reads=R, writes=W32 + PSX)
            elif kind in ("nsa4", "nsa2"):
                nh = 4 if kind == "nsa4" else 2
                P.op("act", lambda e, ps=ps, o3=o3: e.copy(out=o3[:], in_=ps[:]), reads=R, writes=W32 + PSX)
                psv = ps[:].rearrange("p (h d) -> p h d", d=128)
                o3v = o3[:].rearrange("p (h d) -> p h d", d=128)
                cs = tn[:, 0, t:t + 1, :].to_broadcast([128, nh, 16])
                sn = tn[:, 1, t:t + 1, :].to_broadcast([128, nh, 16])
                x1 = psv[:, 0:nh, 0:16]
                x2 = psv[:, 0:nh, 16:32]
                tv = [tm[:, 0:nh, 0:16] for tm in tmp]
                for (dst, a, tb) in ((tv[0], x1, cs), (tv[1], x2, sn), (tv[2], x2, cs), (tv[3], x1, sn)):
                    P.op("dve", lambda e, dst=dst, a=a, tb=tb: e.tensor_tensor(out=dst, in0=a, in1=tb, op=ALU.mult),
                         reads=R + ["tn"], writes=["rtmp"] + PSX)
                P.op("dve", lambda e, o3v=o3v, tv=tv, nh=nh: e.tensor_tensor(out=o3v[:, 0:nh, 0:16], in0=tv[0], in1=tv[1], op=ALU.subtract),
                     reads=["rtmp"], writes=W32)
                P.op("dve", lambda e, o3v=o3v, tv=tv, nh=nh: e.tensor_tensor(out=o3v[:, 0:nh, 16:32], in0=tv[2], in1=tv[3], op=ALU.add),
                     reads=["rtmp"], writes=W32)
            else:
                ci = 0
                zo = 0 if kind == "retq" else 4
                psv = ps[:].rearrange("p (h d) -> p h d", d=128)
                o3v = o3[:].rearrange("p (h d) -> p h d", d=128)
                cs = tr[:, ci, t:t + 1, :].to_broadcast([128, 4, 64])
                sn = tr[:, ci + 1, t:t + 1, :].to_broadcast([128, 4, 64])
                x1 = psv[:, :, 0:64]
                x2 = psv[:, :, 64:128]
                tv = [tm[:] for tm in tmp]
                for (dst, a, tb) in ((tv[0], x1, cs), (tv[1], x2, sn), (tv[2], x2, cs), (tv[3], x1, sn)):
                    P.op("dve", lambda e, dst=dst, a=a, tb=tb: e.tensor_tensor(out=dst, in0=a, in1=tb, op=ALU.mult),
                         reads=R + ["tr"], writes=["rtmp"] + PSX)
                P.op("dve", lambda e, o3v=o3v, tv=tv: e.tensor_tensor(out=o3v[:, :, 0:64], in0=tv[0], in1=tv[1], op=ALU.subtract),
                     reads=["rtmp"], writes=W32)
                P.op("dve", lambda e, o3v=o3v, tv=tv: e.tensor_tensor(out=o3v[:, :, 64:128], in0=tv[2], in1=tv[3], op=ALU.add),
                     reads=["rtmp"], writes=W32)
                zb = zt[:, t, zo:zo + 4].unsqueeze(2).to_broadcast([128, 4, 128])
                P.op("dve", lambda e, o3v=o3v, zb=zb: e.tensor_tensor(out=o3v, in0=o3v, in1=zb, op=ALU.mult),
                     reads=["zt"], writes=W32)
            P.op("dve", lambda e, o3=o3, o1=o1, ncol=ncol: e.tensor_copy(out=o1[:, 0:ncol], in_=o3[:, 0:ncol]),
                 reads=W32, writes=[("ob16", pb)])
            P.dma(o32[t * 128:(t + 1) * 128, c0:c0 + ncol], o3[:, 0:ncol], reads=W32, is_output=True)
            P.dma(o16[t * 128:(t + 1) * 128, c0:c0 + ncol], o1[:, 0:ncol], reads=[("ob16", pb)], is_output=True)
    return C.finish()


def bc(v):
    return np.ascontiguousarray(np.broadcast_to(np.asarray(v, np.float32)[None, :], (128, v.shape[0])))


def run_k1(x2d, w_in_l, mod_l, g_l, tabs):
    nc = build_k1()
    cn, sn, cr, sr = tabs
    sc = np.float32(128 ** -0.5)
    wre = np.ascontiguousarray(w_in_l[:, COL_PERM])
    ident = np.eye(128, dtype=np.float32).astype(ml_dtypes.bfloat16)
    in_maps = []
    for i in range(NCORES):
        sl = slice(1024 * i, 1024 * (i + 1))
        in_maps.append({
            "x": np.ascontiguousarray(x2d[sl]), "w": wre,
            "modA": bc(mod_l[D:2 * D]), "modS": bc(mod_l[0:D]), "gN": bc(g_l), "ident": ident,
            "tabn": np.ascontiguousarray(np.stack([cn[sl], sn[sl]])),
            "tabr": np.ascontiguousarray(np.stack([cr[sl], sr[sl]])), "zt": np.ascontiguousarray(ret_ztab()[sl]),
        })
    res = run(nc, in_maps)
    return (np.concatenate([r["o32"] for r in res], axis=0), np.concatenate([r["o16"] for r in res], axis=0))


SCALE = 128 ** -0.5


def build_k2a(NS=8):
    SU = NS * 1024
    NKT = SU // 128
    NCMP = (SU - 32) // 16 + 1
    NCH = (NCMP + 127) // 128
    C = Ctx()
    P = C.P
    identd = C.din("ident", [128, 128], BF16)
    ident32d = C.din("ident32", [128, 128])
    cvh = C.din("cvh", [128, 4, NS, 158])
    cgh = C.din("cgh", [128, 4, NS, 158])
    dwd = C.din("dw", [128, 4, 32])
    lngd = C.din("lng", [128, 512])
    lnbd = C.din("lnb", [128, 512])
    kzd = C.din("kz", [SU, 512], BF16)
    rvd = C.din("rv", [SU, 512], BF16)
    qpTd = C.din("qpT", [128, NS, 4, 128], BF16)
    kzTd = C.din("kzT", [128, NS, 4, 128], BF16)
    rgd = C.din("rg", [NS * 128, 512])
    rvod = C.din("rvo", [NS * 128, 512], BF16)
    gngd = C.din("gng", [128, 512])
    gnbd = C.din("gnb", [128, 512])
    decd = C.din("dec", [128, 512])
    trid = C.din("tri", [128, 128], BF16)
    indd = C.din("ind", [128, 8])
    kcmpTd = C.din("kcmpT", [2, 2, 128, SU], BF16)
    w1d = C.din("w1", [2, 4096, 128])
    w2d = C.din("w2", [2, 128, 128])
    peTd = C.din("peT", [2, 128, 32])
    qTd = C.din("qT", [128, NS, 8, 128], BF16)
    ksTd = C.din("ksT", [2, 128, SU], BF16)
    vsd = C.din("vs", [SU, 256], BF16)
    kwTd = C.din("kwT", [128, NS, 2, 640], BF16)
    vwd = C.din("vw", [128, NS, 2, 5, 128], BF16)
    gtd = C.din("gt", [NS * 128, 24])
    coverd = C.din("cover", [128, NCH, 128], BF16)
    nt16d = C.din("nt16", [128, NCH, 128])
    b64d = C.din("b64", [128, 128])
    fmd = C.din("fm", [128, 128])
    f0d = C.din("f0", [128, 128])
    q0d = C.din("q0c", [128, 2, NS])
    cmaskd = C.din("cmask", [128, 8, 128], BF16)
    wmaskd = C.din("wmask", [128, NS, 5, 128], BF16)
    ycat = C.dout("ycat", [NS * 128, 2048])

    ident = C.sb("ident", [128, 128], BF16)
    ident32 = C.sb("ident32", [128, 128])
    ones = C.sb("ones", [128, 1], BF16)
    P.dma(ident[:], identd, writes=["ident"])
    P.dma(ident32[:], ident32d, writes=["ident32"])
    P.op("pool", lambda e: e.memset(ones[:], 1.0), writes=["ones"])

    NB = 8
    bank = [C.ps("bk%d" % i, [128, 512]) for i in range(NB)]

    def bk(i):
        return ("bk", i)

    kcT = C.sb("kcT", [128, 2, NCH * 128], BF16)
    vc = C.sb("vc", [128, 2, NCH, 128], BF16)
    P.op("pool", lambda e: e.memset(kcT[:], 0.0), writes=["kcT"])
    P.op("pool", lambda e: e.memset(vc[:], 0.0), writes=["vc"])
    with ExitStack() as ph1:
        def sb1(name, shape, dt=F32):
            return ph1.enter_context(C.nc.sbuf_tensor("sb_" + name, list(shape), dt))
        w1s = sb1("w1s", [128, 32, 128])
        w1b = sb1("w1b", [128, 32, 128], BF16)
        w2s = sb1("w2s", [128, 128])
        w2b = sb1("w2b", [128, 128], BF16)
        pes = sb1("pes", [128, 32])
        peb = sb1("peb", [128, 32], BF16)
        bia = sb1("bia", [128, 1])
        xT = sb1("xT", [128, SU], BF16)
        a1 = sb1("a1", [128, NCH * 128], BF16)
        P.op("pool", lambda e: e.memset(a1[:], 0.0), writes=["a1"])
        for kv in range(2):
            P.dma(w1s[:], w1d[kv].rearrange("(l d) o -> d l o", d=128), writes=["w1s"])
            P.dma(w2s[:], w2d[kv], writes=["w2s"])
            P.dma(pes[:], peTd[kv], writes=["pes"])
            P.op("pool", lambda e: e.tensor_copy(out=w1b[:], in_=w1s[:]), reads=["w1s"], writes=["w1b"])
            P.op("pool", lambda e: e.tensor_copy(out=w2b[:], in_=w2s[:]), reads=["w2s"], writes=["w2b"])
            P.op("pool", lambda e: e.tensor_copy(out=peb[:], in_=pes[:]), reads=["pes"], writes=["peb"])
            for l in range(32):
                P.op("pe", lambda e, l=l: e.matmul(bank[0][:, 0:1], lhsT=w1b[:, l, :], rhs=peb[:, l:l + 1],
                                                   start=(l == 0), stop=(l == 31)),
                     reads=["w1b", "peb"], writes=[bk(0)])
            P.op("act", lambda e: e.copy(out=bia[:], in_=bank[0][:, 0:1]), writes=["bia", bk(0)])
            for hd in range(2):
                P.dma(xT[:], kcmpTd[kv, hd], writes=["xT"])
                for l in range(32):
                    P.op("pe", lambda e, l=l: e.matmul(bank[1][:, 0:NCMP], lhsT=w1b[:, l, :],
                                                       rhs=xT[:, l:l + 16 * (NCMP - 1) + 1:16],
                                                       start=(l == 0), stop=(l == 31)),
                         reads=["w1b", "xT"], writes=[bk(1)])
                P.op("act", lambda e: e.activation(out=a1[:, 0:NCMP], in_=bank[1][:, 0:NCMP], func=AF.Silu, bias=bia[:, 0:1]),
                     reads=["bia"], writes=["a1", bk(1)])
                if kv == 0:
                    P.op("pe", lambda e: e.matmul(bank[2][:, 0:NCMP], lhsT=w2b[:], rhs=a1[:, 0:NCMP], start=True, stop=True),
                         reads=["w2b", "a1"], writes=[bk(2)])
                    P.op("act", lambda e, hd=hd: e.copy(out=kcT[:, hd, 0:NCMP], in_=bank[2][:, 0:NCMP]), writes=["kcT", bk(2)])
                else:
                    for ch in range(NCH):
                        P.op("pe", lambda e, ch=ch: e.matmul(bank[2][:, ch * 128:(ch + 1) * 128], lhsT=a1[:, ch * 128:(ch + 1) * 128],
                                                             rhs=w2b[:], start=True, stop=True),
                             reads=["w2b", "a1"], writes=[bk(2)])
                    P.op("act", lambda e, hd=hd: e.copy(out=vc[:, hd, :, :], in_=bank[2][:, 0:NCH * 128].rearrange("p (c d) -> p c d", d=128)),
                         writes=["vc", bk(2)])
        P.barrier()
    Tst = C.sb("Tst", [128, 512])
    Tacc = C.sb("Tacc", [128, NS, 512])
    Tb = C.sb("Tb", [128, NS, 512], BF16)
    dec = C.sb("dec", [128, 512])
    ind = C.sb("ind", [128, 8])
    kzt = [C.sb("kzt%d" % i, [128, 512], BF16) for i in range(2)]
    rvt = [C.sb("rvt%d" % i, [128, 512], BF16) for i in range(2)]
    P.dma(dec[:], decd, writes=["dec"])
    P.dma(ind[:], indd, writes=["ind"])
    P.op("pool", lambda e: e.memset(Tst[:], 0.0), writes=["Tst"])
    P.op("pool", lambda e: e.memset(Tacc[:], 0.0), writes=["Tacc"])
    for m in range(NKT):
        k = m % 2
        j = m // 8
        P.op("dve", lambda e, j=j, m=m: e.scalar_tensor_tensor(out=Tacc[:, j, :], in0=Tst[:], scalar=ind[:, m % 8:m % 8 + 1],
                                                              in1=Tacc[:, j, :], op0=ALU.mult, op1=ALU.add),
             reads=["Tst", "ind"], writes=["Tacc"])
        if m == NKT - 1:
            break
        P.dma(kzt[k][:], kzd[m * 128:(m + 1) * 128, :], writes=[("kzt", k)])
        P.dma(rvt[k][:], rvd[m * 128:(m + 1) * 128, :], writes=[("rvt", k)])
        for h in range(4):
            P.op("pe", lambda e, k=k, h=h: e.matmul(bank[3][:, h * 128:(h + 1) * 128], lhsT=kzt[k][:, h * 128:(h + 1) * 128],
                                                    rhs=rvt[k][:, h * 128:(h + 1) * 128], start=True, stop=True),
                 reads=[("kzt", k), ("rvt", k)], writes=[bk(3)])
        P.op("dve", lambda e: e.tensor_tensor(out=Tst[:], in0=Tst[:], in1=bank[3][:], op=ALU.add), writes=["Tst", bk(3)])
        P.op("dve", lambda e: e.tensor_tensor(out=Tst[:], in0=Tst[:], in1=dec[:], op=ALU.mult), reads=["dec"], writes=["Tst"])
    P.op("act", lambda e: e.copy(out=Tb[:], in_=Tacc[:]), reads=["Tacc"], writes=["Tb"])

    ksT = C.sb("ksT", [128, 2, SU], BF16)
    vs = C.sb("vs", [128, NKT, 256], BF16)
    for g in range(2):
        P.dma(ksT[:, g, :], ksTd[g], writes=["ksT"])
    for c4 in range(0, NKT, 16):
        n4 = min(16, NKT - c4)
        P.dma(vs[:, c4:c4 + n4, :], vsd[c4 * 128:(c4 + n4) * 128, :].rearrange("(t p) f -> p t f", p=128), writes=["vs"])
    cover = C.sb("cover", [128, NCH, 128], BF16)
    nt16 = C.sb("nt16", [128, NCH, 128])
    b64 = C.sb("b64", [128, 128])
    fm = C.sb("fm", [128, 128])
    f0 = C.sb("f0", [128, 128])
    q0c = C.sb("q0c", [128, 2, NS])
    cmask = C.sb("cmask", [128, 8, 128], BF16)
    wmask = C.sb("wmask", [128, NS, 5, 128], BF16)
    tri = C.sb("tri", [128, 128], BF16)
    dw = C.sb("dw", [128, 4, 32])
    lng = C.sb("lng", [128, 512])
    lnb = C.sb("lnb", [128, 512])
    gng = C.sb("gng", [128, 512])
    gnb = C.sb("gnb", [128, 512])
    for (t_, d_, nm) in ((cover, coverd, "cover"), (nt16, nt16d, "nt16"), (b64, b64d, "b64"), (fm, fmd, "fm"), (f0, f0d, "f0"),
                         (q0c, q0d, "q0c"), (cmask, cmaskd, "cmask"), (wmask, wmaskd, "wmask"), (tri, trid, "tri"), (dw, dwd, "dw"),
                         (lng, lngd, "lng"), (lnb, lnbd, "lnb"), (gng, gngd, "gng"), (gnb, gnbd, "gnb")):
        P.dma(t_[:], d_, writes=[nm])

    yt = C.sb("yt", [128, 2048])
    cv = C.sb("cv", [128, 4, 158])
    cg = C.sb("cg", [128, 4, 158])
    cacc = C.sb("cacc", [128, 4, 128])
    w512 = [C.sb("w512_%d" % i, [128, 512]) for i in range(3)]
    st8 = C.sb("st8", [128, 16])
    qpT = C.sb("qpT", [128, 4, 128], BF16)
    kzT = C.sb("kzT", [128, 4, 128], BF16)
    rg = C.sb("rg", [128, 512])
    rvo = C.sb("rvo", [128, 512], BF16)
    innT = C.sb("innT", [128, 4, 128], BF16)
    qT = C.sb("qT", [128, 8, 128], BF16)
    kwT = C.sb("kwT", [128, 2, 640], BF16)
    vw = C.sb("vw", [128, 2, 5, 128], BF16)
    gt = C.sb("gt", [128, 24])
    gsg = C.sb("gsg", [128, 24])
    eT = [C.sb("eT%d" % i, [128, 512]) for i in range(2)]
    eTm = [C.sb("eTm%d" % i, [128, 4, 128], BF16) for i in range(2)]
    mk = C.sb("mk", [128, NCH, 128], BF16)
    m2 = C.sb("m2", [128, 128], BF16)
    Et = [C.sb("Et%d" % i, [128, 128], BF16) for i in range(2)]
    imp = C.sb("imp", [128, 128])
    impw = C.sb("impw", [128, 128])
    vld = C.sb("vld", [128, 128])
    sel = C.sb("sel", [128, 128], BF16)
    selT = C.sb("selT", [128, 128], BF16)
    mx8 = C.sb("mx8", [128, 16])
    lrec = C.sb("lrec", [128, 8])
    ynsa = C.sb("ynsa", [128, 4, 128])
    B_S, B_O, B_L, B_M, B_X = 4, 5, 6, 7, 3
    sbanks = [4, 0]
    nev = [0]

    mbanks = [B_M, 1]

    def stage1(it):
        b = nev[0] % 2
        nev[0] += 1
        if it.get("pre") is not None:
            it["pre"](b)
        sb_ = sbanks[b]
        P.op("pe", lambda e, sb_=sb_, lhsT=it["lhsT"], qg=it["qg"]: e.matmul(bank[sb_][:], lhsT=lhsT, rhs=qg, start=True, stop=True),
             reads=["qT"] + it["rd"], writes=[bk(sb_)])
        P.op("act", lambda e, b=b, sb_=sb_: e.activation(out=eT[b][:], in_=bank[sb_][:], func=AF.Exp, scale=SCALE),
             writes=[("eT", b), bk(sb_)])
        it["mask_fn"](b)
        return b

    def stage2(it, b, first, last):
        vfn = it["vfn"]
        for h in range(4):
            P.op("pe", lambda e, b=b, h=h, vfn=vfn: e.matmul(bank[B_O][:, h * 128:(h + 1) * 128], lhsT=eTm[b][:, h, :], rhs=vfn(),
                                                             start=(first and h == 0), stop=last, skip_group_check=True),
                 reads=[("eTm", b)] + it["extra"], writes=[bk(B_O)])
        for h in range(4):
            P.op("pe", lambda e, b=b, h=h: e.matmul(bank[B_L][:, h:h + 1], lhsT=eTm[b][:, h, :], rhs=ones[:],
                                                    start=(first and h == 0), stop=last, skip_group_check=True),
                 reads=[("eTm", b), "ones"], writes=[bk(B_L)])
        if it.get("post") is not None:
            it["post"](b, first, last)

    def pipeline(items):
        n = len(items)
        bs = [None] * n
        bs[0] = stage1(items[0])
        for i_ in range(n):
            if i_ + 1 < n:
                bs[i_ + 1] = stage1(items[i_ + 1])
            stage2(items[i_], bs[i_], i_ == 0, i_ == n - 1)

    def finish_branch(g, br, first_branch):
        P.op("dve", lambda e: e.tensor_scalar(out=lrec[:, 0:4], in0=bank[B_L][:, 0:4], scalar1=1e-30, scalar2=None, op0=ALU.max),
             writes=["lrec", bk(B_L)])
        P.op("dve", lambda e: e.reciprocal(out=lrec[:, 4:8], in_=lrec[:, 0:4]), writes=["lrec"])
        P.op("dve", lambda e: e.tensor_tensor(out=lrec[:, 0:4], in0=lrec[:, 4:8], in1=gsg[:, br * 8 + 4 * g:br * 8 + 4 * g + 4], op=ALU.mult),
             reads=["gsg"], writes=["lrec"])
        wb = lrec[:, 0:4].unsqueeze(2).to_broadcast([128, 4, 128])
        ov = bank[B_O][:].rearrange("p (h d) -> p h d", d=128)
        if first_branch:
            P.op("dve", lambda e: e.tensor_tensor(out=ynsa[:], in0=ov, in1=wb, op=ALU.mult), reads=["lrec"], writes=["ynsa", bk(B_O)])
        else:
            tv = w512[0][:].rearrange("p (h d) -> p h d", d=128)
            P.op("dve", lambda e: e.tensor_tensor(out=tv, in0=ov, in1=wb, op=ALU.mult), reads=["lrec"], writes=[("w512", 0), bk(B_O)])
            P.op("dve", lambda e: e.tensor_tensor(out=ynsa[:], in0=ynsa[:], in1=tv, op=ALU.add), reads=[("w512", 0)], writes=["ynsa"])

    def layer_norm_free(src_tag, x3, nh, dd, gam, bet, out3, out_tag, gtag, btag):
        inv = 1.0 / dd
        P.op("dve", lambda e: e.reduce_sum(out=st8[:, 0:nh], in_=x3, axis=AX.X), reads=[src_tag], writes=["st8"])
        P.op("dve", lambda e: e.tensor_scalar(out=st8[:, 0:nh], in0=st8[:, 0:nh], scalar1=-inv, scalar2=None, op0=ALU.mult), writes=["st8"])
        mb = st8[:, 0:nh].unsqueeze(2).to_broadcast([128, nh, dd])
        P.op("dve", lambda e: e.tensor_tensor(out=x3, in0=x3, in1=mb, op=ALU.add), reads=["st8"], writes=[src_tag])
        sq3 = w512[1][:, 0:nh * dd].rearrange("p (h d) -> p h d", d=dd)
        P.op("act", lambda e: e.activation(out=sq3, in_=x3, func=AF.Square), reads=[src_tag], writes=[("w512", 1)])
        P.op("dve", lambda e: e.reduce_sum(out=st8[:, 4:4 + nh], in_=sq3, axis=AX.X), reads=[("w512", 1)], writes=["st8"])
        P.op("act", lambda e: e.activation(out=st8[:, 8:8 + nh], in_=st8[:, 4:4 + nh], func=AF.Sqrt, scale=inv, bias=EPS), writes=["st8"])
        P.op("dve", lambda e: e.reciprocal(out=st8[:, 12:12 + nh], in_=st8[:, 8:8 + nh]), writes=["st8"])
        rb = st8[:, 12:12 + nh].unsqueeze(2).to_broadcast([128, nh, dd])
        P.op("dve", lambda e: e.tensor_tensor(out=x3, in0=x3, in1=rb, op=ALU.mult), reads=["st8"], writes=[src_tag])
        g3 = gam[:, 0:nh * dd].rearrange("p (h d) -> p h d", d=dd)
        b3 = bet[:, 0:nh * dd].rearrange("p (h d) -> p h d", d=dd)
        P.op("dve", lambda e: e.tensor_tensor(out=x3, in0=x3, in1=g3, op=ALU.mult), reads=[gtag], writes=[src_tag])
        P.op("dve", lambda e: e.tensor_tensor(out=out3, in0=x3, in1=b3, op=ALU.add), reads=[src_tag, btag], writes=[out_tag])

    for j in range(NS):
        P.dma(cv[:], cvh[:, :, j, :], writes=["cv"])
        P.dma(cg[:], cgh[:, :, j, :], writes=["cg"])
        P.dma(qpT[:], qpTd[:, j], writes=["qpT"])
        P.dma(kzT[:], kzTd[:, j], writes=["kzT"])
        P.dma(rg[:], rgd[j * 128:(j + 1) * 128, :], writes=["rg"])
        P.dma(qT[:], qTd[:, j], writes=["qT"])
        P.dma(kwT[:], kwTd[:, j], writes=["kwT"])
        P.dma(vw[:], vwd[:, j], writes=["vw"])
        P.dma(gt[:], gtd[j * 128:(j + 1) * 128, :], writes=["gt"])
        P.op("act", lambda e: e.activation(out=cg[:], in_=cg[:], func=AF.Sigmoid), writes=["cg"])
        P.op("dve", lambda e: e.tensor_tensor(out=cv[:], in0=cv[:], in1=cg[:], op=ALU.mult), reads=["cg"], writes=["cv"])
        for ch in range(4):
            en = "dve"
            tg = ("cacc", ch)
            P.op(en, lambda e, ch=ch: e.tensor_scalar(out=cacc[:, ch, :], in0=cv[:, ch, 0:128], scalar1=dw[:, ch, 0:1],
                                                      scalar2=dw[:, ch, 31:32], op0=ALU.mult, op1=ALU.add),
                 reads=["cv", "dw"], writes=[tg])
            for w in range(1, 31):
                P.op(en, lambda e, ch=ch, w=w: e.scalar_tensor_tensor(out=cacc[:, ch, :], in0=cv[:, ch, w:w + 128],
                                                                        scalar=dw[:, ch, w:w + 1], in1=cacc[:, ch, :],
                                                                        op0=ALU.mult, op1=ALU.add),
                     reads=["cv", "dw"], writes=[tg])
        for ch in range(4):
            P.op("pe", lambda e, ch=ch: e.transpose(out=bank[B_X][:, ch * 128:(ch + 1) * 128], in_=cacc[:, ch, :], identity=ident32[:]),
                 reads=[("cacc", ch), "ident32"], writes=[bk(B_X)])
        P.op("act", lambda e: e.copy(out=w512[2][:], in_=bank[B_X][:]), writes=[("w512", 2), bk(B_X)])
        x3 = w512[2][:].rearrange("p (h d) -> p h d", d=512)
        layer_norm_free(("w512", 2), x3, 1, 512, lng, lnb, x3, ("w512", 2), "lng", "lnb")
        P.op("act", lambda e: e.activation(out=yt[:, 0:512], in_=w512[2][:], func=AF.Silu), reads=[("w512", 2)], writes=["yt"])
        P.dma(rvo[:], rvod[j * 128:(j + 1) * 128, :], writes=["rvo"])
        for h in range(4):
            P.op("pe", lambda e, h=h: e.matmul(bank[B_X][:, h * 128:(h + 1) * 128], lhsT=kzT[:, h, :], rhs=qpT[:, h, :], start=True, stop=True),
                 reads=["kzT", "qpT"], writes=[bk(B_X)])
        P.op("dve", lambda e: e.tensor_tensor(out=innT[:], in0=bank[B_X][:].rearrange("p (h d) -> p h d", d=128),
                                              in1=tri[:].unsqueeze(1).to_broadcast([128, 4, 128]), op=ALU.mult),
             reads=["tri"], writes=["innT", bk(B_X)])
        for h in range(4):
            P.op("pe", lambda e, h=h: e.matmul(bank[B_O][:, h * 128:(h + 1) * 128], lhsT=innT[:, h, :], rhs=rvo[:, h * 128:(h + 1) * 128],
                                               start=True, stop=False),
                 reads=["innT", "rvo"], writes=[bk(B_O)])
            P.op("pe", lambda e, h=h, j=j: e.matmul(bank[B_O][:, h * 128:(h + 1) * 128], lhsT=qpT[:, h, :], rhs=Tb[:, j, h * 128:(h + 1) * 128],
                                                    start=False, stop=True),
                 reads=["qpT", "Tb"], writes=[bk(B_O)])
        P.op("act", lambda e: e.copy(out=w512[2][:], in_=bank[B_O][:]), writes=[("w512", 2), bk(B_O)])
        x3 = w512[2][:].rearrange("p (h d) -> p h d", d=128)
        layer_norm_free(("w512", 2), x3, 4, 128, gng, gnb, x3, ("w512", 2), "gng", "gnb")
        P.op("act", lambda e: e.activation(out=rg[:], in_=rg[:], func=AF.Silu), writes=["rg"])
        P.op("dve", lambda e: e.tensor_tensor(out=yt[:, 1536:2048], in0=w512[2][:], in1=rg[:], op=ALU.mult),
             reads=[("w512", 2), "rg"], writes=["yt"])
        P.op("act", lambda e: e.activation(out=gsg[:], in_=gt[:], func=AF.Sigmoid), reads=["gt"], writes=["gsg"])
        P.op("dve", lambda e, j=j: e.tensor_scalar(out=mk[:], in0=nt16[:], scalar1=q0c[:, 0, j:j + 1], scalar2=None, op0=ALU.is_le),
             reads=["nt16", "q0c"], writes=["mk"])
        P.op("dve", lambda e, j=j: e.tensor_scalar(out=vld[:], in0=b64[:], scalar1=q0c[:, 0, j:j + 1], scalar2=None, op0=ALU.is_le),
             reads=["b64", "q0c"], writes=["vld"])
        for g in range(2):
            qg = qT[:].rearrange("p h q -> p (h q)")[:, 512 * g:512 * g + 512]

            def mask_sb(mask_ap, tags):
                def f(b):
                    P.op("dve", lambda e, b=b: e.tensor_tensor(out=eTm[b][:], in0=eT[b][:].rearrange("p (h q) -> p h q", q=128),
                                                               in1=mask_ap.unsqueeze(1).to_broadcast([128, 4, 128]), op=ALU.mult),
                         reads=[("eT", b)] + tags, writes=[("eTm", b)])
                return f

            def imp_post(ch):
                def f(b, first, last):
                    for h in range(4):
                        P.op("pe", lambda e, b=b, h=h: e.matmul(bank[B_M][:, h * 128:(h + 1) * 128], lhsT=eTm[b][:, h, :], rhs=cover[:, ch, :],
                                                                start=(first and h == 0), stop=last, skip_group_check=True),
                             reads=[("eTm", b), "cover"], writes=[bk(B_M)])
                return f
            pipeline([dict(lhsT=kcT[:, g, ch * 128:(ch + 1) * 128], qg=qg, rd=["kcT"], mask_fn=mask_sb(mk[:, ch, :], ["mk"]),
                           vfn=(lambda g=g, ch=ch: vc[:, g, ch, :]), extra=["vc"], post=imp_post(ch)) for ch in range(NCH)])
            P.op("dve", lambda e: e.tensor_scalar(out=lrec[:, 0:4], in0=bank[B_L][:, 0:4], scalar1=1e-30, scalar2=None, op0=ALU.max),
                 writes=["lrec", bk(B_L)])
            P.op("dve", lambda e: e.reciprocal(out=lrec[:, 4:8], in_=lrec[:, 0:4]), writes=["lrec"])
            P.op("dve", lambda e: e.tensor_scalar(out=imp[:], in0=bank[B_M][:, 0:128], scalar1=lrec[:, 4:5], scalar2=None, op0=ALU.mult),
                 reads=["lrec"], writes=["imp", bk(B_M)])
            for h in range(1, 4):
                P.op("dve", lambda e, h=h: e.scalar_tensor_tensor(out=imp[:], in0=bank[B_M][:, h * 128:(h + 1) * 128], scalar=lrec[:, 4 + h:5 + h],
                                                                   in1=imp[:], op0=ALU.mult, op1=ALU.add),
                     reads=["lrec"], writes=["imp", bk(B_M)])
            finish_branch(g, 0, True)
            P.op("dve", lambda e: e.tensor_scalar(out=impw[:], in0=vld[:], scalar1=1.0, scalar2=1e30, op0=ALU.subtract, op1=ALU.mult),
                 reads=["vld"], writes=["impw"])
            P.op("dve", lambda e: e.tensor_tensor(out=imp[:], in0=imp[:], in1=vld[:], op=ALU.mult), reads=["vld"], writes=["imp"])
            P.op("dve", lambda e: e.tensor_tensor(out=imp[:], in0=imp[:], in1=impw[:], op=ALU.add), reads=["impw"], writes=["imp"])
            P.op("dve", lambda e, j=j: e.tensor_scalar(out=impw[:], in0=fm[:], scalar1=q0c[:, 1, j:j + 1], scalar2=None, op0=ALU.is_equal),
                 reads=["fm", "q0c"], writes=["impw"])
            P.op("dve", lambda e: e.tensor_tensor(out=impw[:], in0=impw[:], in1=f0[:], op=ALU.max), reads=["f0"], writes=["impw"])
            P.op("dve", lambda e: e.scalar_tensor_tensor(out=imp[:], in0=impw[:], scalar=1e30, in1=imp[:], op0=ALU.mult, op1=ALU.max),
                 reads=["impw"], writes=["imp"])
            P.op("dve", lambda e: e.max(out=mx8[:, 0:8], in_=imp[:]), reads=["imp"], writes=["mx8"])
            P.op("dve", lambda e: e.match_replace(out=impw[:], in_to_replace=mx8[:, 0:8], in_values=imp[:], imm_value=-3.0e38),
                 reads=["imp", "mx8"], writes=["impw"])
            P.op("dve", lambda e: e.max(out=mx8[:, 8:16], in_=impw[:]), reads=["impw"], writes=["mx8"])
            P.op("dve", lambda e: e.tensor_scalar(out=impw[:], in0=imp[:], scalar1=mx8[:, 15:16], scalar2=None, op0=ALU.is_ge),
                 reads=["imp", "mx8"], writes=["impw"])
            P.op("dve", lambda e: e.tensor_tensor(out=impw[:], in0=impw[:], in1=vld[:], op=ALU.mult), reads=["vld"], writes=["impw"])
            P.op("pe", lambda e: e.transpose(out=bank[B_M][:, 0:128], in_=impw[:], identity=ident32[:]), reads=["impw", "ident32"], writes=[bk(B_M)])
            P.op("act", lambda e: e.copy(out=selT[:], in_=bank[B_M][:, 0:128]), writes=["selT", bk(B_M)])
            nkt = 8 * j + 8

            def sel_pre(kt):
                def f(b):
                    mb = mbanks[b]
                    P.op("pool", lambda e, b=b: e.tensor_copy(out=Et[b][:].rearrange("p (a k) -> p a k", k=64),
                                                              in_=ident[:, 2 * kt:2 * kt + 2].unsqueeze(2).to_broadcast([128, 2, 64])),
                         reads=["ident"], writes=[("Et", b)])
                    P.op("pe", lambda e, b=b, mb=mb: e.matmul(bank[mb][:, 0:128], lhsT=Et[b][:], rhs=selT[:], start=True, stop=True),
                         reads=[("Et", b), "selT"], writes=[bk(mb)])
                return f

            def sel_mask(kt):
                if kt < 8 * j:
                    def mf(b):
                        mb = mbanks[b]
                        P.op("dve", lambda e, b=b, mb=mb: e.tensor_tensor(out=eTm[b][:], in0=eT[b][:].rearrange("p (h q) -> p h q", q=128),
                                                                          in1=bank[mb][:, 0:128].unsqueeze(1).to_broadcast([128, 4, 128]), op=ALU.mult),
                             reads=[("eT", b)], writes=[("eTm", b), bk(mb)])
                else:
                    def mf(b, o=kt - 8 * j):
                        mb = mbanks[b]
                        P.op("dve", lambda e, mb=mb: e.tensor_tensor(out=m2[:], in0=bank[mb][:, 0:128], in1=cmask[:, o, :], op=ALU.mult),
                             reads=["cmask"], writes=["m2", bk(mb)])
                        P.op("dve", lambda e, b=b: e.tensor_tensor(out=eTm[b][:], in0=eT[b][:].rearrange("p (h q) -> p h q", q=128),
                                                                   in1=m2[:].unsqueeze(1).to_broadcast([128, 4, 128]), op=ALU.mult),
                             reads=[("eT", b), "m2"], writes=[("eTm", b)])
                return mf
            pipeline([dict(lhsT=ksT[:, g, kt * 128:(kt + 1) * 128], qg=qg, rd=["ksT"], pre=sel_pre(kt), mask_fn=sel_mask(kt),
                           vfn=(lambda g=g, kt=kt: vs[:, kt, g * 128:(g + 1) * 128]), extra=["vs"]) for kt in range(nkt)])
            finish_branch(g, 1, False)
            pipeline([dict(lhsT=kwT[:, g, o * 128:(o + 1) * 128], qg=qg, rd=["kwT"], mask_fn=mask_sb(wmask[:, j, o, :], ["wmask"]),
                           vfn=(lambda g=g, o=o: vw[:, g, o, :]), extra=["vw"]) for o in range(5)])
            finish_branch(g, 2, False)
            P.op("act", lambda e, g=g: e.copy(out=yt[:, 512 + 512 * g:1024 + 512 * g], in_=ynsa[:].rearrange("p h d -> p (h d)")),
                 reads=["ynsa"], writes=["yt"])
        P.dma(ycat[j * 128:(j + 1) * 128, :], yt[:], reads=["yt"], is_output=True)
    return C.finish()


def prep_k2a(i, NS, p32, p16, lw):
    SU = NS * 1024
    NCMP = (SU - 32) // 16 + 1
    NCH = (NCMP + 127) // 128
    bf = ml_dtypes.bfloat16
    qbs = [8 * j + i for j in range(NS)]
    own = np.concatenate([np.arange(qb * 128, qb * 128 + 128) for qb in qbs])
    m = {}
    m["ident"] = np.eye(128, dtype=np.float32).astype(bf)
    m["ident32"] = np.eye(128, dtype=np.float32)

    def halo(cols):
        a = np.concatenate([np.zeros((30, 512), np.float32), p32[:, cols:cols + 512]], axis=0)
        out = np.zeros((128, 4, NS, 158), np.float32)
        for j, qb in enumerate(qbs):
            blk = a[qb * 128:qb * 128 + 158].T.reshape(4, 128, 158)
            out[:, :, j, :] = blk.transpose(1, 0, 2)
        return out
    m["cvh"] = halo(O_CV)
    m["cgh"] = halo(O_CG)
    dwt = np.concatenate([lw["conv_dw_w"].T, lw["conv_dw_b"][:, None]], axis=1)
    m["dw"] = np.ascontiguousarray(dwt.reshape(4, 128, 32).transpose(1, 0, 2))
    m["lng"] = bc(lw["conv_ln_g"]); m["lnb"] = bc(lw["conv_ln_b"])
    m["kz"] = np.ascontiguousarray(p16[:SU, O_RK:O_RK + 512])
    m["rv"] = np.ascontiguousarray(p16[:SU, O_RV:O_RV + 512])
    m["rvo"] = np.ascontiguousarray(p16[own, O_RV:O_RV + 512])
    m["qpT"] = np.ascontiguousarray(p16[own, O_RQ:O_RQ + 512].reshape(NS, 128, 4, 128).transpose(3, 0, 2, 1))
    m["kzT"] = np.ascontiguousarray(p16[own, O_RK:O_RK + 512].reshape(NS, 128, 4, 128).transpose(3, 0, 2, 1))
    m["rg"] = np.ascontiguousarray(p32[own, O_RG:O_RG + 512])
    m["gng"] = bc(lw["ret_gn_g"]); m["gnb"] = bc(lw["ret_gn_b"])
    lg = ret_consts()
    m["dec"] = bc(np.repeat(np.exp(lg * 128.0), 128).astype(np.float32))
    kk = np.arange(128)
    m["tri"] = (kk[:, None] <= kk[None, :]).astype(np.float32).astype(bf)
    ind = np.zeros((128, 8), np.float32); ind[:, i] = 1.0
    m["ind"] = ind
    m["kcmpT"] = np.ascontiguousarray(np.stack([
        np.stack([p16[:SU, o + hd * 128:o + hd * 128 + 128].T for hd in range(2)]) for o in (O_KC, O_VC)]))
    m["w1"] = np.ascontiguousarray(np.stack([lw["nsa_cmp_k_w1"], lw["nsa_cmp_v_w1"]]))
    m["w2"] = np.ascontiguousarray(np.stack([lw["nsa_cmp_k_w2"], lw["nsa_cmp_v_w2"]]))
    m["peT"] = np.ascontiguousarray(np.stack([lw["nsa_pe_k"].T, lw["nsa_pe_v"].T]))
    m["qT"] = np.ascontiguousarray(p16[own, O_NQ:O_NQ + 1024].reshape(NS, 128, 8, 128).transpose(3, 0, 2, 1))
    m["ksT"] = np.ascontiguousarray(np.stack([p16[:SU, O_KS + g * 128:O_KS + g * 128 + 128].T for g in range(2)]))
    m["vs"] = np.ascontiguousarray(p16[:SU, O_VS:O_VS + 256])
    kwp = np.concatenate([np.zeros((512, 256), bf), p16[:, O_KW:O_KW + 256]], axis=0)
    vwp = np.concatenate([np.zeros((512, 256), bf), p16[:, O_VW:O_VW + 256]], axis=0)
    kwT = np.zeros((128, NS, 2, 640), bf)
    vw = np.zeros((128, NS, 2, 5, 128), bf)
    wmask = np.zeros((128, NS, 5, 128), np.float32)
    for j, qb in enumerate(qbs):
        q0 = qb * 128
        kwT[:, j] = kwp[q0:q0 + 640].reshape(640, 2, 128).transpose(2, 1, 0)
        vw[:, j] = vwp[q0:q0 + 640].reshape(5, 128, 2, 128).transpose(1, 2, 0, 3)
        for o in range(5):
            kp = q0 - 512 + o * 128 + kk[:, None]
            t = q0 + kk[None, :]
            wmask[:, j, o, :] = ((kp >= 0) & (kp <= t) & (kp > t - 512)).astype(np.float32)
    m["kwT"] = kwT; m["vw"] = vw; m["wmask"] = wmask.astype(bf)
    m["gt"] = np.ascontiguousarray(p32[own, O_GT:O_GT + 24])
    n = (np.arange(NCH)[None, :] * 128 + kk[:, None])
    blk = np.arange(128)
    cov = ((16 * n[:, :, None] <= 64 * blk[None, None, :] + 63) & (16 * n[:, :, None] + 31 >= 64 * blk[None, None, :])
           & (n[:, :, None] < NCMP))
    m["cover"] = cov.astype(np.float32).astype(bf)
    m["nt16"] = (16.0 * n[:, :, None] + 31.0 - kk[None, None, :]).astype(np.float32)
    m["b64"] = (64.0 * blk[None, :] - kk[:, None]).astype(np.float32)
    m["fm"] = (blk[None, :] - (kk[:, None] >= 64)).astype(np.float32)
    f0 = np.zeros((128, 128), np.float32); f0[:, 0] = 2.0
    m["f0"] = f0
    q0c = np.zeros((128, 2, NS), np.float32)
    for j, qb in enumerate(qbs):
        q0c[:, 0, j] = qb * 128; q0c[:, 1, j] = 2 * qb
    m["q0c"] = q0c
    cm = np.zeros((128, 8, 128), np.float32)
    for o in range(8):
        if o < i:
            cm[:, o, :] = 1.0
        elif o == i:
            cm[:, o, :] = (kk[:, None] <= kk[None, :])
    m["cmask"] = cm.astype(bf)
    return m, own


def run_k2a(p32, p16, lw):
    nc = build_k2a(8)
    in_maps, owns = [], []
    for i in range(NCORES):
        m, own = prep_k2a(i, 8, p32, p16, lw)
        in_maps.append(m)
        owns.append(own)
    res = run(nc, in_maps)
    y = np.zeros((S, 2048), np.float32)
    for i in range(NCORES):
        y[owns[i]] = res[i]["ycat"]
    return y


def build_k2b(NT=8):
    C = Ctx()
    P = C.P
    HT = 4 if NT >= 4 else NT
    yd = C.din("y", [NT * 128, D])
    xd = C.din("x", [NT * 128, D])
    wd = C.din("w", [D, D])
    gAd = C.din("gateA", [128, D])
    mAd = C.din("modA", [128, D])
    mSd = C.din("modS", [128, D])
    gNd = C.din("gN", [128, D])
    rwd = C.din("rw", [D, 32])
    rbd = C.din("rb", [128, 32])
    identd = C.din("ident", [128, 128], BF16)
    ident32d = C.din("ident32", [128, 128])
    x1d = C.dout("x1", [NT * 128, D])
    hfTd = C.dout("hfT", [128, 16, NT * 128], BF16)
    Gd = C.dout("G", [NT * 128, 32])

    ident = C.sb("ident", [128, 128], BF16)
    ident32 = C.sb("ident32", [128, 128])
    gA = C.sb("gA", [128, D])
    A = C.sb("A", [128, D])
    Sh = C.sb("Sh", [128, D])
    rw = C.sb("rw", [128, 16, 32])
    rb = C.sb("rb", [128, 32])
    P.dma(ident[:], identd, writes=["ident"])
    P.dma(ident32[:], ident32d, writes=["ident32"])
    P.dma(gA[:], gAd, writes=["gA"])
    P.dma(A[:], mAd, writes=["A"])
    P.dma(Sh[:], mSd, writes=["Sh"])
    P.dma(rw[:], rwd.rearrange("(kc p) n -> p kc n", p=128), writes=["rw"])
    P.dma(rb[:], rbd, writes=["rb"])
    sq = C.sb("sq", [128, D])
    P.dma(sq[:], gNd, writes=["sq"])
    P.op("dve", lambda e: e.scalar_tensor_tensor(out=A[:], in0=A[:], scalar=1.0, in1=sq[:], op0=ALU.add, op1=ALU.mult),
         reads=["sq"], writes=["A"])

    xt = [C.sb("xt%d" % i, [128, D]) for i in range(HT)]
    yT = [C.sb("yT%d" % i, [128, 16, 128], BF16) for i in range(HT)]
    yb = C.sb("yb", [128, D], BF16)
    h32 = C.sb("h32", [128, D])
    hb = C.sb("hb", [128, D], BF16)
    hT = C.sb("hT", [128, 16, 128], BF16)
    hT32 = C.sb("hT32", [128, 16, 128])
    st = C.sb("st", [128, 2])
    wst = [C.sb("wst%d" % i, [128, 8, 512]) for i in range(2)]
    wbf = C.sb("wbf", [128, 16, 512], BF16)
    tmp = [C.sb("tmp%d" % i, [128, 512]) for i in range(2)]
    lg = C.sb("lg", [128, 32])
    ex = C.sb("ex", [128, 32])
    mk = C.sb("mk", [128, 32])
    mx = C.sb("mx", [128, 8])
    s1 = C.sb("s1", [128, 2])
    pT = C.ps("pT", [128, 16, 128], BF16)
    pY = [C.ps("pY%d" % i, [128, 512]) for i in range(2)]
    pF = [C.ps("pF%d" % i, [128, 4, 128]) for i in range(2)]
    pL = C.ps("pL", [128, 512])
    wv = wd.rearrange("(kc p) n -> p kc n", p=128)
    nst = 0
    nev = 0
    wbf2 = [wbf, C.sb("wbfB", [128, 16, 512], BF16)]
    wseq = 0

    def load_w2(seq):
        nonlocal nst
        cc_ = seq % 4
        for hf in range(2):
            sbuf = nst % 2
            nst += 1
            P.dma(wst[sbuf][:], wv[:, hf * 8:(hf + 1) * 8, cc_ * 512:cc_ * 512 + 512], writes=[("wst", sbuf)])
            P.op("pool", lambda e, sbuf=sbuf, hf=hf, seq=seq: e.tensor_copy(out=wbf2[seq % 2][:, hf * 8:(hf + 1) * 8, :], in_=wst[sbuf][:]),
                 reads=[("wst", sbuf)], writes=[("wbf", seq % 2, hf)])

    for half in range(NT // HT):
        for tt in range(HT):
            t = half * HT + tt
            P.dma(xt[tt][:], xd[t * 128:(t + 1) * 128, :], writes=[("xt", tt)])
            P.dma(sq[:], yd[t * 128:(t + 1) * 128, :], writes=["sq"])
            P.op("pool", lambda e: e.tensor_copy(out=yb[:], in_=sq[:]), reads=["sq"], writes=["yb"])
            for kc in range(16):
                P.op("pe", lambda e, kc=kc: e.transpose(out=pT[:, kc, :], in_=yb[:, kc * 128:(kc + 1) * 128], identity=ident[:]),
                     reads=["yb", "ident"], writes=["pT"])
            P.op("act", lambda e, tt=tt: e.copy(out=yT[tt][:], in_=pT[:]), writes=[("yT", tt), "pT"])
        for cc in range(4):
            c0 = cc * 512
            if cc == 0 and half == 0:
                load_w2(wseq)
                wseq += 1
            wcur = wbf2[(wseq - 1) % 2]
            wct = (wseq - 1) % 2
            if not (half == NT // HT - 1 and cc == 3):
                load_w2(wseq)
                wseq += 1
                pre_loaded = True
            for tt in range(HT):
                pb = nev % 2
                nev += 1
                for kc in range(16):
                    P.op("pe", lambda e, pb=pb, tt=tt, kc=kc, wcur=wcur: e.matmul(pY[pb][:], lhsT=yT[tt][:, kc, :], rhs=wcur[:, kc, :],
                                                                                  start=(kc == 0), stop=(kc == 15)),
                         reads=[("yT", tt), ("wbf", wct, kc // 8)], writes=[("pY", pb)])
                P.op("dve", lambda e, pb=pb, c0=c0: e.tensor_tensor(out=tmp[pb][:], in0=pY[pb][:], in1=gA[:, c0:c0 + 512], op=ALU.mult),
                     reads=["gA"], writes=[("tmp", pb), ("pY", pb)])
                P.op("pool", lambda e, pb=pb, tt=tt, c0=c0: e.tensor_tensor(out=xt[tt][:, c0:c0 + 512], in0=xt[tt][:, c0:c0 + 512],
                                                                            in1=tmp[pb][:], op=ALU.add),
                     reads=[("tmp", pb)], writes=[("xt", tt)])
        for tt in range(HT):
            t = half * HT + tt
            x1 = xt[tt]
            P.dma(x1d[t * 128:(t + 1) * 128, :], x1[:], reads=[("xt", tt)], is_output=True)
            P.op("act", lambda e, x1=x1: e.activation(out=sq[:], in_=x1[:], func=AF.Square), reads=[("xt", tt)], writes=["sq"])
            P.op("dve", lambda e: e.reduce_sum(out=st[:, 0:1], in_=sq[:], axis=AX.X), reads=["sq"], writes=["st"])
            P.op("act", lambda e: e.activation(out=st[:, 1:2], in_=st[:, 0:1], func=AF.Sqrt, scale=1.0 / D, bias=EPS), writes=["st"])
            P.op("dve", lambda e: e.reciprocal(out=st[:, 0:1], in_=st[:, 1:2]), writes=["st"])
            P.op("dve", lambda e, x1=x1: e.scalar_tensor_tensor(out=sq[:], in0=x1[:], scalar=st[:, 0:1], in1=A[:], op0=ALU.mult, op1=ALU.mult),
                 reads=[("xt", tt), "st", "A"], writes=["sq"])
            P.op("dve", lambda e: e.tensor_tensor(out=h32[:], in0=sq[:], in1=Sh[:], op=ALU.add), reads=["sq", "Sh"], writes=["h32"])
            P.op("pool", lambda e: e.tensor_copy(out=hb[:], in_=h32[:]), reads=["h32"], writes=["hb"])
            for kc in range(16):
                P.op("pe", lambda e, kc=kc: e.transpose(out=pT[:, kc, :], in_=hb[:, kc * 128:(kc + 1) * 128], identity=ident[:]),
                     reads=["hb", "ident"], writes=["pT"])
            P.op("act", lambda e: e.copy(out=hT[:], in_=pT[:]), writes=["hT", "pT"])
            P.dma(hfTd[:, :, t * 128:(t + 1) * 128], hT[:], reads=["hT"], is_output=True)
            for q4 in range(4):
                fb = q4 % 2
                for u in range(4):
                    kc = q4 * 4 + u
                    P.op("pe", lambda e, fb=fb, u=u, kc=kc: e.transpose(out=pF[fb][:, u, :], in_=h32[:, kc * 128:(kc + 1) * 128], identity=ident32[:]),
                         reads=["h32", "ident32"], writes=[("pF", fb)])
                P.op("act", lambda e, fb=fb, q4=q4: e.copy(out=hT32[:, q4 * 4:q4 * 4 + 4, :], in_=pF[fb][:]), writes=[("hT32", q4), ("pF", fb)])
            for kc in range(16):
                P.op("pe", lambda e, kc=kc: e.matmul(pL[:, 0:32], lhsT=hT32[:, kc, :], rhs=rw[:, kc, :], start=(kc == 0), stop=(kc == 15)),
                     reads=[("hT32", kc // 4), "rw"], writes=["pL"])
            P.op("dve", lambda e: e.tensor_tensor(out=lg[:], in0=pL[:, 0:32], in1=rb[:], op=ALU.add), reads=["rb"], writes=["lg", "pL"])
            P.op("dve", lambda e: e.max(out=mx[:], in_=lg[:]), reads=["lg"], writes=["mx"])
            P.op("dve", lambda e: e.tensor_scalar(out=mk[:], in0=lg[:], scalar1=mx[:, 3:4], scalar2=None, op0=ALU.is_ge), reads=["lg", "mx"], writes=["mk"])
            P.op("dve", lambda e: e.tensor_scalar(out=ex[:], in0=lg[:], scalar1=mx[:, 0:1], scalar2=None, op0=ALU.subtract), reads=["lg", "mx"], writes=["ex"])
            P.op("act", lambda e: e.activation(out=ex[:], in_=ex[:], func=AF.Exp), writes=["ex"])
            P.op("dve", lambda e: e.tensor_tensor(out=ex[:], in0=ex[:], in1=mk[:], op=ALU.mult), reads=["mk"], writes=["ex"])
            P.op("dve", lambda e: e.reduce_sum(out=s1[:, 0:1], in_=ex[:], axis=AX.X), reads=["ex"], writes=["s1"])
            P.op("dve", lambda e: e.reciprocal(out=s1[:, 1:2], in_=s1[:, 0:1]), writes=["s1"])
            P.op("dve", lambda e: e.tensor_scalar(out=lg[:], in0=ex[:], scalar1=s1[:, 1:2], scalar2=None, op0=ALU.mult), reads=["ex", "s1"], writes=["lg"])
            P.dma(Gd[t * 128:(t + 1) * 128, :], lg[:], reads=["lg"], is_output=True)
    return C.finish()


def run_k2b(ycat, x2d, w_out_l, mod_l, gffn_l, router_w_l, router_b_l):
    nc = build_k2b(8)
    ident = np.eye(128, dtype=np.float32)
    in_maps = []
    for i in range(NCORES):
        sl = slice(1024 * i, 1024 * (i + 1))
        in_maps.append({"y": np.ascontiguousarray(ycat[sl]), "x": np.ascontiguousarray(x2d[sl]), "w": w_out_l,
                        "gateA": bc(mod_l[2 * D:3 * D]), "modA": bc(mod_l[4 * D:5 * D]), "modS": bc(mod_l[3 * D:4 * D]),
                        "gN": bc(gffn_l), "rw": router_w_l, "rb": bc(router_b_l),
                        "ident": ident.astype(ml_dtypes.bfloat16), "ident32": ident})
    res = run(nc, in_maps)
    x1 = np.concatenate([r["x1"] for r in res], axis=0)
    hfT = np.concatenate([r["hfT"] for r in res], axis=2)
    G = np.concatenate([r["G"] for r in res], axis=0)
    return x1, hfT, G


def build_k3(NTG=16, NE=4):
    C = Ctx()
    P = C.P
    TOK = NTG * 512
    hTd = C.din("hT", [128, 16, TOK], BF16)
    Gbd = C.din("Gb", [NE, 128, TOK])
    wgud = C.din("wgu", [NE, D, 2 * D])
    wdnd = C.din("wdn", [NE, D, D])
    bgud = C.din("bgu", [128, NE, 32])
    bdnd = C.din("bdn", [128, NE, 16])
    outd = C.dout("outT", [128, 16, TOK])
    sgu = C.nc.dram_tensor("sgu", [NE, 32, 128, 2048], BF16).ap()
    sdn = C.nc.dram_tensor("sdn", [NE, 16, 128, 2048], BF16).ap()

    hT = C.sb("hT", [128, 16, 512], BF16)
    Gb = [C.sb("Gb%d" % i, [128, NE, 512]) for i in range(2)]
    bgu = C.sb("bgu", [128, NE, 32])
    bdn = C.sb("bdn", [128, NE, 16])
    actT = C.sb("actT", [128, NE, 16, 512], BF16)
    NST = 3
    wst = [C.sb("wst%d" % i, [128, 16, 128]) for i in range(NST)]
    NWB = 3
    wg = [C.sb("wg%d" % i, [128, 16, 128], BF16) for i in range(NWB)]
    wl = [C.sb("wl%d" % i, [128, 16, 128], BF16) for i in range(NWB)]
    gtt = [C.sb("gtt%d" % i, [128, 512]) for i in range(2)]
    stt = [C.sb("stt%d" % i, [128, 512]) for i in range(2)]
    ltt = [C.sb("ltt%d" % i, [128, 512]) for i in range(2)]
    ot = [C.sb("ot%d" % i, [128, 512]) for i in range(2)]
    pg = [C.ps("pg%d" % i, [128, 512]) for i in range(2)]
    pl = [C.ps("pl%d" % i, [128, 512]) for i in range(2)]
    po = [C.ps("po%d" % i, [128, 512]) for i in range(2)]
    P.dma(bgu[:], bgud, writes=["bgu"])
    P.dma(bdn[:], bdnd, writes=["bdn"])

    nst = [0]
    pend = []

    def fresh(src, dst, dtag, scr, stag, eng):
        s_ = nst[0] % NST
        nst[0] += 1
        P.dma(wst[s_][:], src, writes=[("wst", s_)])
        if eng == "act":
            P.op("act", lambda e, s_=s_: e.copy(out=dst, in_=wst[s_][:]), reads=[("wst", s_)], writes=[dtag])
        else:
            P.op("pool", lambda e, s_=s_: e.tensor_copy(out=dst, in_=wst[s_][:]), reads=[("wst", s_)], writes=[dtag])
        pend.append((scr, dst, dtag, stag))

    def flush(keep):
        while len(pend) > keep:
            scr, dst, dtag, stag = pend.pop(0)
            P.dma(scr.rearrange("p (a j) -> p a j", j=128), dst, reads=[dtag], writes=[stag])

    ia = 0
    ib = 0
    for tg in range(NTG):
        t0 = tg * 512
        gb = Gb[tg % 2]
        gtag = ("Gb", tg % 2)

        def load_group(tg_):
            P.dma(hT[:], hTd[:, :, tg_ * 512:(tg_ + 1) * 512], writes=["hT"])
            for e_ in range(NE):
                P.dma(Gb[tg_ % 2][:, e_, :], Gbd[e_][:, tg_ * 512:(tg_ + 1) * 512], writes=[("Gb", tg_ % 2)])
        if tg == 0:
            load_group(0)
        for e_ in range(NE):
            for c in range(16):
                b = ia % NWB
                pb = ia % 2
                ia += 1
                if tg == 0:
                    wv = wgud[e_].rearrange("(kc p) n -> p kc n", p=128)
                    fresh(wv[:, :, c * 128:(c + 1) * 128], wg[b][:], ("wg", b), sgu[e_, c], ("sgu", e_, c), "act")
                    fresh(wv[:, :, D + c * 128:D + (c + 1) * 128], wl[b][:], ("wl", b), sgu[e_, 16 + c], ("sgu", e_, 16 + c), "pool")
                    flush(2)
                else:
                    P.dma(wg[b][:], sgu[e_, c].rearrange("p (a j) -> p a j", j=128), reads=[("sgu", e_, c)], writes=[("wg", b)])
                    P.dma(wl[b][:], sgu[e_, 16 + c].rearrange("p (a j) -> p a j", j=128), reads=[("sgu", e_, 16 + c)], writes=[("wl", b)])
                for kc in range(16):
                    P.op("pe", lambda e, b=b, pb=pb, kc=kc: e.matmul(pg[pb][:], lhsT=wg[b][:, kc, :], rhs=hT[:, kc, :], start=(kc == 0), stop=(kc == 15)),
                         reads=[("wg", b), "hT"], writes=[("pg", pb)])
                for kc in range(16):
                    P.op("pe", lambda e, b=b, pb=pb, kc=kc: e.matmul(pl[pb][:], lhsT=wl[b][:, kc, :], rhs=hT[:, kc, :], start=(kc == 0), stop=(kc == 15)),
                         reads=[("wl", b), "hT"], writes=[("pl", pb)])
                b = pb
                P.op("dve", lambda e, b=b, e_=e_, c=c: e.tensor_scalar(out=gtt[b][:], in0=pg[b][:], scalar1=bgu[:, e_, c:c + 1], scalar2=7.0,
                                                                       op0=ALU.add, op1=ALU.min),
                     reads=["bgu"], writes=[("gtt", b), ("pg", b)])
                P.op("act", lambda e, b=b: e.activation(out=stt[b][:], in_=gtt[b][:], func=AF.Sigmoid, scale=1.702),
                     reads=[("gtt", b)], writes=[("stt", b)])
                P.op("dve", lambda e, b=b, e_=e_, c=c: e.tensor_scalar(out=ltt[b][:], in0=pl[b][:], scalar1=bgu[:, e_, 16 + c:17 + c], scalar2=7.0,
                                                                       op0=ALU.add, op1=ALU.min),
                     reads=["bgu"], writes=[("ltt", b), ("pl", b)])
                P.op("dve", lambda e, b=b: e.tensor_scalar(out=ltt[b][:], in0=ltt[b][:], scalar1=-7.0, scalar2=1.0, op0=ALU.max, op1=ALU.add),
                     writes=[("ltt", b)])
                P.op("pool", lambda e, b=b: e.tensor_tensor(out=gtt[b][:], in0=gtt[b][:], in1=stt[b][:], op=ALU.mult),
                     reads=[("stt", b)], writes=[("gtt", b)])
                P.op("pool", lambda e, b=b: e.tensor_tensor(out=gtt[b][:], in0=gtt[b][:], in1=ltt[b][:], op=ALU.mult),
                     reads=[("ltt", b)], writes=[("gtt", b)])
                P.op("dve", lambda e, b=b, e_=e_, c=c, gb=gb: e.tensor_tensor(out=actT[:, e_, c, :], in0=gtt[b][:], in1=gb[:, e_, :], op=ALU.mult),
                     reads=[("gtt", b), gtag], writes=[("actT", e_)])
        flush(0)
        if tg + 1 < NTG:
            load_group(tg + 1)
        for m in range(16):
            pb = m % 2
            for e_ in range(NE):
                b = ib % NWB
                ib += 1
                wt_, wtag = (wg[b], ("wg", b)) if ib % 2 else (wl[b], ("wl", b))
                if tg == 0:
                    fresh(wdnd[e_].rearrange("(c p) n -> p c n", p=128)[:, :, m * 128:(m + 1) * 128], wt_[:], wtag, sdn[e_, m], ("sdn", e_, m),
                          "act" if ib % 2 else "pool")
                    flush(1)
                else:
                    P.dma(wt_[:], sdn[e_, m].rearrange("p (a j) -> p a j", j=128), reads=[("sdn", e_, m)], writes=[wtag])
                for c in range(16):
                    P.op("pe", lambda e, wt_=wt_, c=c, e_=e_, pb=pb: e.matmul(po[pb][:], lhsT=wt_[:, c, :], rhs=actT[:, e_, c, :],
                                                                              start=(e_ == 0 and c == 0), stop=(e_ == NE - 1 and c == 15)),
                         reads=[wtag, ("actT", e_)], writes=[("po", pb)])
            P.op("dve", lambda e, pb=pb, m=m, gb=gb: e.scalar_tensor_tensor(out=ot[pb][:], in0=gb[:, 0, :], scalar=bdn[:, 0, m:m + 1], in1=po[pb][:],
                                                                            op0=ALU.mult, op1=ALU.add),
                 reads=[gtag, "bdn"], writes=[("ot", pb), ("po", pb)])
            for e_ in range(1, NE):
                P.op("dve", lambda e, pb=pb, m=m, e_=e_, gb=gb: e.scalar_tensor_tensor(out=ot[pb][:], in0=gb[:, e_, :], scalar=bdn[:, e_, m:m + 1],
                                                                                       in1=ot[pb][:], op0=ALU.mult, op1=ALU.add),
                     reads=[gtag, "bdn"], writes=[("ot", pb)])
            P.dma(outd[:, m, t0:t0 + 512], ot[pb][:], reads=[("ot", pb)], is_output=True, q="act")
        flush(0)
    return C.finish()


def run_k3(hfT, G, wgu_l, bgu_l, wdn_l, bdn_l):
    nc = build_k3(16, 4)
    in_maps = []
    for i in range(NCORES):
        es = slice(4 * i, 4 * i + 4)
        Gb = np.ascontiguousarray(np.broadcast_to(G[:, es].T[:, None, :], (4, 128, S)))
        in_maps.append({"hT": hfT, "Gb": Gb, "wgu": wgu_l[es], "wdn": wdn_l[es],
                        "bgu": np.ascontiguousarray(bgu_l[es].reshape(4, 32, 128).transpose(2, 0, 1)),
                        "bdn": np.ascontiguousarray(bdn_l[es].reshape(4, 16, 128).transpose(2, 0, 1))})
    res = run(nc, in_maps)
    return [r["outT"] for r in res]


def build_k4(NT=8, final=False):
    C = Ctx()
    P = C.P
    x1d = C.din("x1", [NT * 128, D])
    pd = C.din("parts", [NCORES, NT * 128, D])
    gFd = C.din("gateF", [128, D])
    gfd = C.din("gfin", [128, D])
    od = C.dout("out", [NT * 128, D])
    gF = C.sb("gF", [128, D])
    gfin = C.sb("gfin", [128, D])
    P.dma(gF[:], gFd, writes=["gF"])
    P.dma(gfin[:], gfd, writes=["gfin"])
    xt = [C.sb("xt%d" % i, [128, D]) for i in range(2)]
    acc = [C.sb("acc%d" % i, [128, D]) for i in range(2)]
    pt = [C.sb("pt%d" % i, [128, D]) for i in range(3)]
    sq = C.sb("sq", [128, D])
    st = C.sb("st", [128, 2])
    npt = 0
    for t in range(NT):
        k = t % 2
        P.dma(xt[k][:], x1d[t * 128:(t + 1) * 128, :], writes=[("xt", k)])
        P.dma(acc[k][:], pd[0, t * 128:(t + 1) * 128, :], writes=[("acc", k)])
        for c in range(1, NCORES):
            b = npt % 3
            npt += 1
            P.dma(pt[b][:], pd[c, t * 128:(t + 1) * 128, :], writes=[("pt", b)])
            P.op("dve" if c % 2 else "pool", lambda e, k=k, b=b: e.tensor_tensor(out=acc[k][:], in0=acc[k][:], in1=pt[b][:], op=ALU.add),
                 reads=[("pt", b)], writes=[("acc", k)])
        P.op("dve", lambda e, k=k: e.tensor_tensor(out=acc[k][:], in0=acc[k][:], in1=gF[:], op=ALU.mult), reads=["gF"], writes=[("acc", k)])
        P.op("pool", lambda e, k=k: e.tensor_tensor(out=acc[k][:], in0=acc[k][:], in1=xt[k][:], op=ALU.add), reads=[("xt", k)], writes=[("acc", k)])
        if final:
            P.op("act", lambda e, k=k: e.activation(out=sq[:], in_=acc[k][:], func=AF.Square), reads=[("acc", k)], writes=["sq"])
            P.op("dve", lambda e: e.reduce_sum(out=st[:, 0:1], in_=sq[:], axis=AX.X), reads=["sq"], writes=["st"])
            P.op("act", lambda e: e.activation(out=st[:, 1:2], in_=st[:, 0:1], func=AF.Sqrt, scale=1.0 / D, bias=EPS), writes=["st"])
            P.op("dve", lambda e: e.reciprocal(out=st[:, 0:1], in_=st[:, 1:2]), writes=["st"])
            P.op("dve", lambda e, k=k: e.scalar_tensor_tensor(out=acc[k][:], in0=acc[k][:], scalar=st[:, 0:1], in1=gfin[:], op0=ALU.mult, op1=ALU.mult),
                 reads=["st", "gfin"], writes=[("acc", k)])
        P.dma(od[t * 128:(t + 1) * 128, :], acc[k][:], reads=[("acc", k)], is_output=True)
    return C.finish()


def run_k4(x1, parts, gate_f, gfin, final):
    nc = build_k4(8, final)
    in_maps = []
    for i in range(NCORES):
        sl = slice(1024 * i, 1024 * (i + 1))
        pp = np.stack([np.ascontiguousarray(p[:, :, sl].transpose(2, 1, 0)).reshape(1024, D) for p in parts])
        in_maps.append({"x1": np.ascontiguousarray(x1[sl]), "parts": pp, "gateF": bc(gate_f), "gfin": bc(gfin)})
    res = run(nc, in_maps)
    return np.concatenate([r["out"] for r in res], axis=0)


LAYER_KEYS = ("conv_dw_w", "conv_dw_b", "conv_ln_g", "conv_ln_b", "nsa_pe_k", "nsa_pe_v", "nsa_cmp_k_w1", "nsa_cmp_k_w2",
              "nsa_cmp_v_w1", "nsa_cmp_v_w2", "ret_gn_g", "ret_gn_b")


def kernel(x, c, ada_w, ada_b, norm_mix_g, w_in, conv_dw_w, conv_dw_b, conv_ln_g, conv_ln_b,
           nsa_pe_k, nsa_pe_v, nsa_cmp_k_w1, nsa_cmp_k_w2, nsa_cmp_v_w1, nsa_cmp_v_w2,
           ret_gn_g, ret_gn_b, w_out, norm_ffn_g, router_w, router_b,
           moe_w_gate_up, moe_b_gate_up, moe_w_down, moe_b_down, final_norm_g):
    loc = locals()
    f = lambda a: np.asarray(a, dtype=np.float32)
    xs = f(x)[0]
    mod = run_k0(f(c), f(ada_w), f(ada_b))
    tabs = rot_tables()
    for l in range(DEPTH):
        lw = {k: f(loc[k][l]) for k in LAYER_KEYS}
        p32, p16 = run_k1(xs, f(w_in[l]), mod[l], f(norm_mix_g[l]), tabs)
        ycat = run_k2a(p32, p16, lw)
        del p32, p16
        x1, hfT, G = run_k2b(ycat, xs, f(w_out[l]), mod[l], f(norm_ffn_g[l]), f(router_w[l]), f(router_b[l]))
        parts = run_k3(hfT, G, np.asarray(moe_w_gate_up[l]), f(moe_b_gate_up[l]), np.asarray(moe_w_down[l]), f(moe_b_down[l]))
        xs = run_k4(x1, parts, mod[l][5 * D:6 * D], f(final_norm_g), l == DEPTH - 1)
        del parts
    return xs[None].astype(np.float32)
```
